# Optimizing a Trainium2 kernel written in Bass

```python
import math
import jax, jax.numpy as jnp
from jax import lax
import numpy as np

D_MODEL = 4096
BATCH = 2
SEQ = 4096
DEPTH = 2

N_A_LAYERS = DEPTH // 2
N_B_LAYERS = DEPTH - N_A_LAYERS
N_HEADS = 32
HEAD_DIM = D_MODEL // N_HEADS
NSA_KV_HEADS = 4
NSA_CMP_LEN = 32
NSA_CMP_STRIDE = 16
NSA_SEL_BLOCK = 64
NSA_SEL_TOPN = 16
NSA_WINDOW = 512
NSA_Q_CHUNK = 64
NSA_N_BRANCH = 3
NSA_IN_WIDTH = (N_HEADS * HEAD_DIM + NSA_N_BRANCH * 2 * NSA_KV_HEADS * HEAD_DIM
                + NSA_N_BRANCH * N_HEADS * HEAD_DIM + NSA_N_BRANCH * N_HEADS)
MOBA_KV_HEADS = 8
MOBA_BLOCK = 256
MOBA_TOPK = 3
MOBA_Q_CHUNK = 16
RMS_EPS = 1e-6
NEG_INF = -1e30
FORCE_SCORE = 1e9

kernel_name = "yoco_nsa_moba_alibi_hybrid"


def rmsnorm(x, g):
    xf = x.astype(jnp.float32)
    y = xf * lax.rsqrt(jnp.mean(xf * xf, axis=-1, keepdims=True) + RMS_EPS)
    return (y * g.astype(jnp.float32)).astype(x.dtype)


def alibi_slopes(n_heads):
    return 2.0 ** (-8.0 * jnp.arange(1, n_heads + 1, dtype=jnp.float32) / n_heads)


def nsa_mixer(h, norm_g, w_in, pos_k, pos_v, w1_k, w2_k, w1_v, w2_v, w_out):
    B, S, _ = h.shape
    H, Dh, G = N_HEADS, HEAD_DIM, NSA_KV_HEADS
    hpg = H // G
    L, d, Ls, W, Cq = NSA_CMP_LEN, NSA_CMP_STRIDE, NSA_SEL_BLOCK, NSA_WINDOW, NSA_Q_CHUNK
    xn = rmsnorm(h, norm_g)
    proj = xn @ w_in
    q_end = H * Dh
    kv_end = q_end + NSA_N_BRANCH * 2 * G * Dh
    z_end = kv_end + NSA_N_BRANCH * H * Dh
    q = proj[..., :q_end].reshape(B, S, G, hpg, Dh).transpose(0, 2, 3, 1, 4) * (Dh ** -0.5)
    kv = proj[..., q_end:kv_end].reshape(B, S, NSA_N_BRANCH, 2, G, Dh).transpose(2, 3, 0, 4, 1, 5)
    z = proj[..., kv_end:z_end].reshape(B, S, NSA_N_BRANCH, H, Dh)
    gates = jax.nn.sigmoid(proj[..., z_end:].astype(jnp.float32)).reshape(B, S, NSA_N_BRANCH, H)
    k_cmp_raw, v_cmp_raw = kv[0, 0], kv[0, 1]
    k_slc, v_slc = kv[1, 0], kv[1, 1]
    k_win, v_win = kv[2, 0], kv[2, 1]

    n_cmp = (S - L) // d + 1
    cmp_idx = jnp.arange(n_cmp)[:, None] * d + jnp.arange(L)[None, :]
    cmp_end = cmp_idx[:, -1]

    def compress(raw, pos, w1, w2):
        blk = raw[:, :, cmp_idx] + pos
        hid = jax.nn.silu(jnp.einsum('bgnld,lde->bgne', blk, w1))
        return hid @ w2

    k_c = compress(k_cmp_raw, pos_k, w1_k, w2_k)
    v_c = compress(v_cmp_raw, pos_v, w1_v, w2_v)

    n_slc = S // Ls
    n_top = min(NSA_SEL_TOPN, n_slc)
    ks_blk = k_slc.reshape(B, G, n_slc, Ls, Dh)
    vs_blk = v_slc.reshape(B, G, n_slc, Ls, Dh)
    slc_start = jnp.arange(n_slc) * Ls
    overlap = ((cmp_idx[:, :1] <= (slc_start + Ls - 1)[None, :]) &
               (cmp_end[:, None] >= slc_start[None, :])).astype(jnp.float32)

    pad = ((0, 0), (0, 0), (W, 0), (0, 0))
    kw_p = jnp.pad(k_win, pad)
    vw_p = jnp.pad(v_win, pad)

    sl = alibi_slopes(H).reshape(G, hpg)[None, :, :, None, None]
    bi = jnp.arange(B)[:, None, None, None]
    gi = jnp.arange(G)[None, :, None, None]
    j_slc = jnp.arange(n_slc)

    def chunk(c):
        t0 = c * Cq
        tq = t0 + jnp.arange(Cq)
        qc = lax.dynamic_slice_in_dim(q, t0, Cq, axis=3)
        dist_c = (tq[:, None] - cmp_end[None, :]).astype(jnp.float32)
        mask_c = dist_c >= 0
        s_c = jnp.einsum('bghqd,bgnd->bghqn', qc, k_c).astype(jnp.float32) - sl * dist_c
        p_c = jnp.where(mask_c, jax.nn.softmax(jnp.where(mask_c, s_c, NEG_INF), axis=-1), 0.0)
        o_c = jnp.einsum('bghqn,bgnd->bghqd', p_c.astype(v_c.dtype), v_c)
        imp = jnp.einsum('bghqn,nj->bgqj', p_c, overlap)
        blk_q = (tq // Ls)[:, None]
        forced = (j_slc[None, :] == 0) | (j_slc[None, :] == blk_q) | (j_slc[None, :] == blk_q - 1)
        imp = jnp.where(forced, FORCE_SCORE, jnp.where(j_slc[None, :] > blk_q, NEG_INF, imp))
        _, sel = lax.top_k(imp, n_top)
        kg = ks_blk[bi, gi, sel].reshape(B, G, Cq, n_top * Ls, Dh)
        vg = vs_blk[bi, gi, sel].reshape(B, G, Cq, n_top * Ls, Dh)
        pos_s = (sel[..., None] * Ls + jnp.arange(Ls)).reshape(B, G, Cq, n_top * Ls)
        dist_s = (tq[None, None, :, None] - pos_s)[:, :, None].astype(jnp.float32)
        mask_s = dist_s >= 0
        s_s = jnp.einsum('bghqd,bgqkd->bghqk', qc, kg).astype(jnp.float32) - sl * dist_s
        p_s = jax.nn.softmax(jnp.where(mask_s, s_s, NEG_INF), axis=-1)
        o_s = jnp.einsum('bghqk,bgqkd->bghqd', p_s.astype(vg.dtype), vg)
        kw = lax.dynamic_slice_in_dim(kw_p, t0, W + Cq, axis=2)
        vw = lax.dynamic_slice_in_dim(vw_p, t0, W + Cq, axis=2)
        pos_w = t0 - W + jnp.arange(W + Cq)
        dist_w = tq[:, None] - pos_w[None, :]
        mask_w = (dist_w >= 0) & (dist_w < W)
        s_w = jnp.einsum('bghqd,bgkd->bghqk', qc, kw).astype(jnp.float32) - sl * dist_w.astype(jnp.float32)
        p_w = jax.nn.softmax(jnp.where(mask_w, s_w, NEG_INF), axis=-1)
        o_w = jnp.einsum('bghqk,bgkd->bghqd', p_w.astype(vw.dtype), vw)
        return jnp.stack([o_c, o_s, o_w], axis=0)

    outs = lax.map(chunk, jnp.arange(S // Cq))
    O = outs.transpose(2, 0, 5, 1, 3, 4, 6).reshape(B, S, NSA_N_BRANCH, H, Dh)
    mix = jnp.sum(gates.astype(O.dtype)[..., None] * O * jax.nn.silu(z), axis=2)
    return mix.reshape(B, S, H * Dh) @ w_out


def moba_shared_kv(h, norm_g, w_kv):
    B, S, _ = h.shape
    Gm, Dh, Bk = MOBA_KV_HEADS, HEAD_DIM, MOBA_BLOCK
    nb = -(-S // Bk)
    kv = (rmsnorm(h, norm_g) @ w_kv).reshape(B, S, 2, Gm, Dh).transpose(2, 0, 3, 1, 4)
    kv = jnp.pad(kv, ((0, 0), (0, 0), (0, 0), (0, nb * Bk - S), (0, 0)))
    kb = kv[0].reshape(B, Gm, nb, Bk, Dh)
    vb = kv[1].reshape(B, Gm, nb, Bk, Dh)
    kmean = jnp.mean(kb.astype(jnp.float32), axis=3).astype(kb.dtype)
    return kb, vb, kmean


def moba_mixer(h, norm_g, w_in, w_out, kb, vb, kmean):
    B, S, _ = h.shape
    H, Dh, Gm, Bk, Cq = N_HEADS, HEAD_DIM, MOBA_KV_HEADS, MOBA_BLOCK, MOBA_Q_CHUNK
    hpg = H // Gm
    nb = kb.shape[2]
    proj = rmsnorm(h, norm_g) @ w_in
    q = proj[..., :H * Dh].reshape(B, S, Gm, hpg, Dh).transpose(0, 2, 3, 1, 4) * (Dh ** -0.5)
    z = proj[..., H * Dh:].reshape(B, S, H, Dh)
    blk_t = jnp.arange(S) // Bk
    past = jnp.arange(nb)[None, :] < blk_t[:, None]
    s_blk = jnp.einsum('bghsd,bgnd->bghsn', q, kmean).astype(jnp.float32)
    s_blk = jnp.where(past, s_blk, NEG_INF)
    _, top_idx = lax.top_k(s_blk, min(MOBA_TOPK, nb))
    top_ok = top_idx < blk_t[:, None]
    k_top = top_idx.shape[-1]

    sm = alibi_slopes(H).reshape(Gm, hpg)[None, :, :, None, None]
    bi = jnp.arange(B)[:, None, None, None, None]
    gi = jnp.arange(Gm)[None, :, None, None, None]

    def chunk(c):
        t0 = c * Cq
        tq = t0 + jnp.arange(Cq)
        qc = lax.dynamic_slice_in_dim(q, t0, Cq, axis=3)
        idx = lax.dynamic_slice_in_dim(top_idx, t0, Cq, axis=3)
        ok = lax.dynamic_slice_in_dim(top_ok, t0, Cq, axis=3)
        own = t0 // Bk
        k_own = lax.dynamic_index_in_dim(kb, own, axis=2, keepdims=False)
        v_own = lax.dynamic_index_in_dim(vb, own, axis=2, keepdims=False)
        dist_o = tq[:, None] - (own * Bk + jnp.arange(Bk))[None, :]
        s_o = jnp.einsum('bghqd,bgkd->bghqk', qc, k_own).astype(jnp.float32) - sm * dist_o.astype(jnp.float32)
        s_o = jnp.where(dist_o >= 0, s_o, NEG_INF)
        kg = kb[bi, gi, idx].reshape(B, Gm, hpg, Cq, k_top * Bk, Dh)
        vg = vb[bi, gi, idx].reshape(B, Gm, hpg, Cq, k_top * Bk, Dh)
        pos_g = (idx[..., None] * Bk + jnp.arange(Bk)).reshape(B, Gm, hpg, Cq, k_top * Bk)
        ok_g = jnp.broadcast_to(ok[..., None], idx.shape + (Bk,)).reshape(B, Gm, hpg, Cq, k_top * Bk)
        dist_g = (tq[:, None] - pos_g).astype(jnp.float32)
        s_g = jnp.einsum('bghqd,bghqkd->bghqk', qc, kg).astype(jnp.float32) - sm * dist_g
        s_g = jnp.where(ok_g, s_g, NEG_INF)
        p = jax.nn.softmax(jnp.concatenate([s_o, s_g], axis=-1), axis=-1)
        p_o, p_g = p[..., :Bk], p[..., Bk:]
        return (jnp.einsum('bghqk,bgkd->bghqd', p_o.astype(v_own.dtype), v_own)
                + jnp.einsum('bghqk,bghqkd->bghqd', p_g.astype(vg.dtype), vg))

    outs = lax.map(chunk, jnp.arange(S // Cq))
    o = outs.transpose(1, 0, 4, 2, 3, 5).reshape(B, S, H, Dh)
    return (o * jax.nn.silu(z)).reshape(B, S, H * Dh) @ w_out


def setup_inputs(seed: int = 0) -> dict:
    key = jax.random.key(seed)
    ks = jax.random.split(key, 20)
    D, H, Dh, L = D_MODEL, N_HEADS, HEAD_DIM, NSA_CMP_LEN
    nA, nB = N_A_LAYERS, N_B_LAYERS
    f32 = jnp.float32

    def nrm(k, shape, scale):
        return jax.random.normal(k, shape, f32) * scale

    return {
        "x": nrm(ks[0], (BATCH, SEQ, D), 1.0),
        "a_norm_g": 1.0 + nrm(ks[1], (nA, D), 0.01),
        "a_w_in": nrm(ks[2], (nA, D, NSA_IN_WIDTH), D ** -0.5),
        "a_cmp_pos_k": nrm(ks[3], (nA, L, Dh), 0.02),
        "a_cmp_pos_v": nrm(ks[4], (nA, L, Dh), 0.02),
        "a_cmp_w1_k": nrm(ks[5], (nA, L, Dh, Dh), (L * Dh) ** -0.5),
        "a_cmp_w2_k": nrm(ks[6], (nA, Dh, Dh), Dh ** -0.5),
        "a_cmp_w1_v": nrm(ks[7], (nA, L, Dh, Dh), (L * Dh) ** -0.5),
        "a_cmp_w2_v": nrm(ks[8], (nA, Dh, Dh), Dh ** -0.5),
        "a_w_out": nrm(ks[9], (nA, H * Dh, D), (H * Dh) ** -0.5),
        "kv_norm_g": 1.0 + nrm(ks[10], (D,), 0.01),
        "kv_w": nrm(ks[11], (D, 2 * MOBA_KV_HEADS * Dh), D ** -0.5),
        "b_norm_g": 1.0 + nrm(ks[12], (nB, D), 0.01),
        "b_w_in": nrm(ks[13], (nB, D, 2 * H * Dh), D ** -0.5),
        "b_w_out": nrm(ks[14], (nB, H * Dh, D), (H * Dh) ** -0.5),
        "final_norm_g": 1.0 + nrm(ks[15], (D,), 0.01),
    }


def reference(x, a_norm_g, a_w_in, a_cmp_pos_k, a_cmp_pos_v, a_cmp_w1_k, a_cmp_w2_k,
              a_cmp_w1_v, a_cmp_w2_v, a_w_out, kv_norm_g, kv_w, b_norm_g, b_w_in,
              b_w_out, final_norm_g):
    h = x
    kb = vb = kmean = None
    for layer in range(DEPTH):
        if layer < N_A_LAYERS:
            h = h + nsa_mixer(h, a_norm_g[layer], a_w_in[layer], a_cmp_pos_k[layer],
                              a_cmp_pos_v[layer], a_cmp_w1_k[layer], a_cmp_w2_k[layer],
                              a_cmp_w1_v[layer], a_cmp_w2_v[layer], a_w_out[layer])
        else:
            if layer == N_A_LAYERS:
                kb, vb, kmean = moba_shared_kv(h, kv_norm_g, kv_w)
            i = layer - N_A_LAYERS
            h = h + moba_mixer(h, b_norm_g[i], b_w_in[i], b_w_out[i], kb, vb, kmean)
    return rmsnorm(h, final_norm_g)
```

```python
import os, sys, time
import numpy as np
import concourse.bass as bass
import concourse.mybir as mybir
from concourse.bass_utils import run_bass_kernel_spmd
from contextlib import ExitStack
import ml_dtypes

F32 = mybir.dt.float32
BF16 = mybir.dt.bfloat16
AF = mybir.ActivationFunctionType
ALU = mybir.AluOpType
AX = mybir.AxisListType
NPBF = ml_dtypes.bfloat16


class Buf:
    __slots__ = ("name", "w", "r", "excl")

    def __init__(self, name="", excl=False):
        self.name = name
        self.excl = excl
        self.w = {}
        self.r = {}


class Prog:
    NDMA = 16

    def __init__(self, nc, stack):
        self.nc = nc
        self.E = {"pe": nc.tensor, "act": nc.scalar, "dve": nc.vector, "pool": nc.gpsimd, "sp": nc.sync}
        self.sems = {}
        self.stack = stack
        self.epoch = 0
        self.cur = {}
        for k in self.E:
            self.cur[k] = k + "_0"
            self.sems[k + "_0"] = stack.enter_context(nc.semaphore("sem_" + k + "_0"))
        for i in range(self.NDMA):
            self.sems["d%d" % i] = stack.enter_context(nc.semaphore("sem_d%d" % i))
        self.cnt = {k: 0 for k in self.sems}
        self.waited = {k: {} for k in self.E}
        self.dma_rr = 0
        self.nwaits = 0
        self.ninst = 0

    def new_epoch(self):
        self.epoch += 1
        for k in self.E:
            sk = "%s_%d" % (k, self.epoch)
            self.cur[k] = sk
            self.sems[sk] = self.stack.enter_context(self.nc.semaphore("sem_" + sk))
            self.cnt[sk] = 0

    def _wait(self, eng, sk, v):
        w = self.waited[eng]
        if w.get(sk, 0) >= v:
            return
        if sk == self.cur[eng] and v <= self.cnt[sk] - 64:
            return
        w[sk] = v
        self.E[eng].wait_ge(self.sems[sk], v)
        self.nwaits += 1

    def _deps(self, eng, reads, writes):
        toks = {}
        for b in reads:
            for sk, v in b.w.items():
                if eng == "pe" and sk.startswith("pe_"):
                    continue
                if toks.get(sk, 0) < v:
                    toks[sk] = v
            if b.excl:
                for sk, v in b.r.items():
                    if sk.startswith(eng + "_"):
                        continue
                    if toks.get(sk, 0) < v:
                        toks[sk] = v
        for b in writes:
            for d in (b.w, b.r):
                for sk, v in d.items():
                    if sk.startswith(eng + "_"):
                        continue
                    if toks.get(sk, 0) < v:
                        toks[sk] = v
        for sk, v in toks.items():
            self._wait(eng, sk, v)

    def _commit(self, sk, v, reads, writes):
        for b in reads:
            if b.r.get(sk, 0) < v:
                b.r[sk] = v
        for b in writes:
            if b.w.get(sk, 0) < v:
                b.w[sk] = v

    def op(self, eng, fn, reads=(), writes=()):
        self._deps(eng, reads, writes)
        ins = fn(self.E[eng])
        sk = self.cur[eng]
        self.cnt[sk] += 1
        ins.then_inc(self.sems[sk], 1)
        self.ninst += 1
        self._commit(sk, self.cnt[sk], reads, writes)

    def dma(self, q, out, in_, reads=(), writes=(), **kw):
        sk = "d%d" % self.dma_rr
        self.dma_rr = (self.dma_rr + 1) % self.NDMA
        if self.cnt[sk] > 0:
            self._wait(q, sk, self.cnt[sk])
        self._deps(q, reads, writes)
        ins = self.E[q].dma_start(out=out, in_=in_, **kw)
        self.cnt[sk] += 16
        ins.then_inc(self.sems[sk], 16)
        self.ninst += 1
        self._commit(sk, self.cnt[sk], reads, writes)

    def barrier(self):
        toks = {sk: c for sk, c in self.cnt.items() if c > 0}
        for eng in self.E:
            for sk, v in toks.items():
                if sk == self.cur[eng]:
                    continue
                self._wait(eng, sk, v)

    def finish(self):
        for sk in self.sems:
            if sk.startswith("d") and self.cnt[sk] > 0:
                self._wait("sp", sk, self.cnt[sk])


def w_layout(w):
    return np.ascontiguousarray(w.reshape(32, 128, -1).transpose(1, 0, 2))


def bf16_split3(v):
    v = v.astype(np.float32)
    r0 = v.astype(NPBF)
    e1 = v - r0.astype(np.float32)
    r1 = e1.astype(NPBF)
    e2 = e1 - r1.astype(np.float32)
    r2 = e2.astype(NPBF)
    return np.stack([r0, r1, r2])


S = 4096
D = 4096
DH = 128
TB = 512
EPS = 1e-6
NEGM = -30000.0
QSCALE = DH ** -0.5
NQK = 1536
NVG = 280
NZ = 3072
NCOL = NQK + NVG + NZ


def nsa_tables(g):
    hl = np.arange(8)
    m = (2.0 ** (-8.0 * (8 * g + hl + 1) / 32)).astype(np.float32)
    p = np.arange(128)
    t = {}
    dl = np.arange(32) - 28
    kb = m[None, :, None] * (128.0 * dl[None, None, :] + p[:, None, None])
    t["kb"] = kb.astype(np.float32)
    ct = np.arange(2); qt = np.arange(8)
    kbc = m[None, :, None, None] * (16.0 * (128 * ct[None, None, :, None] + p[:, None, None, None]) + 31 - 512.0 * qt[None, None, None, :])
    t["kbc"] = kbc.astype(np.float32).reshape(128, 8, 16)
    c = np.arange(512, dtype=np.float32)
    v = -(m[:, None] * c[None, :]).astype(np.float32)
    t["rsinit"] = bf16_split3(v)
    ex = np.zeros((67, 32, 128), np.float32)
    for kt in range(32):
        for pp in range(128):
            ex[2 * kt + pp // 64, kt, pp] = 30000.0
    ex[64:67] = 1.0
    t["ex"] = ex.astype(NPBF)
    r = p[:, None]; cc = np.arange(128)[None, :]
    t["tric"] = np.where(r > cc, NEGM, 0.0).astype(NPBF)
    t["triw"] = np.where(r <= cc, NEGM, 0.0).astype(NPBF)
    cm = np.zeros((128, 8, 2, 512), np.float32)
    for q_ in range(8):
        for c_ in range(2):
            n = 128 * c_ + p
            tt = 512 * q_ + np.arange(512)
            valid = (16 * n[:, None] + 31 <= tt[None, :]) & (n[:, None] <= 254)
            cm[:, q_, c_, :] = np.where(valid, 0.0, NEGM)
    t["cmpm"] = cm.astype(NPBF)
    tq = np.arange(S); j = np.arange(64)
    bq = tq // 64
    forced = (j[None, :] == 0) | (j[None, :] == bq[:, None]) | (j[None, :] == bq[:, None] - 1)
    fut = j[None, :] > bq[:, None]
    t["bimp"] = np.where(forced, 1e9, np.where(fut, -1e30, 0.0)).astype(np.float32)
    n = np.arange(256)
    ov = ((16 * n[:, None] <= 64 * j[None, :] + 63) & (16 * n[:, None] + 31 >= 64 * j[None, :]) & (n[:, None] <= 254))
    t["ov"] = np.ascontiguousarray(ov.reshape(2, 128, 64).transpose(1, 0, 2)).astype(NPBF)
    t["idn"] = np.eye(128, dtype=np.float32).astype(NPBF)
    dd = np.arange(1, 512, dtype=np.float64)
    e = np.exp(-m.astype(np.float64)[:, None] * dd[None, :])
    csum = np.concatenate([np.cumsum(e[:, ::-1], axis=1)[:, ::-1], np.zeros((8, 1))], axis=1)
    tt = np.arange(512)
    wx = csum[:, tt]
    t["winx"] = np.ascontiguousarray(wx.T.reshape(4, 128, 8).transpose(1, 0, 2)).astype(np.float32)
    return t


def nsa_weight_cols(g):
    H, G = 32, 4
    q_end = H * DH
    kv_end = q_end + 3 * 2 * G * DH
    z_end = kv_end + 3 * H * DH
    cols = []
    d = np.arange(DH)
    for hl in range(8):
        cols.append((8 * g + hl) * DH + d)
    kvcol = lambda br, kvi: q_end + ((br * 2 + kvi) * G + g) * DH + d
    cols += [kvcol(0, 0), kvcol(1, 0), kvcol(2, 0), kvcol(0, 1)]
    cols += [kvcol(1, 1), kvcol(2, 1)]
    cols.append(np.array([z_end + br * H + 8 * g + hl for br in range(3) for hl in range(8)]))
    for br in range(3):
        for hl in range(8):
            cols.append(kv_end + (br * H + 8 * g + hl) * DH + d)
    cols = np.concatenate(cols)
    assert cols.shape[0] == NCOL
    return cols


def build_nsa(NTB=8, stage=9, dbg=False, ctx=None, io=None):
    nc = bass.Bass("TRN2", target_bir_lowering=False) if ctx is None else ctx["nc"]
    pfx = "" if ctx is None else ctx["pfx"]

    def dram(name, shape, dt, kind="ExternalInput"):
        if io is not None and name in io:
            return io[name]
        return nc.dram_tensor(pfx + name, shape, dt, kind=kind).ap()
    x = dram("x", [S, D], F32)
    gcol_d = dram("gcol", [128, 32], F32)
    w = dram("w", [128, 32, NCOL], F32)
    w1k_d = dram("w1k", [128, 32, 128], F32); w1v_d = dram("w1v", [128, 32, 128], F32)
    w2k_d = dram("w2k", [128, 128], F32); w2v_d = dram("w2v", [128, 128], F32)
    posk_d = dram("posk", [128, 32], F32); posv_d = dram("posv", [128, 32], F32)
    kb_d = dram("kb", [128, 8, 32], F32); kbc_d = dram("kbc", [128, 8, 16], F32)
    rsinit_d = dram("rsinit", [3, 8, 512], BF16)
    ex_d = dram("ex", [67, 32, 128], BF16)
    tric_d = dram("tric", [128, 128], BF16); triw_d = dram("triw", [128, 128], BF16)
    cmpm_d = dram("cmpm", [128, 8, 2, 512], BF16)
    bimp_d = dram("bimp", [S, 64], F32)
    ov_d = dram("ov", [128, 2, 64], BF16)
    idn_d = dram("idn", [128, 128], BF16)
    winx_d = dram("winx", [128, 4, 8], F32)
    mix_d = dram("mix", [S, 1024], BF16, kind="ExternalOutput")

    with ExitStack() as st:
        P = Prog(nc, st) if ctx is None else ctx["P"]
        sb = lambda name, shape, dt: st.enter_context(nc.sbuf_tensor(pfx + "s_" + name, shape, dt))
        ps = lambda name, shape, dt: st.enter_context(nc.psum_tensor(pfx + "p_" + name, shape, dt))
        cB = Buf("const")
        ident = sb("ident", [128, 128], BF16)
        gcol = sb("gcol", [128, 32], F32)
        kb = sb("kb", [128, 8, 32], F32); kbc = sb("kbc", [128, 8, 16], F32)
        EX = sb("EX", [67, 32, 128], BF16)
        tric = sb("tric", [128, 128], BF16); triw = sb("triw", [128, 128], BF16)
        OV = sb("OV", [128, 2, 64], BF16)
        w2k = sb("w2k", [128, 128], BF16); w2v = sb("w2v", [128, 128], BF16)
        posk = sb("posk", [128, 32], BF16); posv = sb("posv", [128, 32], BF16)
        RS = sb("RS", [67, 8, 512], BF16); b_RS = Buf("RS")
        winx = sb("winx", [128, 4, 8], F32)
        for (dst, src, q) in [(winx, winx_d, "sp"), (ident, idn_d, "sp"), (gcol, gcol_d, "sp"), (kb, kb_d, "sp"), (kbc, kbc_d, "sp"),
                              (EX, ex_d, "sp"), (tric, tric_d, "sp"), (triw, triw_d, "sp"), (OV, ov_d, "sp"),
                              (w2k, w2k_d, "pool"), (w2v, w2v_d, "pool"), (posk, posk_d, "pool"), (posv, posv_d, "pool")]:
            bb = Buf()
            P.dma(q, dst[:], src, writes=[bb])
            for sk, v in bb.w.items():
                cB.w[sk] = max(cB.w.get(sk, 0), v)
        P.dma("sp", RS[64:67, :, :], rsinit_d, writes=[b_RS])
        epst = sb("epst", [128, 1], F32)
        P.op("dve", lambda e: e.memset(epst[:], EPS), writes=[cB])
        kT = sb("kT", [128, 2, S], BF16); b_kT = Buf("kT")
        Vaug = sb("Vaug", [128, 2, 32, 129], BF16); b_V = Buf("V")
        rawT = sb("rawT", [128, 2, 528], BF16); b_raw = Buf("raw")
        kcT = sb("kcT", [128, 256], BF16); b_kc = Buf("kc")
        vcaug = sb("vcaug", [128, 2, 129], BF16); b_vc = Buf("vc")
        cbias = sb("cbias", [128, 2], F32); b_cbias = Buf("cbias")
        P.op("dve", lambda e: e.memset(Vaug[:], 1.0), writes=[b_V])
        P.op("dve", lambda e: e.memset(rawT[:], 0.0), writes=[b_raw])
        P.op("dve", lambda e: e.memset(kcT[:], 0.0), writes=[b_kc])
        P.op("dve", lambda e: e.memset(vcaug[:], 0.0), writes=[b_vc])
        P.op("dve", lambda e: e.memset(vcaug[:, :, 128:129], 1.0), writes=[b_vc])
        xf = sb("xf", [128, D], F32); b_xf = Buf("xf")
        xh = sb("xh", [128, D], BF16); b_xh = Buf("xh")
        junk = xh
        ss = sb("ss", [128, 4], F32); b_ss = Buf("ss")
        xnT = sb("xnT", [128, 32, TB], BF16); b_xnT = [Buf("xnT%d" % i) for i in range(4)]
        NW = 4
        wt = [sb("wt%d" % i, [128, 8, 512], BF16) for i in range(NW)]; b_wt = [Buf("wt%d" % i) for i in range(NW)]
        qT = sb("qT", [128, 8, TB], BF16); b_qT = Buf("qT")
        zs = sb("zs", [128, 4, NZ], BF16); b_zs = Buf("zs")
        gates = sb("gates", [128, 4, 24], F32); b_gates = Buf("gates")
        mix = sb("mix", [128, 4, 1024], F32); b_mix = Buf("mix")
        NPT = 4
        PT = [sb("PT%d" % i, [128, TB], BF16) for i in range(NPT)]; b_PT = [Buf("PT%d" % i) for i in range(NPT)]
        cmpm = sb("cmpm", [128, 2, 512], BF16); b_cmpm = Buf("cmpm")
        bimp = sb("bimp", [128, 4, 64], F32); b_bimp = Buf("bimp")
        impacc = sb("impacc", [128, 4, 64], F32); b_imp = Buf("imp")
        impw = sb("impw", [128, 4, 64], F32); b_impw = Buf("impw")
        top = sb("top", [128, 4, 16], F32); b_top = Buf("top")
        selm = sb("selm", [128, 4, 64], BF16); b_selm = Buf("selm")
        rsb = sb("rsb", [128, 8], F32); b_rsb = Buf("rsb")
        otmp = sb("otmp", [128, 4, 128], F32); b_otmp = Buf("otmp")
        hid = sb("hid", [128, 2, 32], BF16); b_hid = Buf("hid")
        vctmp = sb("vctmp", [32, 128], BF16); b_vctmp = Buf("vctmp")
        pA = [ps("pA%d" % i, [128, 512], F32) for i in range(4)]; b_pA = [Buf("pA%d" % i, True) for i in range(4)]
        pO = [ps("pO%d" % i, [128, 2, 256], F32) for i in range(2)]; b_pO = [Buf("pO%d" % i, True) for i in range(2)]
        pT = [ps("pT%d" % i, [128, 8, 128], BF16) for i in range(2)]; b_pT = [Buf("pT%d" % i, True) for i in range(2)]
        print("sbuf remaining", nc.sbuf_bytes_remaining, flush=True)

        state = {"wi": 0, "pti": 0, "pai": 0, "ptr": 0, "alt": 0}

        def evac_engine():
            state["alt"] ^= 1
            return "dve" if state["alt"] else "act"

        def front_end(tb):
            t0 = tb * TB
            for tt in range(4):
                P.dma("sp", xf[:], x[t0 + tt * 128: t0 + (tt + 1) * 128, :], writes=[b_xf])
                P.op("dve", lambda e: e.memset(ss[:, 0:1], 0.0), writes=[b_ss])
                P.op("act", lambda e, tt=tt: e.activation(out=junk[:], in_=xf[:], func=AF.Square, accum_out=ss[:, 0:1]),
                     reads=[b_xf], writes=[b_xh, b_ss])
                P.op("act", lambda e: e.activation(out=ss[:, 1:2], in_=ss[:, 0:1], func=AF.Ln, bias=epst[:], scale=1.0 / D),
                     reads=[b_ss, cB], writes=[b_ss])
                P.op("act", lambda e: e.activation(out=ss[:, 2:3], in_=ss[:, 1:2], func=AF.Exp, scale=-0.5),
                     reads=[b_ss], writes=[b_ss])
                P.op("dve", lambda e: e.tensor_scalar(out=xh[:], in0=xf[:], scalar1=ss[:, 2:3], scalar2=None, op0=ALU.mult),
                     reads=[b_xf, b_ss], writes=[b_xh])
                for k8 in range(4):
                    pt = pT[state["ptr"] % 2]; bpt = b_pT[state["ptr"] % 2]; state["ptr"] += 1
                    for j in range(8):
                        kc = k8 * 8 + j
                        P.op("pe", lambda e, pt=pt, j=j, kc=kc: e.transpose(out=pt[:, j, :], in_=xh[:, kc * 128:(kc + 1) * 128], identity=ident[:]),
                             reads=[b_xh, cB], writes=[bpt])
                    P.op("dve", lambda e, pt=pt, k8=k8, tt=tt: e.tensor_tensor(
                        out=xnT[:, k8 * 8:(k8 + 1) * 8, tt * 128:(tt + 1) * 128], in0=pt[:],
                        in1=gcol[:, k8 * 8:(k8 + 1) * 8].unsqueeze(2).to_broadcast([128, 8, 128]), op=ALU.mult),
                        reads=[bpt, cB], writes=[b_xnT[k8]])

        def load_w(col0, ncols, kq):
            i = state["wi"] % NW; state["wi"] += 1
            P.dma("pool", wt[i][:, :, 0:ncols], w[:, kq * 8:(kq + 1) * 8, col0:col0 + ncols], writes=[b_wt[i]])
            return wt[i], b_wt[i]

        def projections(tb):
            t0 = tb * TB
            for cg in range(3):
                for kq in range(4):
                    ws, bws = load_w(cg * 512, 512, kq)
                    order = range(4)
                    for mt in order:
                        for kci in range(8):
                            kc = kq * 8 + kci
                            P.op("pe", lambda e, mt=mt, kci=kci, kc=kc, ws=ws: e.matmul(
                                pA[mt][:], lhsT=ws[:, kci, mt * 128:(mt + 1) * 128], rhs=xnT[:, kc, :], start=(kc == 0), stop=(kc == 31)),
                                reads=[bws, b_xnT[kc // 8]], writes=[b_pA[mt]])
                for mt in range(4):
                    M = cg * 4 + mt
                    if M < 8:
                        dst, bd, scale = qT[:, M, :], b_qT, QSCALE
                    elif M == 8:
                        dst, bd, scale = rawT[:, 0, 16:528], b_raw, 1.0
                    elif M == 9:
                        dst, bd, scale = kT[:, 0, t0:t0 + TB], b_kT, 1.0
                    elif M == 10:
                        dst, bd, scale = kT[:, 1, t0:t0 + TB], b_kT, 1.0
                    else:
                        dst, bd, scale = rawT[:, 1, 16:528], b_raw, 1.0
                    if evac_engine() == "act":
                        P.op("act", lambda e, dst=dst, mt=mt, scale=scale: e.activation(out=dst, in_=pA[mt][:], func=AF.Copy, scale=scale),
                             reads=[b_pA[mt]], writes=[bd])
                    else:
                        P.op("dve", lambda e, dst=dst, mt=mt, scale=scale: e.tensor_scalar(out=dst, in0=pA[mt][:], scalar1=scale, scalar2=None, op0=ALU.mult),
                             reads=[b_pA[mt]], writes=[bd])
            for kq in range(4):
                ws, bws = load_w(NQK, NVG, kq)
                for tt in range(4):
                    for kci in range(8):
                        kc = kq * 8 + kci
                        P.op("pe", lambda e, tt=tt, kci=kci, kc=kc, ws=ws: e.matmul(
                            pA[tt][:, 0:NVG], lhsT=xnT[:, kc, tt * 128:(tt + 1) * 128], rhs=ws[:, kci, 0:NVG], start=(kc == 0), stop=(kc == 31)),
                            reads=[bws, b_xnT[kc // 8]], writes=[b_pA[tt]])
            for tt in range(4):
                kt = 4 * tb + tt
                P.op("dve", lambda e, tt=tt, kt=kt: e.tensor_copy(out=Vaug[:, :, kt, 0:128], in_=pA[tt][:, 0:256].rearrange("p (b d) -> p b d", b=2)),
                     reads=[b_pA[tt]], writes=[b_V])
                P.op("act", lambda e, tt=tt: e.activation(out=gates[:, tt, :], in_=pA[tt][:, 256:280], func=AF.Sigmoid),
                     reads=[b_pA[tt]], writes=[b_gates])
            for cg in range(6):
                for kq in range(4):
                    ws, bws = load_w(NQK + NVG + cg * 512, 512, kq)
                    for tt in range(4):
                        for kci in range(8):
                            kc = kq * 8 + kci
                            P.op("pe", lambda e, tt=tt, kci=kci, kc=kc, ws=ws: e.matmul(
                                pA[tt][:], lhsT=xnT[:, kc, tt * 128:(tt + 1) * 128], rhs=ws[:, kci, :], start=(kc == 0), stop=(kc == 31)),
                                reads=[bws, b_xnT[kc // 8]], writes=[b_pA[tt]])
                for tt in range(4):
                    P.op("act", lambda e, tt=tt, cg=cg: e.activation(out=zs[:, tt, cg * 512:(cg + 1) * 512], in_=pA[tt][:], func=AF.Silu),
                         reads=[b_pA[tt]], writes=[b_zs])

        def compress(tb):
            t0 = tb * TB
            n_lo = max(0, 32 * tb - 1)
            n_hi = 32 * tb + 30
            cnt = n_hi - n_lo + 1
            col0 = 16 * n_lo - (t0 - 16)
            i = state["wi"] % NW; state["wi"] += 1
            w1 = wt[i]; bw1 = b_wt[i]
            w1v_ = w1[:].rearrange("p a c -> p (a c)").rearrange("p (l e) -> p l e", e=128)
            i2 = state["wi"] % NW; state["wi"] += 1
            w1b = wt[i2]; bw1b = b_wt[i2]
            w1bv_ = w1b[:].rearrange("p a c -> p (a c)").rearrange("p (l e) -> p l e", e=128)
            P.dma("pool", w1v_, w1k_d, writes=[bw1])
            P.dma("pool", w1bv_, w1v_d, writes=[bw1b])
            if tb == 0:
                for kv, (wv, bw, pos) in enumerate([(w1v_, bw1, posk), (w1bv_, bw1b, posv)]):
                    for l in range(32):
                        P.op("pe", lambda e, wv=wv, l=l, pos=pos, kv=kv: e.matmul(pA[kv][:, 0:1], lhsT=wv[:, l, :], rhs=pos[:, l:l + 1], start=(l == 0), stop=(l == 31)),
                             reads=[bw, cB], writes=[b_pA[kv]])
                    P.op("dve", lambda e, kv=kv: e.tensor_copy(out=cbias[:, kv:kv + 1], in_=pA[kv][:, 0:1]), reads=[b_pA[kv]], writes=[b_cbias])
            for kv, (wv, bw) in enumerate([(w1v_, bw1), (w1bv_, bw1b)]):
                for l in range(32):
                    P.op("pe", lambda e, wv=wv, l=l, kv=kv: e.matmul(pA[2 + kv][:, 0:cnt], lhsT=wv[:, l, :],
                                                                   rhs=rawT[:, kv, col0 + l: col0 + l + 16 * (cnt - 1) + 1: 16], start=(l == 0), stop=(l == 31)),
                         reads=[bw, b_raw], writes=[b_pA[2 + kv]])
                P.op("act", lambda e, kv=kv: e.activation(out=hid[:, kv, 0:cnt], in_=pA[2 + kv][:, 0:cnt], func=AF.Silu, bias=cbias[:, kv:kv + 1], scale=1.0),
                     reads=[b_pA[2 + kv], b_cbias], writes=[b_hid])
            P.op("pe", lambda e: e.matmul(pA[0][:, 0:cnt], lhsT=w2k[:], rhs=hid[:, 0, 0:cnt], start=True, stop=True),
                 reads=[b_hid, cB], writes=[b_pA[0]])
            P.op("dve", lambda e: e.tensor_copy(out=kcT[:, n_lo:n_lo + cnt], in_=pA[0][:, 0:cnt]), reads=[b_pA[0]], writes=[b_kc])
            P.op("pe", lambda e: e.matmul(pA[1][0:cnt, 0:128], lhsT=hid[:, 1, 0:cnt], rhs=w2v[:], start=True, stop=True),
                 reads=[b_hid, cB], writes=[b_pA[1]])
            P.op("dve", lambda e: e.tensor_copy(out=vctmp[0:cnt, :], in_=pA[1][0:cnt, 0:128]), reads=[b_pA[1]], writes=[b_vctmp])
            a = n_lo
            while a <= n_hi:
                ctt = a // 128
                bnd = min(n_hi, 128 * ctt + 127)
                P.dma("sp", vcaug[a - 128 * ctt: bnd - 128 * ctt + 1, ctt, 0:128], vctmp[a - n_lo: bnd - n_lo + 1, :], reads=[b_vctmp], writes=[b_vc])
                a = bnd + 1
            P.op("dve", lambda e: e.tensor_copy(out=rawT[:, :, 0:16], in_=rawT[:, :, 512:528]), reads=[b_raw], writes=[b_raw])

        def next_pa():
            i = state["pai"] % 4; state["pai"] += 1
            return i

        def next_pt():
            i = state["pti"] % NPT; state["pti"] += 1
            return i

        def combine(tb, hl, br, first):
            for bk in range(2):
                P.op("dve", lambda e, bk=bk: e.tensor_scalar(out=rsb[:, 2 * bk:2 * bk + 2], in0=pO[bk][:, :, 128], scalar1=1e-30, scalar2=None, op0=ALU.max),
                     reads=[b_pO[bk]], writes=[b_rsb])
            if br == 2 and tb == 0:
                P.op("dve", lambda e: e.tensor_tensor(out=rsb[:, 0:4], in0=rsb[:, 0:4], in1=winx[:, :, hl], op=ALU.add), reads=[b_rsb, cB], writes=[b_rsb])
            P.op("dve", lambda e: e.reciprocal(out=rsb[:, 4:8], in_=rsb[:, 0:4]), reads=[b_rsb], writes=[b_rsb])
            return

        def combine2(tb, hl, br):
            gi = br * 8 + hl
            P.op("dve", lambda e: e.tensor_tensor(out=rsb[:, 0:4], in0=rsb[:, 4:8], in1=gates[:, :, gi], op=ALU.mult),
                 reads=[b_rsb, b_gates], writes=[b_rsb])
            for i in range(4):
                bk, j = i // 2, i % 2
                zsl = zs[:, i, gi * 128:(gi + 1) * 128]
                if br == 0:
                    P.op("dve", lambda e, i=i, bk=bk, j=j, zsl=zsl: e.scalar_tensor_tensor(
                        out=mix[:, i, hl * 128:(hl + 1) * 128], in0=pO[bk][:, j, 0:128], scalar=rsb[:, i:i + 1], in1=zsl, op0=ALU.mult, op1=ALU.mult),
                        reads=[b_pO[bk], b_rsb, b_zs], writes=[b_mix])
                else:
                    P.op("dve", lambda e, i=i, bk=bk, j=j, zsl=zsl: e.scalar_tensor_tensor(
                        out=otmp[:, i, :], in0=pO[bk][:, j, 0:128], scalar=rsb[:, i:i + 1], in1=zsl, op0=ALU.mult, op1=ALU.mult),
                        reads=[b_pO[bk], b_rsb, b_zs], writes=[b_otmp])
                    P.op("dve", lambda e, i=i: e.tensor_tensor(out=mix[:, i, hl * 128:(hl + 1) * 128], in0=mix[:, i, hl * 128:(hl + 1) * 128], in1=otmp[:, i, :], op=ALU.add),
                         reads=[b_otmp, b_mix], writes=[b_mix])

        def pv(pt_i, a, b_, vrhs, bv, first_flags, last_sub):
            for i in range(a // 128, b_ // 128):
                bk, j = i // 2, i % 2
                st_flag = first_flags[bk]
                first_flags[bk] = False
                P.op("pe", lambda e, i=i, bk=bk, j=j, st_flag=st_flag: e.matmul(
                    pO[bk][:, j, 0:129], lhsT=PT[pt_i][:, i * 128:(i + 1) * 128], rhs=vrhs, start=st_flag, stop=False, skip_group_check=True),
                    reads=[b_PT[pt_i], bv], writes=[b_pO[bk]])

        def score_tile(hl, klhs, bk_, a, b_, bias_ap, ex_lhs, ex_rhs_fn, masks, extra_reads):
            pa = next_pa()
            n_extra = 1 + len(masks)
            P.op("pe", lambda e: e.matmul(pA[pa][:, a:b_], lhsT=klhs, rhs=qT[:, hl, a:b_], start=True, stop=False),
                 reads=[bk_, b_qT], writes=[b_pA[pa]])
            P.op("pe", lambda e: e.matmul(pA[pa][:, a:b_], lhsT=ex_lhs, rhs=ex_rhs_fn(a, b_), start=False, stop=(len(masks) == 0)),
                 reads=[cB, b_RS], writes=[b_pA[pa]])
            for mi, (c0, c1, tab, btab) in enumerate(masks):
                P.op("pe", lambda e, c0=c0, c1=c1, tab=tab, mi=mi: e.matmul(pA[pa][:, c0:c1], lhsT=ident[:], rhs=tab, start=False, stop=(mi == len(masks) - 1)),
                     reads=[cB, btab], writes=[b_pA[pa]])
            pti = next_pt()
            P.op("act", lambda e: e.activation(out=PT[pti][:, a:b_], in_=pA[pa][:, a:b_], func=AF.Exp, bias=bias_ap, scale=1.0),
                 reads=[b_pA[pa], cB], writes=[b_PT[pti]])
            return pti

        def attn_cmp(tb):
            qt = tb
            P.dma("sp", cmpm[:], cmpm_d[:, qt, :, :], writes=[b_cmpm])
            cts = [0] if qt < 4 else [0, 1]
            for hl in range(8):
                ff = [True, True]
                ptis = []
                for ct in cts:
                    need_mask = not (ct == 0 and qt >= 5)
                    masks = [(0, 512, cmpm[:, ct, :], b_cmpm)] if need_mask else []
                    pti = score_tile(hl, kcT[:, ct * 128:(ct + 1) * 128], b_kc, 0, 512,
                                     kbc[:, hl, ct * 8 + qt: ct * 8 + qt + 1],
                                     EX[64:67, 0, :], lambda a, b_, hl=hl: RS[64:67, hl, a:b_], masks, [])
                    ptis.append((ct, pti))
                    pv(pti, 0, 512, vcaug[:, ct, :], b_vc, ff, None)
                pa = next_pa()
                for i in range(4):
                    for k, (ct, pti) in enumerate(ptis):
                        P.op("pe", lambda e, i=i, ct=ct, pti=pti, k=k: e.matmul(
                            pA[pa][:, i * 64:(i + 1) * 64], lhsT=PT[pti][:, i * 128:(i + 1) * 128], rhs=OV[:, ct, :], start=(k == 0), stop=(k == len(ptis) - 1)),
                            reads=[b_PT[pti], cB], writes=[b_pA[pa]])
                combine(tb, hl, 0, None)
                for i in range(4):
                    if hl == 0:
                        P.op("dve", lambda e, i=i: e.tensor_scalar(out=impacc[:, i, :], in0=pA[pa][:, i * 64:(i + 1) * 64], scalar1=rsb[:, 4 + i:5 + i], scalar2=None, op0=ALU.mult),
                             reads=[b_pA[pa], b_rsb], writes=[b_imp])
                    else:
                        P.op("dve", lambda e, i=i: e.scalar_tensor_tensor(out=impacc[:, i, :], in0=pA[pa][:, i * 64:(i + 1) * 64], scalar=rsb[:, 4 + i:5 + i], in1=impacc[:, i, :], op0=ALU.mult, op1=ALU.add),
                             reads=[b_pA[pa], b_rsb, b_imp], writes=[b_imp])
                combine2(tb, hl, 0)

        def topk(tb):
            t0 = tb * TB
            P.dma("sp", bimp[:], bimp_d[t0:t0 + TB, :].rearrange("(t p) j -> p t j", p=128), writes=[b_bimp])
            P.op("dve", lambda e: e.tensor_tensor(out=impacc[:], in0=impacc[:], in1=bimp[:], op=ALU.add), reads=[b_imp, b_bimp], writes=[b_imp])
            for i in range(4):
                P.op("dve", lambda e, i=i: e.max(out=top[:, i, 0:8], in_=impacc[:, i, :]), reads=[b_imp], writes=[b_top])
                P.op("dve", lambda e, i=i: e.match_replace(out=impw[:, i, :], in_to_replace=top[:, i, 0:8], in_values=impacc[:, i, :], imm_value=-3.0e38),
                     reads=[b_imp, b_top], writes=[b_impw])
                P.op("dve", lambda e, i=i: e.max(out=top[:, i, 8:16], in_=impw[:, i, :]), reads=[b_impw], writes=[b_top])
                P.op("dve", lambda e, i=i: e.tensor_scalar(out=selm[:, i, :], in0=impacc[:, i, :], scalar1=top[:, i, 15:16], scalar2=1.0, op0=ALU.is_ge, op1=ALU.subtract),
                     reads=[b_imp, b_top], writes=[b_selm])

        def sel_transpose(tb):
            pt = pT[state["ptr"] % 2]; bpt = b_pT[state["ptr"] % 2]; state["ptr"] += 1
            for i in range(4):
                P.op("pe", lambda e, i=i: e.transpose(out=pt[0:64, i, :], in_=selm[:, i, :], identity=ident[:]),
                     reads=[b_selm, cB], writes=[bpt])
            P.op("dve", lambda e: e.tensor_copy(out=RS[0:64, 0, :].rearrange("p (i c) -> p i c", i=4), in_=pt[0:64, 0:4, :]), reads=[bpt], writes=[b_RS])
            for h in range(1, 8):
                P.op("dve", lambda e, h=h: e.tensor_copy(out=RS[0:64, h, :], in_=RS[0:64, 0, :]), reads=[b_RS], writes=[b_RS])

        def attn_dense(tb, br):
            qt = tb
            bi = br - 1
            for hl in range(8):
                ff = [True, True]
                tiles = []
                if br == 2:
                    for u in range(4):
                        kt = 4 * qt - 4 + u
                        if kt >= 0:
                            tiles.append((kt, 0, 128 * (u + 1), [(128 * u, 128 * u + 128, triw[:], cB)]))
                else:
                    for kt in range(4 * qt):
                        tiles.append((kt, 0, 512, []))
                for u in range(4):
                    tiles.append((4 * qt + u, 128 * u, 512, [(128 * u, 128 * u + 128, tric[:], cB)]))
                for (kt, a, b_, masks) in tiles:
                    dl = kt - 4 * qt + 28
                    if br == 1:
                        ex_lhs = EX[0:67, kt, :]
                        ex_rhs = lambda a, b_, hl=hl: RS[0:67, hl, a:b_]
                    else:
                        ex_lhs = EX[64:67, 0, :]
                        ex_rhs = lambda a, b_, hl=hl: RS[64:67, hl, a:b_]
                    pti = score_tile(hl, kT[:, bi, kt * 128:(kt + 1) * 128], b_kT, a, b_, kb[:, hl, dl:dl + 1], ex_lhs, ex_rhs, masks, [])
                    pv(pti, a, b_, Vaug[:, bi, kt, :], b_V, ff, None)
                combine(tb, hl, br, None)
                combine2(tb, hl, br)

        def store_mix(tb):
            t0 = tb * TB
            for tt in range(4):
                P.dma("pool", mix_d[t0 + tt * 128:t0 + (tt + 1) * 128, :], mix[:, tt, :], reads=[b_mix])

        for tb in range(NTB):
            front_end(tb)
            if stage >= 1: projections(tb)
            if stage >= 2: compress(tb)
            if stage >= 3: attn_cmp(tb)
            if stage >= 4: topk(tb)
            if stage >= 5: attn_dense(tb, 2)
            if stage >= 6: sel_transpose(tb)
            if stage >= 7: attn_dense(tb, 1)
            if stage >= 3: store_mix(tb)
        if dbg:
            def dump(name, t, bufs, q="sp"):
                shp = list(t.shape)
                d_ = dram("dbg_" + name, shp, t.dtype, kind="ExternalOutput")
                P.dma(q, d_, t[:], reads=bufs)
            dump("xnT", xnT, b_xnT); dump("qT", qT, [b_qT]); dump("kT", kT, [b_kT]); dump("raw", rawT, [b_raw])
            dump("zs", zs, [b_zs]); dump("gates", gates, [b_gates]); dump("V", Vaug, [b_V])
            dump("kc", kcT, [b_kc]); dump("vc", vcaug, [b_vc]); dump("mixf", mix, [b_mix])
            dump("imp", impacc, [b_imp]); dump("selm", selm, [b_selm]); dump("RS", RS, [b_RS]); dump("top", top, [b_top])
        if ctx is None:
            P.finish()
        else:
            P.barrier()
        print("ninst", P.ninst, "nwaits", P.nwaits, flush=True)
    return nc


def nsa_inputs(z, b, g, tabs=None):
    cols = nsa_weight_cols(g)
    wl = w_layout(np.ascontiguousarray(z["a_w_in"][0][:, cols]))
    m = {"x": np.ascontiguousarray(z["x"][b]), "gcol": np.ascontiguousarray(z["a_norm_g"][0].reshape(32, 128).T), "w": wl}
    m["w1k"] = np.ascontiguousarray(z["a_cmp_w1_k"][0].transpose(1, 0, 2)); m["w1v"] = np.ascontiguousarray(z["a_cmp_w1_v"][0].transpose(1, 0, 2))
    m["w2k"] = z["a_cmp_w2_k"][0]; m["w2v"] = z["a_cmp_w2_v"][0]
    m["posk"] = np.ascontiguousarray(z["a_cmp_pos_k"][0].T); m["posv"] = np.ascontiguousarray(z["a_cmp_pos_v"][0].T)
    m.update(nsa_tables(g))
    return m


MQK = 1280
MV = 256
MZ = 1024
MCOL = MQK + MV + MZ


def moba_tables(j):
    hl = np.arange(8)
    m = (2.0 ** (-8.0 * (8 * j + hl + 1) / 32)).astype(np.float32)
    p = np.arange(128)
    t = {}
    dl = np.arange(32) - 28
    t["kb"] = (m[None, :, None] * (128.0 * dl[None, None, :] + p[:, None, None])).astype(np.float32)
    c = np.arange(512, dtype=np.float32)
    v = -(m[:, None] * c[None, :]).astype(np.float32)
    t["rsinit"] = bf16_split3(v)
    ex = np.zeros((67, 32, 128), np.float32)
    for kt in range(32):
        ex[kt // 2, kt, :] = 30000.0
    ex[64:67] = 1.0
    t["ex"] = ex.astype(NPBF)
    r = p[:, None]; cc = np.arange(128)[None, :]
    t["tric"] = np.where(r > cc, NEGM, 0.0).astype(NPBF)
    t["idn"] = np.eye(128, dtype=np.float32).astype(NPBF)
    mt = np.zeros((8, 4, 16), np.float32)
    for tb in range(8):
        for tt in range(4):
            bt = (4 * tb + tt) // 2
            mt[tb, tt, bt:] = -1e30
    t["mtab"] = np.ascontiguousarray(np.broadcast_to(mt.reshape(1, 8, 64), (128, 8, 64))).astype(np.float32)
    return t


def moba_weight(z, j):
    H = 32
    d = np.arange(DH)
    wq = [z["b_w_in"][0][:, (8 * j + hl) * DH + d] for hl in range(8)]
    wk = [z["kv_w"][:, (0 * 8 + 2 * j + gi) * DH + d] for gi in range(2)]
    wv = [z["kv_w"][:, (1 * 8 + 2 * j + gi) * DH + d] for gi in range(2)]
    wz = [z["b_w_in"][0][:, H * DH + (8 * j + hl) * DH + d] for hl in range(8)]
    w = np.concatenate(wq + wk + wv + wz, axis=1)
    assert w.shape[1] == MCOL
    return w_layout(w)


def build_moba(NTB=8, dbg=False, ctx=None, io=None):
    nc = bass.Bass("TRN2", target_bir_lowering=False) if ctx is None else ctx["nc"]
    pfx = "" if ctx is None else ctx["pfx"]

    def dram(name, shape, dt, kind="ExternalInput"):
        if io is not None and name in io:
            return io[name]
        return nc.dram_tensor(pfx + name, shape, dt, kind=kind).ap()
    x = dram("x", [S, D], F32)
    gcol_d = dram("gcol", [128, 32], F32)
    gkv_d = dram("gkv", [128, 32], F32)
    w = dram("w", [128, 32, MCOL], F32)
    kb_d = dram("kb", [128, 8, 32], F32)
    rsinit_d = dram("rsinit", [3, 8, 512], BF16)
    ex_d = dram("ex", [67, 32, 128], BF16)
    tric_d = dram("tric", [128, 128], BF16)
    idn_d = dram("idn", [128, 128], BF16)
    mtab_d = dram("mtab", [128, 8, 64], F32)
    mix_d = dram("mix", [S, 1024], BF16, kind="ExternalOutput")

    with ExitStack() as st:
        P = Prog(nc, st) if ctx is None else ctx["P"]
        sb = lambda name, shape, dt: st.enter_context(nc.sbuf_tensor(pfx + "s_" + name, shape, dt))
        ps = lambda name, shape, dt: st.enter_context(nc.psum_tensor(pfx + "p_" + name, shape, dt))
        cB = Buf("const")
        ident = sb("ident", [128, 128], BF16)
        gcol = sb("gcol", [128, 32], F32); gkv = sb("gkv", [128, 32], F32); rcol = sb("rcol", [128, 32], F32)
        kb = sb("kb", [128, 8, 32], F32)
        EX = sb("EX", [67, 32, 128], BF16)
        tric = sb("tric", [128, 128], BF16)
        RS = sb("RS", [67, 8, 512], BF16); b_RS = Buf("RS")
        P.op("dve", lambda e: e.memset(RS[0:64, :, :], 0.0), writes=[b_RS])
        for (dst, src) in [(ident, idn_d), (gcol, gcol_d), (gkv, gkv_d), (kb, kb_d), (EX, ex_d), (tric, tric_d)]:
            bb = Buf()
            P.dma("sp", dst[:], src, writes=[bb])
            for sk, v in bb.w.items():
                cB.w[sk] = max(cB.w.get(sk, 0), v)
        P.dma("sp", RS[64:67, :, :], rsinit_d, writes=[b_RS])
        epst = sb("epst", [128, 1], F32)
        P.op("dve", lambda e: e.memset(epst[:], EPS), writes=[cB])
        P.op("dve", lambda e: e.reciprocal(out=rcol[:], in_=gcol[:]), reads=[cB], writes=[cB])
        P.op("dve", lambda e: e.tensor_tensor(out=rcol[:], in0=rcol[:], in1=gkv[:], op=ALU.mult), reads=[cB], writes=[cB])
        kT = sb("kT", [128, 2, S], BF16); b_kT = Buf("kT")
        Vaug = sb("Vaug", [128, 2, 32, 129], BF16); b_V = Buf("V")
        kmean = sb("kmean", [128, 2, 16], BF16); b_km = Buf("km")
        kms = sb("kms", [128, 2, 2], F32); b_kms = Buf("kms")
        P.op("dve", lambda e: e.memset(Vaug[:], 1.0), writes=[b_V])
        P.op("dve", lambda e: e.memset(kmean[:], 0.0), writes=[b_km])
        xf = sb("xf", [128, D], F32); b_xf = Buf("xf")
        xh = sb("xh", [128, D], BF16); b_xh = Buf("xh")
        ss = sb("ss", [128, 4], F32); b_ss = Buf("ss")
        xnT = sb("xnT", [128, 32, TB], BF16); b_xnT = [Buf("xnT%d" % i) for i in range(4)]
        NW = 4
        wt = [sb("wt%d" % i, [128, 8, 512], BF16) for i in range(NW)]; b_wt = [Buf("wt%d" % i) for i in range(NW)]
        qT = sb("qT", [128, 8, TB], BF16); b_qT = Buf("qT")
        zs = sb("zs", [128, 4, MZ], BF16); b_zs = Buf("zs")
        mix = sb("mix", [128, 4, 1024], F32); b_mix = Buf("mix")
        NPT = 4
        PT = [sb("PT%d" % i, [128, TB], BF16) for i in range(NPT)]; b_PT = [Buf("PT%d" % i) for i in range(NPT)]
        mtab = sb("mtab", [128, 64], F32); b_mtab = Buf("mtab")
        sblk = sb("sblk", [128, 8, 64], F32); b_sblk = Buf("sblk")
        top = sb("top", [128, 32, 8], F32); b_top = Buf("top")
        selm = sb("selm", [128, 8, 4, 16], BF16); b_selm = Buf("selm")
        rsb = sb("rsb", [128, 8], F32); b_rsb = Buf("rsb")
        pA = [ps("pA%d" % i, [128, 512], F32) for i in range(4)]; b_pA = [Buf("pA%d" % i, True) for i in range(4)]
        pO = [ps("pO%d" % i, [128, 2, 256], F32) for i in range(2)]; b_pO = [Buf("pO%d" % i, True) for i in range(2)]
        pT = [ps("pT%d" % i, [128, 8, 128], BF16) for i in range(2)]; b_pT = [Buf("pT%d" % i, True) for i in range(2)]
        print("sbuf remaining", nc.sbuf_bytes_remaining, flush=True)
        state = {"wi": 0, "pti": 0, "pai": 0, "ptr": 0, "alt": 0}

        def evac_engine():
            state["alt"] ^= 1
            return "dve" if state["alt"] else "act"

        def front_end(tb):
            t0 = tb * TB
            for tt in range(4):
                P.dma("sp", xf[:], x[t0 + tt * 128: t0 + (tt + 1) * 128, :], writes=[b_xf])
                P.op("dve", lambda e: e.memset(ss[:, 0:1], 0.0), writes=[b_ss])
                P.op("act", lambda e: e.activation(out=xh[:], in_=xf[:], func=AF.Square, accum_out=ss[:, 0:1]), reads=[b_xf], writes=[b_xh, b_ss])
                P.op("act", lambda e: e.activation(out=ss[:, 1:2], in_=ss[:, 0:1], func=AF.Ln, bias=epst[:], scale=1.0 / D), reads=[b_ss, cB], writes=[b_ss])
                P.op("act", lambda e: e.activation(out=ss[:, 2:3], in_=ss[:, 1:2], func=AF.Exp, scale=-0.5), reads=[b_ss], writes=[b_ss])
                P.op("dve", lambda e: e.tensor_scalar(out=xh[:], in0=xf[:], scalar1=ss[:, 2:3], scalar2=None, op0=ALU.mult), reads=[b_xf, b_ss], writes=[b_xh])
                for k8 in range(4):
                    pt = pT[state["ptr"] % 2]; bpt = b_pT[state["ptr"] % 2]; state["ptr"] += 1
                    for j in range(8):
                        kc = k8 * 8 + j
                        P.op("pe", lambda e, pt=pt, j=j, kc=kc: e.transpose(out=pt[:, j, :], in_=xh[:, kc * 128:(kc + 1) * 128], identity=ident[:]),
                             reads=[b_xh, cB], writes=[bpt])
                    P.op("dve", lambda e, pt=pt, k8=k8, tt=tt: e.tensor_tensor(
                        out=xnT[:, k8 * 8:(k8 + 1) * 8, tt * 128:(tt + 1) * 128], in0=pt[:],
                        in1=gcol[:, k8 * 8:(k8 + 1) * 8].unsqueeze(2).to_broadcast([128, 8, 128]), op=ALU.mult),
                        reads=[bpt, cB], writes=[b_xnT[k8]])

        def load_w(col0, ncols, kq, rescale=False):
            i = state["wi"] % NW; state["wi"] += 1
            P.dma("pool", wt[i][:, :, 0:ncols], w[:, kq * 8:(kq + 1) * 8, col0:col0 + ncols], writes=[b_wt[i]])
            if rescale:
                P.op("dve", lambda e: e.tensor_tensor(out=wt[i][:, :, 0:ncols], in0=wt[i][:, :, 0:ncols],
                                                      in1=rcol[:, kq * 8:(kq + 1) * 8].unsqueeze(2).to_broadcast([128, 8, ncols]), op=ALU.mult),
                     reads=[b_wt[i], cB], writes=[b_wt[i]])
            return wt[i], b_wt[i]

        def projections(tb):
            t0 = tb * TB
            for cg in range(3):
                nmt = 4 if cg < 2 else 2
                for kq in range(4):
                    ws, bws = load_w(cg * 512, nmt * 128, kq, rescale=(cg == 2))
                    for mt in range(nmt):
                        for kci in range(8):
                            kc = kq * 8 + kci
                            P.op("pe", lambda e, mt=mt, kci=kci, kc=kc, ws=ws: e.matmul(
                                pA[mt][:], lhsT=ws[:, kci, mt * 128:(mt + 1) * 128], rhs=xnT[:, kc, :], start=(kc == 0), stop=(kc == 31)),
                                reads=[bws, b_xnT[kc // 8]], writes=[b_pA[mt]])
                for mt in range(nmt):
                    M = cg * 4 + mt
                    if M < 8:
                        dst, bd, scale = qT[:, M, :], b_qT, QSCALE
                    else:
                        gi = M - 8
                        dst, bd, scale = kT[:, gi, t0:t0 + TB], b_kT, 1.0
                        P.op("dve", lambda e, mt=mt, gi=gi: e.tensor_reduce(out=kms[:, gi, :], in_=pA[mt][:].rearrange("p (b t) -> p b t", b=2), axis=AX.X, op=ALU.add),
                             reads=[b_pA[mt]], writes=[b_kms])
                        P.op("dve", lambda e, gi=gi: e.tensor_scalar(out=kmean[:, gi, 2 * tb:2 * tb + 2], in0=kms[:, gi, :], scalar1=1.0 / 256, scalar2=None, op0=ALU.mult),
                             reads=[b_kms], writes=[b_km])
                    if evac_engine() == "act":
                        P.op("act", lambda e, dst=dst, mt=mt, scale=scale: e.activation(out=dst, in_=pA[mt][:], func=AF.Copy, scale=scale),
                             reads=[b_pA[mt]], writes=[bd])
                    else:
                        P.op("dve", lambda e, dst=dst, mt=mt, scale=scale: e.tensor_scalar(out=dst, in0=pA[mt][:], scalar1=scale, scalar2=None, op0=ALU.mult),
                             reads=[b_pA[mt]], writes=[bd])
            for kq in range(4):
                ws, bws = load_w(MQK, MV, kq, rescale=True)
                for tt in range(4):
                    for kci in range(8):
                        kc = kq * 8 + kci
                        P.op("pe", lambda e, tt=tt, kci=kci, kc=kc, ws=ws: e.matmul(
                            pA[tt][:, 0:MV], lhsT=xnT[:, kc, tt * 128:(tt + 1) * 128], rhs=ws[:, kci, 0:MV], start=(kc == 0), stop=(kc == 31)),
                            reads=[bws, b_xnT[kc // 8]], writes=[b_pA[tt]])
            for tt in range(4):
                kt = 4 * tb + tt
                P.op("act", lambda e, tt=tt, kt=kt: e.activation(out=Vaug[:, :, kt, 0:128], in_=pA[tt][:, 0:256].rearrange("p (b d) -> p b d", b=2), func=AF.Copy),
                     reads=[b_pA[tt]], writes=[b_V])
            for cg in range(2):
                for kq in range(4):
                    ws, bws = load_w(MQK + MV + cg * 512, 512, kq)
                    for tt in range(4):
                        for kci in range(8):
                            kc = kq * 8 + kci
                            P.op("pe", lambda e, tt=tt, kci=kci, kc=kc, ws=ws: e.matmul(
                                pA[tt][:], lhsT=xnT[:, kc, tt * 128:(tt + 1) * 128], rhs=ws[:, kci, :], start=(kc == 0), stop=(kc == 31)),
                                reads=[bws, b_xnT[kc // 8]], writes=[b_pA[tt]])
                for tt in range(4):
                    P.op("act", lambda e, tt=tt, cg=cg: e.activation(out=zs[:, tt, cg * 512:(cg + 1) * 512], in_=pA[tt][:], func=AF.Silu),
                         reads=[b_pA[tt]], writes=[b_zs])

        def gating(tb):
            P.dma("sp", mtab[:], mtab_d[:, tb, :], writes=[b_mtab])
            pa = 0
            for hl in range(8):
                gi = hl // 4
                for tt in range(4):
                    c0 = (hl * 4 + tt) * 16
                    P.op("pe", lambda e, hl=hl, tt=tt, gi=gi, c0=c0: e.matmul(pA[pa][:, c0:c0 + 16], lhsT=qT[:, hl, tt * 128:(tt + 1) * 128], rhs=kmean[:, gi, :], start=True, stop=True),
                         reads=[b_qT, b_km], writes=[b_pA[pa]])
            P.op("dve", lambda e: e.tensor_tensor(out=sblk[:], in0=pA[pa][:].rearrange("p (h c) -> p h c", h=8), in1=mtab[:].unsqueeze(1).to_broadcast([128, 8, 64]), op=ALU.add),
                 reads=[b_pA[pa], b_mtab], writes=[b_sblk])
            for hl in range(8):
                for tt in range(4):
                    idx = hl * 4 + tt
                    P.op("dve", lambda e, hl=hl, tt=tt, idx=idx: e.max(out=top[:, idx, :], in_=sblk[:, hl, tt * 16:(tt + 1) * 16]), reads=[b_sblk], writes=[b_top])
            for hl in range(8):
                for tt in range(4):
                    idx = hl * 4 + tt
                    P.op("dve", lambda e, hl=hl, tt=tt, idx=idx: e.tensor_scalar(out=selm[:, hl, tt, :], in0=sblk[:, hl, tt * 16:(tt + 1) * 16], scalar1=top[:, idx, 2:3], scalar2=1.0, op0=ALU.is_ge, op1=ALU.subtract),
                         reads=[b_sblk, b_top], writes=[b_selm])
            for tt in range(4):
                bt = (4 * tb + tt) // 2
                P.op("dve", lambda e, tt=tt, bt=bt: e.memset(selm[:, :, tt, bt:bt + 1], 0.0), writes=[b_selm])
            for h2 in range(4):
                pt = pT[state["ptr"] % 2]; bpt = b_pT[state["ptr"] % 2]; state["ptr"] += 1
                for k in range(8):
                    hl = h2 * 2 + k // 4; tt = k % 4
                    P.op("pe", lambda e, k=k, hl=hl, tt=tt: e.transpose(out=pt[0:16, k, :], in_=selm[:, hl, tt, :], identity=ident[:]),
                         reads=[b_selm, cB], writes=[bpt])
                P.op("dve", lambda e, h2=h2: e.tensor_copy(out=RS[0:16, 2 * h2:2 * h2 + 2, :].rearrange("p h (t c) -> p (h t) c", t=4), in_=pt[0:16, :, :]),
                     reads=[bpt], writes=[b_RS])

        def next_pa():
            i = state["pai"] % 4; state["pai"] += 1
            return i

        def next_pt():
            i = state["pti"] % NPT; state["pti"] += 1
            return i

        def attn(tb):
            qt = tb
            for hl in range(8):
                gi = hl // 4
                ff = [True, True]
                tiles = [(kt, 0, 512, False) for kt in range(4 * qt)] + [(4 * qt + u, 128 * u, 512, True) for u in range(4)]
                for (kt, a, b_, diag) in tiles:
                    dl = kt - 4 * qt + 28
                    pa = next_pa()
                    P.op("pe", lambda e: e.matmul(pA[pa][:, a:b_], lhsT=kT[:, gi, kt * 128:(kt + 1) * 128], rhs=qT[:, hl, a:b_], start=True, stop=False),
                         reads=[b_kT, b_qT], writes=[b_pA[pa]])
                    P.op("pe", lambda e: e.matmul(pA[pa][:, a:b_], lhsT=EX[0:67, kt, :], rhs=RS[0:67, hl, a:b_], start=False, stop=(not diag)),
                         reads=[cB, b_RS], writes=[b_pA[pa]])
                    if diag:
                        P.op("pe", lambda e: e.matmul(pA[pa][:, a:a + 128], lhsT=ident[:], rhs=tric[:], start=False, stop=True),
                             reads=[cB], writes=[b_pA[pa]])
                    pti = next_pt()
                    P.op("act", lambda e: e.activation(out=PT[pti][:, a:b_], in_=pA[pa][:, a:b_], func=AF.Exp, bias=kb[:, hl, dl:dl + 1], scale=1.0),
                         reads=[b_pA[pa], cB], writes=[b_PT[pti]])
                    for i in range(a // 128, b_ // 128):
                        bk, jj = i // 2, i % 2
                        stf = ff[bk]; ff[bk] = False
                        P.op("pe", lambda e, i=i, bk=bk, jj=jj, stf=stf: e.matmul(
                            pO[bk][:, jj, 0:129], lhsT=PT[pti][:, i * 128:(i + 1) * 128], rhs=Vaug[:, gi, kt, :], start=stf, stop=False, skip_group_check=True),
                            reads=[b_PT[pti], b_V], writes=[b_pO[bk]])
                for bk in range(2):
                    P.op("dve", lambda e, bk=bk: e.tensor_scalar(out=rsb[:, 2 * bk:2 * bk + 2], in0=pO[bk][:, :, 128], scalar1=1e-30, scalar2=None, op0=ALU.max),
                         reads=[b_pO[bk]], writes=[b_rsb])
                P.op("dve", lambda e: e.reciprocal(out=rsb[:, 4:8], in_=rsb[:, 0:4]), reads=[b_rsb], writes=[b_rsb])
                for i in range(4):
                    bk, jj = i // 2, i % 2
                    P.op("dve", lambda e, i=i, bk=bk, jj=jj: e.scalar_tensor_tensor(
                        out=mix[:, i, hl * 128:(hl + 1) * 128], in0=pO[bk][:, jj, 0:128], scalar=rsb[:, 4 + i:5 + i], in1=zs[:, i, hl * 128:(hl + 1) * 128], op0=ALU.mult, op1=ALU.mult),
                        reads=[b_pO[bk], b_rsb, b_zs], writes=[b_mix])

        def store_mix(tb):
            t0 = tb * TB
            for tt in range(4):
                P.dma("pool", mix_d[t0 + tt * 128:t0 + (tt + 1) * 128, :], mix[:, tt, :], reads=[b_mix])

        for tb in range(NTB):
            front_end(tb)
            projections(tb)
            gating(tb)
            attn(tb)
            store_mix(tb)
        if dbg:
            def dump(name, t, bufs):
                d_ = dram("dbg_" + name, list(t.shape), t.dtype, kind="ExternalOutput")
                P.dma("sp", d_, t[:], reads=bufs)
            dump("qT", qT, [b_qT]); dump("kT", kT, [b_kT]); dump("V", Vaug, [b_V]); dump("zs", zs, [b_zs])
            dump("kmean", kmean, [b_km]); dump("sblk", sblk, [b_sblk]); dump("selm", selm, [b_selm]); dump("RS", RS, [b_RS]); dump("top", top, [b_top])
        if ctx is None:
            P.finish()
        else:
            P.barrier()
        print("ninst", P.ninst, "nwaits", P.nwaits, flush=True)
    return nc


def moba_inputs(z, h1b, j):
    m = {"x": (None if h1b is None else np.ascontiguousarray(h1b)), "gcol": np.ascontiguousarray(z["b_norm_g"][0].reshape(32, 128).T),
         "gkv": np.ascontiguousarray(z["kv_norm_g"].reshape(32, 128).T), "w": moba_weight(z, j)}
    m.update(moba_tables(j))
    return m


def build_outproj(final, NT=1024, ctx=None, io=None):
    nc = bass.Bass("TRN2", target_bir_lowering=False) if ctx is None else ctx["nc"]
    pfx = "" if ctx is None else ctx["pfx"]

    def dram(name, shape, dt, kind="ExternalInput"):
        if io is not None and name in io:
            return io[name]
        return nc.dram_tensor(pfx + name, shape, dt, kind=kind).ap()
    mixin = dram("mixin", [NT, D], BF16)
    res = dram("res", [NT, D], F32)
    w = dram("w", [128, 32, D], F32)
    idn = dram("idn", [128, 128], BF16)
    if final:
        gfin = dram("gfin", [128, D], F32)
    hout = dram("hout", [NT, D], F32, kind="ExternalOutput")
    with ExitStack() as st:
        P = Prog(nc, st) if ctx is None else ctx["P"]
        sb = lambda name, shape, dt: st.enter_context(nc.sbuf_tensor(pfx + "s_" + name, shape, dt))
        ps = lambda name, shape, dt: st.enter_context(nc.psum_tensor(pfx + "p_" + name, shape, dt))
        ident = sb("ident", [128, 128], BF16); cB = Buf()
        P.dma("sp", ident[:], idn, writes=[cB])
        if final:
            grep_ = sb("grep", [128, D], F32); b_grep = Buf()
            P.dma("sp", grep_[:], gfin, writes=[b_grep])
            ss = sb("ss", [128, 12], F32); b_ss = Buf()
            junk = sb("junk", [128, D], BF16); b_junk = Buf()
            epst = sb("epst", [128, 1], F32)
            P.op("dve", lambda e: e.memset(epst[:], EPS), writes=[cB])
        mixtok = [sb("mixtok%d" % i, [128, D], BF16) for i in range(2)]; b_mixtok = [Buf() for _ in range(2)]
        mixT = sb("mixT", [128, 32, 512], BF16); b_mixT = [Buf() for _ in range(4)]
        hb = [sb("hb%d" % i, [128, D], F32) for i in range(4)]; b_hb = [Buf() for _ in range(4)]
        NW = 3
        wt = [sb("wt%d" % i, [128, 16, 512], BF16) for i in range(NW)]; b_wt = [Buf() for _ in range(NW)]
        pacc = [ps("pacc%d" % i, [128, 512], F32) for i in range(4)]; b_pacc = [Buf("", True) for _ in range(4)]
        ptr = [ps("ptr%d" % i, [128, 8, 128], BF16) for i in range(2)]; b_ptr = [Buf("", True) for _ in range(2)]
        wi = 0
        tri = 0
        for half in range(NT // 512):
            t0 = half * 512
            for tt in range(4):
                P.dma("sp", hb[tt][:], res[t0 + tt * 128: t0 + (tt + 1) * 128, :], writes=[b_hb[tt]])
            for tt in range(4):
                mt = mixtok[tt % 2]; bmt = b_mixtok[tt % 2]
                P.dma("sp", mt[:], mixin[t0 + tt * 128: t0 + (tt + 1) * 128, :], writes=[bmt])
                for k8 in range(4):
                    pt = ptr[tri % 2]; bpt = b_ptr[tri % 2]; tri += 1
                    for j in range(8):
                        kc = k8 * 8 + j
                        P.op("pe", lambda e, pt=pt, j=j, kc=kc, mt=mt: e.transpose(out=pt[:, j, :], in_=mt[:, kc * 128:(kc + 1) * 128], identity=ident[:]),
                             reads=[bmt, cB], writes=[bpt])
                    if k8 % 2 == 0:
                        P.op("dve", lambda e, pt=pt, k8=k8, tt=tt: e.tensor_copy(out=mixT[:, k8 * 8:(k8 + 1) * 8, tt * 128:(tt + 1) * 128], in_=pt[:]),
                             reads=[bpt], writes=[b_mixT[k8]])
                    else:
                        P.op("act", lambda e, pt=pt, k8=k8, tt=tt: e.copy(out=mixT[:, k8 * 8:(k8 + 1) * 8, tt * 128:(tt + 1) * 128], in_=pt[:]),
                             reads=[bpt], writes=[b_mixT[k8]])
            for cg in range(8):
                for kh in range(2):
                    ws = wt[wi % NW]; bws = b_wt[wi % NW]; wi += 1
                    P.dma("pool", ws[:], w[:, kh * 16:(kh + 1) * 16, cg * 512:(cg + 1) * 512], writes=[bws])
                    for tt in range(4):
                        for kci in range(16):
                            kc = kh * 16 + kci
                            P.op("pe", lambda e, tt=tt, kci=kci, kc=kc, ws=ws: e.matmul(pacc[tt][:], lhsT=mixT[:, kc, tt * 128:(tt + 1) * 128], rhs=ws[:, kci, :], start=(kc == 0), stop=(kc == 31)),
                                 reads=[b_mixT[kc // 8], bws], writes=[b_pacc[tt]])
                for tt in range(4):
                    P.op("dve", lambda e, tt=tt, cg=cg: e.tensor_tensor(out=hb[tt][:, cg * 512:(cg + 1) * 512], in0=pacc[tt][:], in1=hb[tt][:, cg * 512:(cg + 1) * 512], op=ALU.add),
                         reads=[b_pacc[tt], b_hb[tt]], writes=[b_hb[tt]])
            for tt in range(4):
                if final:
                    P.op("dve", lambda e, tt=tt: e.memset(ss[:, tt:tt + 1], 0.0), writes=[b_ss])
                    P.op("act", lambda e, tt=tt: e.activation(out=junk[:], in_=hb[tt][:], func=AF.Square, accum_out=ss[:, tt:tt + 1]),
                         reads=[b_hb[tt]], writes=[b_junk, b_ss])
                    P.op("act", lambda e, tt=tt: e.activation(out=ss[:, 4 + tt:5 + tt], in_=ss[:, tt:tt + 1], func=AF.Ln, bias=epst[:], scale=1.0 / D),
                         reads=[b_ss, cB], writes=[b_ss])
                    P.op("act", lambda e, tt=tt: e.activation(out=ss[:, 8 + tt:9 + tt], in_=ss[:, 4 + tt:5 + tt], func=AF.Exp, scale=-0.5),
                         reads=[b_ss], writes=[b_ss])
                    P.op("dve", lambda e, tt=tt: e.scalar_tensor_tensor(out=hb[tt][:], in0=hb[tt][:], scalar=ss[:, 8 + tt:9 + tt], in1=grep_[:], op0=ALU.mult, op1=ALU.mult),
                         reads=[b_hb[tt], b_ss, b_grep], writes=[b_hb[tt]])
                P.dma("sp", hout[t0 + tt * 128: t0 + (tt + 1) * 128, :], hb[tt][:], reads=[b_hb[tt]])
        if ctx is None:
            P.finish()
        else:
            P.barrier()
    return nc


def build_fused():
    nc = bass.Bass("TRN2", target_bir_lowering=False)
    x = nc.dram_tensor("x", [S, D], F32, kind="ExternalInput").ap()
    out = nc.dram_tensor("out", [S, D], F32, kind="ExternalOutput").ap()
    mix0_s = nc.dram_tensor("mix0_s", [S, D], BF16, kind="Internal").ap()
    h1_s = nc.dram_tensor("h1_s", [S, D], F32, kind="Internal").ap()
    mix1_s = nc.dram_tensor("mix1_s", [S, D], BF16, kind="Internal").ap()
    with ExitStack() as st:
        P = Prog(nc, st)
        first = True
        for g in range(4):
            if not first:
                P.new_epoch()
            first = False
            build_nsa(8, ctx={"nc": nc, "P": P, "pfx": "a%d_" % g}, io={"x": x, "mix": mix0_s[:, g * 1024:(g + 1) * 1024]})
        P.new_epoch()
        build_outproj(False, NT=S, ctx={"nc": nc, "P": P, "pfx": "b_"}, io={"mixin": mix0_s, "res": x, "hout": h1_s})
        for j in range(4):
            P.new_epoch()
            build_moba(8, ctx={"nc": nc, "P": P, "pfx": "c%d_" % j}, io={"x": h1_s, "mix": mix1_s[:, j * 1024:(j + 1) * 1024]})
        P.new_epoch()
        build_outproj(True, NT=S, ctx={"nc": nc, "P": P, "pfx": "d_"}, io={"mixin": mix1_s, "res": h1_s, "hout": out})
        P.finish()
        print("fused ninst", P.ninst, "nwaits", P.nwaits, flush=True)
    return nc


def fused_inputs(z, b):
    m = {"x": np.ascontiguousarray(z["x"][b])}
    idn = np.eye(128, dtype=np.float32).astype(NPBF)
    for g in range(4):
        for k, v in nsa_inputs(z, b, g).items():
            if k != "x":
                m["a%d_%s" % (g, k)] = v
    m["b_w"] = w_layout(z["a_w_out"][0]); m["b_idn"] = idn
    for j in range(4):
        for k, v in moba_inputs(z, None, j).items():
            if k != "x":
                m["c%d_%s" % (j, k)] = v
    m["d_w"] = w_layout(z["b_w_out"][0]); m["d_idn"] = idn
    m["d_gfin"] = np.ascontiguousarray(np.broadcast_to(z["final_norm_g"].reshape(1, D), (128, D))).astype(np.float32)
    return m


def kernel(**z):
    z = {k: np.asarray(v) for k, v in z.items()}
    nc = build_fused()
    m0 = fused_inputs(z, 0)
    m1 = dict(m0); m1["x"] = np.ascontiguousarray(z["x"][1])
    res = run_bass_kernel_spmd(nc, [m0, m1], core_ids=[0, 1])
    out = np.stack([np.asarray(r["out"]) for r in res.results])
    return out.astype(np.float32)
```

```python
import os, sys, time
import numpy as np
import concourse.bass as bass
import concourse.mybir as mybir
from concourse.bass_utils import run_bass_kernel_spmd
from contextlib import ExitStack
import ml_dtypes

F32 = mybir.dt.float32
BF16 = mybir.dt.bfloat16
AF = mybir.ActivationFunctionType
ALU = mybir.AluOpType
AX = mybir.AxisListType
NPBF = ml_dtypes.bfloat16


class Buf:
    __slots__ = ("name", "w", "r", "excl")

    def __init__(self, name="", excl=False):
        self.name = name
        self.excl = excl
        self.w = {}
        self.r = {}


class Prog:
    NDMA = 16

    def __init__(self, nc, stack):
        self.nc = nc
        self.E = {"pe": nc.tensor, "act": nc.scalar, "dve": nc.vector, "pool": nc.gpsimd, "sp": nc.sync}
        self.sems = {}
        self.stack = stack
        self.epoch = 0
        self.cur = {}
        for k in self.E:
            self.cur[k] = k + "_0"
            self.sems[k + "_0"] = stack.enter_context(nc.semaphore("sem_" + k + "_0"))
        for i in range(self.NDMA):
            self.sems["d%d" % i] = stack.enter_context(nc.semaphore("sem_d%d" % i))
        self.cnt = {k: 0 for k in self.sems}
        self.waited = {k: {} for k in self.E}
        self.dma_rr = 0
        self.nwaits = 0
        self.ninst = 0

    def new_epoch(self):
        self.epoch += 1
        for k in self.E:
            sk = "%s_%d" % (k, self.epoch)
            self.cur[k] = sk
            self.sems[sk] = self.stack.enter_context(self.nc.semaphore("sem_" + sk))
            self.cnt[sk] = 0

    def _wait(self, eng, sk, v):
        w = self.waited[eng]
        if w.get(sk, 0) >= v:
            return
        if sk == self.cur[eng] and v <= self.cnt[sk] - 64:
            return
        w[sk] = v
        self.E[eng].wait_ge(self.sems[sk], v)
        self.nwaits += 1

    def _deps(self, eng, reads, writes):
        toks = {}
        for b in reads:
            for sk, v in b.w.items():
                if eng == "pe" and sk.startswith("pe_"):
                    continue
                if toks.get(sk, 0) < v:
                    toks[sk] = v
            if b.excl:
                for sk, v in b.r.items():
                    if sk.startswith(eng + "_"):
                        continue
                    if toks.get(sk, 0) < v:
                        toks[sk] = v
        for b in writes:
            for d in (b.w, b.r):
                for sk, v in d.items():
                    if sk.startswith(eng + "_"):
                        continue
                    if toks.get(sk, 0) < v:
                        toks[sk] = v
        for sk, v in toks.items():
            self._wait(eng, sk, v)

    def _commit(self, sk, v, reads, writes):
        for b in reads:
            if b.r.get(sk, 0) < v:
                b.r[sk] = v
        for b in writes:
            if b.w.get(sk, 0) < v:
                b.w[sk] = v

    def op(self, eng, fn, reads=(), writes=()):
        self._deps(eng, reads, writes)
        ins = fn(self.E[eng])
        sk = self.cur[eng]
        self.cnt[sk] += 1
        ins.then_inc(self.sems[sk], 1)
        self.ninst += 1
        self._commit(sk, self.cnt[sk], reads, writes)

    def dma(self, q, out, in_, reads=(), writes=(), **kw):
        sk = "d%d" % self.dma_rr
        self.dma_rr = (self.dma_rr + 1) % self.NDMA
        if self.cnt[sk] > 0:
            self._wait(q, sk, self.cnt[sk])
        self._deps(q, reads, writes)
        ins = self.E[q].dma_start(out=out, in_=in_, **kw)
        self.cnt[sk] += 16
        ins.then_inc(self.sems[sk], 16)
        self.ninst += 1
        self._commit(sk, self.cnt[sk], reads, writes)

    def barrier(self):
        toks = {sk: c for sk, c in self.cnt.items() if c > 0}
        for eng in self.E:
            for sk, v in toks.items():
                if sk == self.cur[eng]:
                    continue
                self._wait(eng, sk, v)

    def finish(self):
        for sk in self.sems:
            if sk.startswith("d") and self.cnt[sk] > 0:
                self._wait("sp", sk, self.cnt[sk])


def w_layout(w):
    return np.ascontiguousarray(w.reshape(32, 128, -1).transpose(1, 0, 2))


def bf16_split3(v):
    v = v.astype(np.float32)
    r0 = v.astype(NPBF)
    e1 = v - r0.astype(np.float32)
    r1 = e1.astype(NPBF)
    e2 = e1 - r1.astype(np.float32)
    r2 = e2.astype(NPBF)
    return np.stack([r0, r1, r2])


S = 4096
D = 4096
DH = 128
TB = 512
EPS = 1e-6
NEGM = -30000.0
QSCALE = DH ** -0.5
NQK = 1536
NVG = 280
NZ = 3072
NCOL = NQK + NVG + NZ


def nsa_tables(g):
    hl = np.arange(8)
    m = (2.0 ** (-8.0 * (8 * g + hl + 1) / 32)).astype(np.float32)
    p = np.arange(128)
    t = {}
    dl = np.arange(32) - 28
    kb = m[None, :, None] * (128.0 * dl[None, None, :] + p[:, None, None])
    t["kb"] = kb.astype(np.float32)
    ct = np.arange(2); qt = np.arange(8)
    kbc = m[None, :, None, None] * (16.0 * (128 * ct[None, None, :, None] + p[:, None, None, None]) + 31 - 512.0 * qt[None, None, None, :])
    t["kbc"] = kbc.astype(np.float32).reshape(128, 8, 16)
    c = np.arange(512, dtype=np.float32)
    v = -(m[:, None] * c[None, :]).astype(np.float32)
    t["rsinit"] = bf16_split3(v)
    ex = np.zeros((67, 32, 128), np.float32)
    for kt in range(32):
        for pp in range(128):
            ex[2 * kt + pp // 64, kt, pp] = 30000.0
    ex[64:67] = 1.0
    t["ex"] = ex.astype(NPBF)
    r = p[:, None]; cc = np.arange(128)[None, :]
    t["tric"] = np.where(r > cc, NEGM, 0.0).astype(NPBF)
    t["triw"] = np.where(r <= cc, NEGM, 0.0).astype(NPBF)
    cm = np.zeros((128, 8, 2, 512), np.float32)
    for q_ in range(8):
        for c_ in range(2):
            n = 128 * c_ + p
            tt = 512 * q_ + np.arange(512)
            valid = (16 * n[:, None] + 31 <= tt[None, :]) & (n[:, None] <= 254)
            cm[:, q_, c_, :] = np.where(valid, 0.0, NEGM)
    t["cmpm"] = cm.astype(NPBF)
    tq = np.arange(S); j = np.arange(64)
    bq = tq // 64
    forced = (j[None, :] == 0) | (j[None, :] == bq[:, None]) | (j[None, :] == bq[:, None] - 1)
    fut = j[None, :] > bq[:, None]
    t["bimp"] = np.where(forced, 1e9, np.where(fut, -1e30, 0.0)).astype(np.float32)
    n = np.arange(256)
    ov = ((16 * n[:, None] <= 64 * j[None, :] + 63) & (16 * n[:, None] + 31 >= 64 * j[None, :]) & (n[:, None] <= 254))
    t["ov"] = np.ascontiguousarray(ov.reshape(2, 128, 64).transpose(1, 0, 2)).astype(NPBF)
    t["idn"] = np.eye(128, dtype=np.float32).astype(NPBF)
    dd = np.arange(1, 512, dtype=np.float64)
    e = np.exp(-m.astype(np.float64)[:, None] * dd[None, :])
    csum = np.concatenate([np.cumsum(e[:, ::-1], axis=1)[:, ::-1], np.zeros((8, 1))], axis=1)
    tt = np.arange(512)
    wx = csum[:, tt]
    t["winx"] = np.ascontiguousarray(wx.T.reshape(4, 128, 8).transpose(1, 0, 2)).astype(np.float32)
    return t


def nsa_weight_cols(g):
    H, G = 32, 4
    q_end = H * DH
    kv_end = q_end + 3 * 2 * G * DH
    z_end = kv_end + 3 * H * DH
    cols = []
    d = np.arange(DH)
    for hl in range(8):
        cols.append((8 * g + hl) * DH + d)
    kvcol = lambda br, kvi: q_end + ((br * 2 + kvi) * G + g) * DH + d
    cols += [kvcol(0, 0), kvcol(1, 0), kvcol(2, 0), kvcol(0, 1)]
    cols += [kvcol(1, 1), kvcol(2, 1)]
    cols.append(np.array([z_end + br * H + 8 * g + hl for br in range(3) for hl in range(8)]))
    for br in range(3):
        for hl in range(8):
            cols.append(kv_end + (br * H + 8 * g + hl) * DH + d)
    cols = np.concatenate(cols)
    assert cols.shape[0] == NCOL
    return cols


def build_nsa(NTB=8, stage=9, dbg=False, ctx=None, io=None):
    nc = bass.Bass("TRN2", target_bir_lowering=False) if ctx is None else ctx["nc"]
    pfx = "" if ctx is None else ctx["pfx"]

    def dram(name, shape, dt, kind="ExternalInput"):
        if io is not None and name in io:
            return io[name]
        return nc.dram_tensor(pfx + name, shape, dt, kind=kind).ap()
    x = dram("x", [S, D], F32)
    gcol_d = dram("gcol", [128, 32], F32)
    w = dram("w", [128, 32, NCOL], F32)
    w1k_d = dram("w1k", [128, 32, 128], F32); w1v_d = dram("w1v", [128, 32, 128], F32)
    w2k_d = dram("w2k", [128, 128], F32); w2v_d = dram("w2v", [128, 128], F32)
    posk_d = dram("posk", [128, 32], F32); posv_d = dram("posv", [128, 32], F32)
    kb_d = dram("kb", [128, 8, 32], F32); kbc_d = dram("kbc", [128, 8, 16], F32)
    rsinit_d = dram("rsinit", [3, 8, 512], BF16)
    ex_d = dram("ex", [67, 32, 128], BF16)
    tric_d = dram("tric", [128, 128], BF16); triw_d = dram("triw", [128, 128], BF16)
    cmpm_d = dram("cmpm", [128, 8, 2, 512], BF16)
    bimp_d = dram("bimp", [S, 64], F32)
    ov_d = dram("ov", [128, 2, 64], BF16)
    idn_d = dram("idn", [128, 128], BF16)
    winx_d = dram("winx", [128, 4, 8], F32)
    mix_d = dram("mix", [S, 1024], BF16, kind="ExternalOutput")

    with ExitStack() as st:
        P = Prog(nc, st) if ctx is None else ctx["P"]
        sb = lambda name, shape, dt: st.enter_context(nc.sbuf_tensor(pfx + "s_" + name, shape, dt))
        ps = lambda name, shape, dt: st.enter_context(nc.psum_tensor(pfx + "p_" + name, shape, dt))
        cB = Buf("const")
        ident = sb("ident", [128, 128], BF16)
        gcol = sb("gcol", [128, 32], F32)
        kb = sb("kb", [128, 8, 32], F32); kbc = sb("kbc", [128, 8, 16], F32)
        EX = sb("EX", [67, 32, 128], BF16)
        tric = sb("tric", [128, 128], BF16); triw = sb("triw", [128, 128], BF16)
        OV = sb("OV", [128, 2, 64], BF16)
        w2k = sb("w2k", [128, 128], BF16); w2v = sb("w2v", [128, 128], BF16)
        posk = sb("posk", [128, 32], BF16); posv = sb("posv", [128, 32], BF16)
        RS = sb("RS", [67, 8, 512], BF16); b_RS = Buf("RS")
        winx = sb("winx", [128, 4, 8], F32)
        for (dst, src, q) in [(winx, winx_d, "sp"), (ident, idn_d, "sp"), (gcol, gcol_d, "sp"), (kb, kb_d, "sp"), (kbc, kbc_d, "sp"),
                              (EX, ex_d, "sp"), (tric, tric_d, "sp"), (triw, triw_d, "sp"), (OV, ov_d, "sp"),
                              (w2k, w2k_d, "pool"), (w2v, w2v_d, "pool"), (posk, posk_d, "pool"), (posv, posv_d, "pool")]:
            bb = Buf()
            P.dma(q, dst[:], src, writes=[bb])
            for sk, v in bb.w.items():
                cB.w[sk] = max(cB.w.get(sk, 0), v)
        P.dma("sp", RS[64:67, :, :], rsinit_d, writes=[b_RS])
        epst = sb("epst", [128, 1], F32)
        P.op("dve", lambda e: e.memset(epst[:], EPS), writes=[cB])
        kT = sb("kT", [128, 2, S], BF16); b_kT = Buf("kT")
        Vaug = sb("Vaug", [128, 2, 32, 129], BF16); b_V = Buf("V")
        rawT = sb("rawT", [128, 2, 528], BF16); b_raw = Buf("raw")
        kcT = sb("kcT", [128, 256], BF16); b_kc = Buf("kc")
        vcaug = sb("vcaug", [128, 2, 129], BF16); b_vc = Buf("vc")
        cbias = sb("cbias", [128, 2], F32); b_cbias = Buf("cbias")
        P.op("dve", lambda e: e.memset(Vaug[:], 1.0), writes=[b_V])
        P.op("dve", lambda e: e.memset(rawT[:], 0.0), writes=[b_raw])
        P.op("dve", lambda e: e.memset(kcT[:], 0.0), writes=[b_kc])
        P.op("dve", lambda e: e.memset(vcaug[:], 0.0), writes=[b_vc])
        P.op("dve", lambda e: e.memset(vcaug[:, :, 128:129], 1.0), writes=[b_vc])
        xf = sb("xf", [128, D], F32); b_xf = Buf("xf")
        xh = sb("xh", [128, D], BF16); b_xh = Buf("xh")
        junk = xh
        ss = sb("ss", [128, 4], F32); b_ss = Buf("ss")
        xnT = sb("xnT", [128, 32, TB], BF16); b_xnT = [Buf("xnT%d" % i) for i in range(4)]
        NW = 4
        wt = [sb("wt%d" % i, [128, 8, 512], BF16) for i in range(NW)]; b_wt = [Buf("wt%d" % i) for i in range(NW)]
        qT = sb("qT", [128, 8, TB], BF16); b_qT = Buf("qT")
        zs = sb("zs", [128, 4, NZ], BF16); b_zs = Buf("zs")
        gates = sb("gates", [128, 4, 24], F32); b_gates = Buf("gates")
        mix = sb("mix", [128, 4, 1024], F32); b_mix = Buf("mix")
        NPT = 4
        PT = [sb("PT%d" % i, [128, TB], BF16) for i in range(NPT)]; b_PT = [Buf("PT%d" % i) for i in range(NPT)]
        cmpm = sb("cmpm", [128, 2, 512], BF16); b_cmpm = Buf("cmpm")
        bimp = sb("bimp", [128, 4, 64], F32); b_bimp = Buf("bimp")
        impacc = sb("impacc", [128, 4, 64], F32); b_imp = Buf("imp")
        impw = sb("impw", [128, 4, 64], F32); b_impw = Buf("impw")
        top = sb("top", [128, 4, 16], F32); b_top = Buf("top")
        selm = sb("selm", [128, 4, 64], BF16); b_selm = Buf("selm")
        rsb = sb("rsb", [128, 8], F32); b_rsb = Buf("rsb")
        otmp = sb("otmp", [128, 4, 128], F32); b_otmp = Buf("otmp")
        hid = sb("hid", [128, 2, 32], BF16); b_hid = Buf("hid")
        vctmp = sb("vctmp", [32, 128], BF16); b_vctmp = Buf("vctmp")
        pA = [ps("pA%d" % i, [128, 512], F32) for i in range(4)]; b_pA = [Buf("pA%d" % i, True) for i in range(4)]
        pO = [ps("pO%d" % i, [128, 2, 256], F32) for i in range(2)]; b_pO = [Buf("pO%d" % i, True) for i in range(2)]
        pT = [ps("pT%d" % i, [128, 8, 128], BF16) for i in range(2)]; b_pT = [Buf("pT%d" % i, True) for i in range(2)]
        print("sbuf remaining", nc.sbuf_bytes_remaining, flush=True)

        state = {"wi": 0, "pti": 0, "pai": 0, "ptr": 0, "alt": 0}

        def evac_engine():
            state["alt"] ^= 1
            return "dve" if state["alt"] else "act"

        def front_end(tb):
            t0 = tb * TB
            for tt in range(4):
                P.dma("sp", xf[:], x[t0 + tt * 128: t0 + (tt + 1) * 128, :], writes=[b_xf])
                P.op("dve", lambda e: e.memset(ss[:, 0:1], 0.0), writes=[b_ss])
                P.op("act", lambda e, tt=tt: e.activation(out=junk[:], in_=xf[:], func=AF.Square, accum_out=ss[:, 0:1]),
                     reads=[b_xf], writes=[b_xh, b_ss])
                P.op("act", lambda e: e.activation(out=ss[:, 1:2], in_=ss[:, 0:1], func=AF.Ln, bias=epst[:], scale=1.0 / D),
                     reads=[b_ss, cB], writes=[b_ss])
                P.op("act", lambda e: e.activation(out=ss[:, 2:3], in_=ss[:, 1:2], func=AF.Exp, scale=-0.5),
                     reads=[b_ss], writes=[b_ss])
                P.op("dve", lambda e: e.tensor_scalar(out=xh[:], in0=xf[:], scalar1=ss[:, 2:3], scalar2=None, op0=ALU.mult),
                     reads=[b_xf, b_ss], writes=[b_xh])
                for k8 in range(4):
                    pt = pT[state["ptr"] % 2]; bpt = b_pT[state["ptr"] % 2]; state["ptr"] += 1
                    for j in range(8):
                        kc = k8 * 8 + j
                        P.op("pe", lambda e, pt=pt, j=j, kc=kc: e.transpose(out=pt[:, j, :], in_=xh[:, kc * 128:(kc + 1) * 128], identity=ident[:]),
                             reads=[b_xh, cB], writes=[bpt])
                    P.op("dve", lambda e, pt=pt, k8=k8, tt=tt: e.tensor_tensor(
                        out=xnT[:, k8 * 8:(k8 + 1) * 8, tt * 128:(tt + 1) * 128], in0=pt[:],
                        in1=gcol[:, k8 * 8:(k8 + 1) * 8].unsqueeze(2).to_broadcast([128, 8, 128]), op=ALU.mult),
                        reads=[bpt, cB], writes=[b_xnT[k8]])

        def load_w(col0, ncols, kq):
            i = state["wi"] % NW; state["wi"] += 1
            P.dma("pool", wt[i][:, :, 0:ncols], w[:, kq * 8:(kq + 1) * 8, col0:col0 + ncols], writes=[b_wt[i]])
            return wt[i], b_wt[i]

        def projections(tb):
            t0 = tb * TB
            for cg in range(3):
                for kq in range(4):
                    ws, bws = load_w(cg * 512, 512, kq)
                    order = range(4)
                    for mt in order:
                        for kci in range(8):
                            kc = kq * 8 + kci
                            P.op("pe", lambda e, mt=mt, kci=kci, kc=kc, ws=ws: e.matmul(
                                pA[mt][:], lhsT=ws[:, kci, mt * 128:(mt + 1) * 128], rhs=xnT[:, kc, :], start=(kc == 0), stop=(kc == 31)),
                                reads=[bws, b_xnT[kc // 8]], writes=[b_pA[mt]])
                for mt in range(4):
                    M = cg * 4 + mt
                    if M < 8:
                        dst, bd, scale = qT[:, M, :], b_qT, QSCALE
                    elif M == 8:
                        dst, bd, scale = rawT[:, 0, 16:528], b_raw, 1.0
                    elif M == 9:
                        dst, bd, scale = kT[:, 0, t0:t0 + TB], b_kT, 1.0
                    elif M == 10:
                        dst, bd, scale = kT[:, 1, t0:t0 + TB], b_kT, 1.0
                    else:
                        dst, bd, scale = rawT[:, 1, 16:528], b_raw, 1.0
                    if evac_engine() == "act":
                        P.op("act", lambda e, dst=dst, mt=mt, scale=scale: e.activation(out=dst, in_=pA[mt][:], func=AF.Copy, scale=scale),
                             reads=[b_pA[mt]], writes=[bd])
                    else:
                        P.op("dve", lambda e, dst=dst, mt=mt, scale=scale: e.tensor_scalar(out=dst, in0=pA[mt][:], scalar1=scale, scalar2=None, op0=ALU.mult),
                             reads=[b_pA[mt]], writes=[bd])
            for kq in range(4):
                ws, bws = load_w(NQK, NVG, kq)
                for tt in range(4):
                    for kci in range(8):
                        kc = kq * 8 + kci
                        P.op("pe", lambda e, tt=tt, kci=kci, kc=kc, ws=ws: e.matmul(
                            pA[tt][:, 0:NVG], lhsT=xnT[:, kc, tt * 128:(tt + 1) * 128], rhs=ws[:, kci, 0:NVG], start=(kc == 0), stop=(kc == 31)),
                            reads=[bws, b_xnT[kc // 8]], writes=[b_pA[tt]])
            for tt in range(4):
                kt = 4 * tb + tt
                P.op("dve", lambda e, tt=tt, kt=kt: e.tensor_copy(out=Vaug[:, :, kt, 0:128], in_=pA[tt][:, 0:256].rearrange("p (b d) -> p b d", b=2)),
                     reads=[b_pA[tt]], writes=[b_V])
                P.op("act", lambda e, tt=tt: e.activation(out=gates[:, tt, :], in_=pA[tt][:, 256:280], func=AF.Sigmoid),
                     reads=[b_pA[tt]], writes=[b_gates])
            for cg in range(6):
                for kq in range(4):
                    ws, bws = load_w(NQK + NVG + cg * 512, 512, kq)
                    for tt in range(4):
                        for kci in range(8):
                            kc = kq * 8 + kci
                            P.op("pe", lambda e, tt=tt, kci=kci, kc=kc, ws=ws: e.matmul(
                                pA[tt][:], lhsT=xnT[:, kc, tt * 128:(tt + 1) * 128], rhs=ws[:, kci, :], start=(kc == 0), stop=(kc == 31)),
                                reads=[bws, b_xnT[kc // 8]], writes=[b_pA[tt]])
                for tt in range(4):
                    P.op("act", lambda e, tt=tt, cg=cg: e.activation(out=zs[:, tt, cg * 512:(cg + 1) * 512], in_=pA[tt][:], func=AF.Silu),
                         reads=[b_pA[tt]], writes=[b_zs])

        def compress(tb):
            t0 = tb * TB
            n_lo = max(0, 32 * tb - 1)
            n_hi = 32 * tb + 30
            cnt = n_hi - n_lo + 1
            col0 = 16 * n_lo - (t0 - 16)
            i = state["wi"] % NW; state["wi"] += 1
            w1 = wt[i]; bw1 = b_wt[i]
            w1v_ = w1[:].rearrange("p a c -> p (a c)").rearrange("p (l e) -> p l e", e=128)
            i2 = state["wi"] % NW; state["wi"] += 1
            w1b = wt[i2]; bw1b = b_wt[i2]
            w1bv_ = w1b[:].rearrange("p a c -> p (a c)").rearrange("p (l e) -> p l e", e=128)
            P.dma("pool", w1v_, w1k_d, writes=[bw1])
            P.dma("pool", w1bv_, w1v_d, writes=[bw1b])
            if tb == 0:
                for kv, (wv, bw, pos) in enumerate([(w1v_, bw1, posk), (w1bv_, bw1b, posv)]):
                    for l in range(32):
                        P.op("pe", lambda e, wv=wv, l=l, pos=pos, kv=kv: e.matmul(pA[kv][:, 0:1], lhsT=wv[:, l, :], rhs=pos[:, l:l + 1], start=(l == 0), stop=(l == 31)),
                             reads=[bw, cB], writes=[b_pA[kv]])
                    P.op("dve", lambda e, kv=kv: e.tensor_copy(out=cbias[:, kv:kv + 1], in_=pA[kv][:, 0:1]), reads=[b_pA[kv]], writes=[b_cbias])
            for kv, (wv, bw) in enumerate([(w1v_, bw1), (w1bv_, bw1b)]):
                for l in range(32):
                    P.op("pe", lambda e, wv=wv, l=l, kv=kv: e.matmul(pA[2 + kv][:, 0:cnt], lhsT=wv[:, l, :],
                                                                   rhs=rawT[:, kv, col0 + l: col0 + l + 16 * (cnt - 1) + 1: 16], start=(l == 0), stop=(l == 31)),
                         reads=[bw, b_raw], writes=[b_pA[2 + kv]])
                P.op("act", lambda e, kv=kv: e.activation(out=hid[:, kv, 0:cnt], in_=pA[2 + kv][:, 0:cnt], func=AF.Silu, bias=cbias[:, kv:kv + 1], scale=1.0),
                     reads=[b_pA[2 + kv], b_cbias], writes=[b_hid])
            P.op("pe", lambda e: e.matmul(pA[0][:, 0:cnt], lhsT=w2k[:], rhs=hid[:, 0, 0:cnt], start=True, stop=True),
                 reads=[b_hid, cB], writes=[b_pA[0]])
            P.op("dve", lambda e: e.tensor_copy(out=kcT[:, n_lo:n_lo + cnt], in_=pA[0][:, 0:cnt]), reads=[b_pA[0]], writes=[b_kc])
            P.op("pe", lambda e: e.matmul(pA[1][0:cnt, 0:128], lhsT=hid[:, 1, 0:cnt], rhs=w2v[:], start=True, stop=True),
                 reads=[b_hid, cB], writes=[b_pA[1]])
            P.op("dve", lambda e: e.tensor_copy(out=vctmp[0:cnt, :], in_=pA[1][0:cnt, 0:128]), reads=[b_pA[1]], writes=[b_vctmp])
            a = n_lo
            while a <= n_hi:
                ctt = a // 128
                bnd = min(n_hi, 128 * ctt + 127)
                P.dma("sp", vcaug[a - 128 * ctt: bnd - 128 * ctt + 1, ctt, 0:128], vctmp[a - n_lo: bnd - n_lo + 1, :], reads=[b_vctmp], writes=[b_vc])
                a = bnd + 1
            P.op("dve", lambda e: e.tensor_copy(out=rawT[:, :, 0:16], in_=rawT[:, :, 512:528]), reads=[b_raw], writes=[b_raw])

        def next_pa():
            i = state["pai"] % 4; state["pai"] += 1
            return i

        def next_pt():
            i = state["pti"] % NPT; state["pti"] += 1
            return i

        def combine(tb, hl, br, first):
            for bk in range(2):
                P.op("dve", lambda e, bk=bk: e.tensor_scalar(out=rsb[:, 2 * bk:2 * bk + 2], in0=pO[bk][:, :, 128], scalar1=1e-30, scalar2=None, op0=ALU.max),
                     reads=[b_pO[bk]], writes=[b_rsb])
            if br == 2 and tb == 0:
                P.op("dve", lambda e: e.tensor_tensor(out=rsb[:, 0:4], in0=rsb[:, 0:4], in1=winx[:, :, hl], op=ALU.add), reads=[b_rsb, cB], writes=[b_rsb])
            P.op("dve", lambda e: e.reciprocal(out=rsb[:, 4:8], in_=rsb[:, 0:4]), reads=[b_rsb], writes=[b_rsb])
            return

        def combine2(tb, hl, br):
            gi = br * 8 + hl
            P.op("dve", lambda e: e.tensor_tensor(out=rsb[:, 0:4], in0=rsb[:, 4:8], in1=gates[:, :, gi], op=ALU.mult),
                 reads=[b_rsb, b_gates], writes=[b_rsb])
            for i in range(4):
                bk, j = i // 2, i % 2
                zsl = zs[:, i, gi * 128:(gi + 1) * 128]
                if br == 0:
                    P.op("dve", lambda e, i=i, bk=bk, j=j, zsl=zsl: e.scalar_tensor_tensor(
                        out=mix[:, i, hl * 128:(hl + 1) * 128], in0=pO[bk][:, j, 0:128], scalar=rsb[:, i:i + 1], in1=zsl, op0=ALU.mult, op1=ALU.mult),
                        reads=[b_pO[bk], b_rsb, b_zs], writes=[b_mix])
                else:
                    P.op("dve", lambda e, i=i, bk=bk, j=j, zsl=zsl: e.scalar_tensor_tensor(
                        out=otmp[:, i, :], in0=pO[bk][:, j, 0:128], scalar=rsb[:, i:i + 1], in1=zsl, op0=ALU.mult, op1=ALU.mult),
                        reads=[b_pO[bk], b_rsb, b_zs], writes=[b_otmp])
                    P.op("dve", lambda e, i=i: e.tensor_tensor(out=mix[:, i, hl * 128:(hl + 1) * 128], in0=mix[:, i, hl * 128:(hl + 1) * 128], in1=otmp[:, i, :], op=ALU.add),
                         reads=[b_otmp, b_mix], writes=[b_mix])

        def pv(pt_i, a, b_, vrhs, bv, first_flags, last_sub):
            for i in range(a // 128, b_ // 128):
                bk, j = i // 2, i % 2
                st_flag = first_flags[bk]
                first_flags[bk] = False
                P.op("pe", lambda e, i=i, bk=bk, j=j, st_flag=st_flag: e.matmul(
                    pO[bk][:, j, 0:129], lhsT=PT[pt_i][:, i * 128:(i + 1) * 128], rhs=vrhs, start=st_flag, stop=False, skip_group_check=True),
                    reads=[b_PT[pt_i], bv], writes=[b_pO[bk]])

        def score_tile(hl, klhs, bk_, a, b_, bias_ap, ex_lhs, ex_rhs_fn, masks, extra_reads):
            pa = next_pa()
            n_extra = 1 + len(masks)
            P.op("pe", lambda e: e.matmul(pA[pa][:, a:b_], lhsT=klhs, rhs=qT[:, hl, a:b_], start=True, stop=False),
                 reads=[bk_, b_qT], writes=[b_pA[pa]])
            P.op("pe", lambda e: e.matmul(pA[pa][:, a:b_], lhsT=ex_lhs, rhs=ex_rhs_fn(a, b_), start=False, stop=(len(masks) == 0)),
                 reads=[cB, b_RS], writes=[b_pA[pa]])
            for mi, (c0, c1, tab, btab) in enumerate(masks):
                P.op("pe", lambda e, c0=c0, c1=c1, tab=tab, mi=mi: e.matmul(pA[pa][:, c0:c1], lhsT=ident[:], rhs=tab, start=False, stop=(mi == len(masks) - 1)),
                     reads=[cB, btab], writes=[b_pA[pa]])
            pti = next_pt()
            P.op("act", lambda e: e.activation(out=PT[pti][:, a:b_], in_=pA[pa][:, a:b_], func=AF.Exp, bias=bias_ap, scale=1.0),
                 reads=[b_pA[pa], cB], writes=[b_PT[pti]])
            return pti

        def attn_cmp(tb):
            qt = tb
            P.dma("sp", cmpm[:], cmpm_d[:, qt, :, :], writes=[b_cmpm])
            cts = [0] if qt < 4 else [0, 1]
            for hl in range(8):
                ff = [True, True]
                ptis = []
                for ct in cts:
                    need_mask = not (ct == 0 and qt >= 5)
                    masks = [(0, 512, cmpm[:, ct, :], b_cmpm)] if need_mask else []
                    pti = score_tile(hl, kcT[:, ct * 128:(ct + 1) * 128], b_kc, 0, 512,
                                     kbc[:, hl, ct * 8 + qt: ct * 8 + qt + 1],
                                     EX[64:67, 0, :], lambda a, b_, hl=hl: RS[64:67, hl, a:b_], masks, [])
                    ptis.append((ct, pti))
                    pv(pti, 0, 512, vcaug[:, ct, :], b_vc, ff, None)
                pa = next_pa()
                for i in range(4):
                    for k, (ct, pti) in enumerate(ptis):
                        P.op("pe", lambda e, i=i, ct=ct, pti=pti, k=k: e.matmul(
                            pA[pa][:, i * 64:(i + 1) * 64], lhsT=PT[pti][:, i * 128:(i + 1) * 128], rhs=OV[:, ct, :], start=(k == 0), stop=(k == len(ptis) - 1)),
                            reads=[b_PT[pti], cB], writes=[b_pA[pa]])
                combine(tb, hl, 0, None)
                for i in range(4):
                    if hl == 0:
                        P.op("dve", lambda e, i=i: e.tensor_scalar(out=impacc[:, i, :], in0=pA[pa][:, i * 64:(i + 1) * 64], scalar1=rsb[:, 4 + i:5 + i], scalar2=None, op0=ALU.mult),
                             reads=[b_pA[pa], b_rsb], writes=[b_imp])
                    else:
                        P.op("dve", lambda e, i=i: e.scalar_tensor_tensor(out=impacc[:, i, :], in0=pA[pa][:, i * 64:(i + 1) * 64], scalar=rsb[:, 4 + i:5 + i], in1=impacc[:, i, :], op0=ALU.mult, op1=ALU.add),
                             reads=[b_pA[pa], b_rsb, b_imp], writes=[b_imp])
                combine2(tb, hl, 0)

        def topk(tb):
            t0 = tb * TB
            P.dma("sp", bimp[:], bimp_d[t0:t0 + TB, :].rearrange("(t p) j -> p t j", p=128), writes=[b_bimp])
            P.op("dve", lambda e: e.tensor_tensor(out=impacc[:], in0=impacc[:], in1=bimp[:], op=ALU.add), reads=[b_imp, b_bimp], writes=[b_imp])
            for i in range(4):
                P.op("dve", lambda e, i=i: e.max(out=top[:, i, 0:8], in_=impacc[:, i, :]), reads=[b_imp], writes=[b_top])
                P.op("dve", lambda e, i=i: e.match_replace(out=impw[:, i, :], in_to_replace=top[:, i, 0:8], in_values=impacc[:, i, :], imm_value=-3.0e38),
                     reads=[b_imp, b_top], writes=[b_impw])
                P.op("dve", lambda e, i=i: e.max(out=top[:, i, 8:16], in_=impw[:, i, :]), reads=[b_impw], writes=[b_top])
                P.op("dve", lambda e, i=i: e.tensor_scalar(out=selm[:, i, :], in0=impacc[:, i, :], scalar1=top[:, i, 15:16], scalar2=1.0, op0=ALU.is_ge, op1=ALU.subtract),
                     reads=[b_imp, b_top], writes=[b_selm])

        def sel_transpose(tb):
            pt = pT[state["ptr"] % 2]; bpt = b_pT[state["ptr"] % 2]; state["ptr"] += 1
            for i in range(4):
                P.op("pe", lambda e, i=i: e.transpose(out=pt[0:64, i, :], in_=selm[:, i, :], identity=ident[:]),
                     reads=[b_selm, cB], writes=[bpt])
            P.op("dve", lambda e: e.tensor_copy(out=RS[0:64, 0, :].rearrange("p (i c) -> p i c", i=4), in_=pt[0:64, 0:4, :]), reads=[bpt], writes=[b_RS])
            for h in range(1, 8):
                P.op("dve", lambda e, h=h: e.tensor_copy(out=RS[0:64, h, :], in_=RS[0:64, 0, :]), reads=[b_RS], writes=[b_RS])

        def attn_dense(tb, br):
            qt = tb
            bi = br - 1
            for hl in range(8):
                ff = [True, True]
                tiles = []
                if br == 2:
                    for u in range(4):
                        kt = 4 * qt - 4 + u
                        if kt >= 0:
                            tiles.append((kt, 0, 128 * (u + 1), [(128 * u, 128 * u + 128, triw[:], cB)]))
                else:
                    for kt in range(4 * qt):
                        tiles.append((kt, 0, 512, []))
                for u in range(4):
                    tiles.append((4 * qt + u, 128 * u, 512, [(128 * u, 128 * u + 128, tric[:], cB)]))
                pend = []
                LOOK = 2
                for (kt, a, b_, masks) in tiles:
                    dl = kt - 4 * qt + 28
                    if br == 1:
                        ex_lhs = EX[0:67, kt, :]
                        ex_rhs = lambda a, b_, hl=hl: RS[0:67, hl, a:b_]
                    else:
                        ex_lhs = EX[64:67, 0, :]
                        ex_rhs = lambda a, b_, hl=hl: RS[64:67, hl, a:b_]
                    pti = score_tile(hl, kT[:, bi, kt * 128:(kt + 1) * 128], b_kT, a, b_, kb[:, hl, dl:dl + 1], ex_lhs, ex_rhs, masks, [])
                    pend.append((pti, a, b_, Vaug[:, bi, kt, :]))
                    if len(pend) > LOOK:
                        p_ = pend.pop(0)
                        pv(p_[0], p_[1], p_[2], p_[3], b_V, ff, None)
                for p_ in pend:
                    pv(p_[0], p_[1], p_[2], p_[3], b_V, ff, None)
                combine(tb, hl, br, None)
                combine2(tb, hl, br)

        def store_mix(tb):
            t0 = tb * TB
            for tt in range(4):
                P.dma("pool", mix_d[t0 + tt * 128:t0 + (tt + 1) * 128, :], mix[:, tt, :], reads=[b_mix])

        for tb in range(NTB):
            front_end(tb)
            if stage >= 1: projections(tb)
            if stage >= 2: compress(tb)
            if stage >= 3: attn_cmp(tb)
            if stage >= 4: topk(tb)
            if stage >= 5: attn_dense(tb, 2)
            if stage >= 6: sel_transpose(tb)
            if stage >= 7: attn_dense(tb, 1)
            if stage >= 3: store_mix(tb)
        if dbg:
            def dump(name, t, bufs, q="sp"):
                shp = list(t.shape)
                d_ = dram("dbg_" + name, shp, t.dtype, kind="ExternalOutput")
                P.dma(q, d_, t[:], reads=bufs)
            dump("xnT", xnT, b_xnT); dump("qT", qT, [b_qT]); dump("kT", kT, [b_kT]); dump("raw", rawT, [b_raw])
            dump("zs", zs, [b_zs]); dump("gates", gates, [b_gates]); dump("V", Vaug, [b_V])
            dump("kc", kcT, [b_kc]); dump("vc", vcaug, [b_vc]); dump("mixf", mix, [b_mix])
            dump("imp", impacc, [b_imp]); dump("selm", selm, [b_selm]); dump("RS", RS, [b_RS]); dump("top", top, [b_top])
        if ctx is None:
            P.finish()
        else:
            P.barrier()
        print("ninst", P.ninst, "nwaits", P.nwaits, flush=True)
    return nc


def nsa_inputs(z, b, g, tabs=None):
    cols = nsa_weight_cols(g)
    wl = w_layout(np.ascontiguousarray(z["a_w_in"][0][:, cols]))
    m = {"x": np.ascontiguousarray(z["x"][b]), "gcol": np.ascontiguousarray(z["a_norm_g"][0].reshape(32, 128).T), "w": wl}
    m["w1k"] = np.ascontiguousarray(z["a_cmp_w1_k"][0].transpose(1, 0, 2)); m["w1v"] = np.ascontiguousarray(z["a_cmp_w1_v"][0].transpose(1, 0, 2))
    m["w2k"] = z["a_cmp_w2_k"][0]; m["w2v"] = z["a_cmp_w2_v"][0]
    m["posk"] = np.ascontiguousarray(z["a_cmp_pos_k"][0].T); m["posv"] = np.ascontiguousarray(z["a_cmp_pos_v"][0].T)
    m.update(nsa_tables(g))
    return m


MQK = 1280
MV = 256
MZ = 1024
MCOL = MQK + MV + MZ


def moba_tables(j):
    hl = np.arange(8)
    m = (2.0 ** (-8.0 * (8 * j + hl + 1) / 32)).astype(np.float32)
    p = np.arange(128)
    t = {}
    dl = np.arange(32) - 28
    t["kb"] = (m[None, :, None] * (128.0 * dl[None, None, :] + p[:, None, None])).astype(np.float32)
    c = np.arange(512, dtype=np.float32)
    v = -(m[:, None] * c[None, :]).astype(np.float32)
    t["rsinit"] = bf16_split3(v)
    ex = np.zeros((67, 32, 128), np.float32)
    for kt in range(32):
        ex[kt // 2, kt, :] = 30000.0
    ex[64:67] = 1.0
    t["ex"] = ex.astype(NPBF)
    r = p[:, None]; cc = np.arange(128)[None, :]
    t["tric"] = np.where(r > cc, NEGM, 0.0).astype(NPBF)
    t["idn"] = np.eye(128, dtype=np.float32).astype(NPBF)
    mt = np.zeros((8, 4, 16), np.float32)
    for tb in range(8):
        for tt in range(4):
            bt = (4 * tb + tt) // 2
            mt[tb, tt, bt:] = -1e30
    t["mtab"] = np.ascontiguousarray(np.broadcast_to(mt.reshape(1, 8, 64), (128, 8, 64))).astype(np.float32)
    return t


def moba_weight(z, j):
    H = 32
    d = np.arange(DH)
    wq = [z["b_w_in"][0][:, (8 * j + hl) * DH + d] for hl in range(8)]
    wk = [z["kv_w"][:, (0 * 8 + 2 * j + gi) * DH + d] for gi in range(2)]
    wv = [z["kv_w"][:, (1 * 8 + 2 * j + gi) * DH + d] for gi in range(2)]
    wz = [z["b_w_in"][0][:, H * DH + (8 * j + hl) * DH + d] for hl in range(8)]
    w = np.concatenate(wq + wk + wv + wz, axis=1)
    assert w.shape[1] == MCOL
    return w_layout(w)


def build_moba(NTB=8, dbg=False, ctx=None, io=None):
    nc = bass.Bass("TRN2", target_bir_lowering=False) if ctx is None else ctx["nc"]
    pfx = "" if ctx is None else ctx["pfx"]

    def dram(name, shape, dt, kind="ExternalInput"):
        if io is not None and name in io:
            return io[name]
        return nc.dram_tensor(pfx + name, shape, dt, kind=kind).ap()
    x = dram("x", [S, D], F32)
    gcol_d = dram("gcol", [128, 32], F32)
    gkv_d = dram("gkv", [128, 32], F32)
    w = dram("w", [128, 32, MCOL], F32)
    kb_d = dram("kb", [128, 8, 32], F32)
    rsinit_d = dram("rsinit", [3, 8, 512], BF16)
    ex_d = dram("ex", [67, 32, 128], BF16)
    tric_d = dram("tric", [128, 128], BF16)
    idn_d = dram("idn", [128, 128], BF16)
    mtab_d = dram("mtab", [128, 8, 64], F32)
    mix_d = dram("mix", [S, 1024], BF16, kind="ExternalOutput")

    with ExitStack() as st:
        P = Prog(nc, st) if ctx is None else ctx["P"]
        sb = lambda name, shape, dt: st.enter_context(nc.sbuf_tensor(pfx + "s_" + name, shape, dt))
        ps = lambda name, shape, dt: st.enter_context(nc.psum_tensor(pfx + "p_" + name, shape, dt))
        cB = Buf("const")
        ident = sb("ident", [128, 128], BF16)
        gcol = sb("gcol", [128, 32], F32); gkv = sb("gkv", [128, 32], F32); rcol = sb("rcol", [128, 32], F32)
        kb = sb("kb", [128, 8, 32], F32)
        EX = sb("EX", [67, 32, 128], BF16)
        tric = sb("tric", [128, 128], BF16)
        RS = sb("RS", [67, 8, 512], BF16); b_RS = Buf("RS")
        P.op("dve", lambda e: e.memset(RS[0:64, :, :], 0.0), writes=[b_RS])
        for (dst, src) in [(ident, idn_d), (gcol, gcol_d), (gkv, gkv_d), (kb, kb_d), (EX, ex_d), (tric, tric_d)]:
            bb = Buf()
            P.dma("sp", dst[:], src, writes=[bb])
            for sk, v in bb.w.items():
                cB.w[sk] = max(cB.w.get(sk, 0), v)
        P.dma("sp", RS[64:67, :, :], rsinit_d, writes=[b_RS])
        epst = sb("epst", [128, 1], F32)
        P.op("dve", lambda e: e.memset(epst[:], EPS), writes=[cB])
        P.op("dve", lambda e: e.reciprocal(out=rcol[:], in_=gcol[:]), reads=[cB], writes=[cB])
        P.op("dve", lambda e: e.tensor_tensor(out=rcol[:], in0=rcol[:], in1=gkv[:], op=ALU.mult), reads=[cB], writes=[cB])
        kT = sb("kT", [128, 2, S], BF16); b_kT = Buf("kT")
        Vaug = sb("Vaug", [128, 2, 32, 129], BF16); b_V = Buf("V")
        kmean = sb("kmean", [128, 2, 16], BF16); b_km = Buf("km")
        kms = sb("kms", [128, 2, 2], F32); b_kms = Buf("kms")
        P.op("dve", lambda e: e.memset(Vaug[:], 1.0), writes=[b_V])
        P.op("dve", lambda e: e.memset(kmean[:], 0.0), writes=[b_km])
        xf = sb("xf", [128, D], F32); b_xf = Buf("xf")
        xh = sb("xh", [128, D], BF16); b_xh = Buf("xh")
        ss = sb("ss", [128, 4], F32); b_ss = Buf("ss")
        xnT = sb("xnT", [128, 32, TB], BF16); b_xnT = [Buf("xnT%d" % i) for i in range(4)]
        NW = 4
        wt = [sb("wt%d" % i, [128, 8, 512], BF16) for i in range(NW)]; b_wt = [Buf("wt%d" % i) for i in range(NW)]
        qT = sb("qT", [128, 8, TB], BF16); b_qT = Buf("qT")
        zs = sb("zs", [128, 4, MZ], BF16); b_zs = Buf("zs")
        mix = sb("mix", [128, 4, 1024], F32); b_mix = Buf("mix")
        NPT = 4
        PT = [sb("PT%d" % i, [128, TB], BF16) for i in range(NPT)]; b_PT = [Buf("PT%d" % i) for i in range(NPT)]
        mtab = sb("mtab", [128, 64], F32); b_mtab = Buf("mtab")
        sblk = sb("sblk", [128, 8, 64], F32); b_sblk = Buf("sblk")
        top = sb("top", [128, 32, 8], F32); b_top = Buf("top")
        selm = sb("selm", [128, 8, 4, 16], BF16); b_selm = Buf("selm")
        rsb = sb("rsb", [128, 8], F32); b_rsb = Buf("rsb")
        pA = [ps("pA%d" % i, [128, 512], F32) for i in range(4)]; b_pA = [Buf("pA%d" % i, True) for i in range(4)]
        pO = [ps("pO%d" % i, [128, 2, 256], F32) for i in range(2)]; b_pO = [Buf("pO%d" % i, True) for i in range(2)]
        pT = [ps("pT%d" % i, [128, 8, 128], BF16) for i in range(2)]; b_pT = [Buf("pT%d" % i, True) for i in range(2)]
        print("sbuf remaining", nc.sbuf_bytes_remaining, flush=True)
        state = {"wi": 0, "pti": 0, "pai": 0, "ptr": 0, "alt": 0}

        def evac_engine():
            state["alt"] ^= 1
            return "dve" if state["alt"] else "act"

        def front_end(tb):
            t0 = tb * TB
            for tt in range(4):
                P.dma("sp", xf[:], x[t0 + tt * 128: t0 + (tt + 1) * 128, :], writes=[b_xf])
                P.op("dve", lambda e: e.memset(ss[:, 0:1], 0.0), writes=[b_ss])
                P.op("act", lambda e: e.activation(out=xh[:], in_=xf[:], func=AF.Square, accum_out=ss[:, 0:1]), reads=[b_xf], writes=[b_xh, b_ss])
                P.op("act", lambda e: e.activation(out=ss[:, 1:2], in_=ss[:, 0:1], func=AF.Ln, bias=epst[:], scale=1.0 / D), reads=[b_ss, cB], writes=[b_ss])
                P.op("act", lambda e: e.activation(out=ss[:, 2:3], in_=ss[:, 1:2], func=AF.Exp, scale=-0.5), reads=[b_ss], writes=[b_ss])
                P.op("dve", lambda e: e.tensor_scalar(out=xh[:], in0=xf[:], scalar1=ss[:, 2:3], scalar2=None, op0=ALU.mult), reads=[b_xf, b_ss], writes=[b_xh])
                for k8 in range(4):
                    pt = pT[state["ptr"] % 2]; bpt = b_pT[state["ptr"] % 2]; state["ptr"] += 1
                    for j in range(8):
                        kc = k8 * 8 + j
                        P.op("pe", lambda e, pt=pt, j=j, kc=kc: e.transpose(out=pt[:, j, :], in_=xh[:, kc * 128:(kc + 1) * 128], identity=ident[:]),
                             reads=[b_xh, cB], writes=[bpt])
                    P.op("dve", lambda e, pt=pt, k8=k8, tt=tt: e.tensor_tensor(
                        out=xnT[:, k8 * 8:(k8 + 1) * 8, tt * 128:(tt + 1) * 128], in0=pt[:],
                        in1=gcol[:, k8 * 8:(k8 + 1) * 8].unsqueeze(2).to_broadcast([128, 8, 128]), op=ALU.mult),
                        reads=[bpt, cB], writes=[b_xnT[k8]])

        def load_w(col0, ncols, kq, rescale=False):
            i = state["wi"] % NW; state["wi"] += 1
            P.dma("pool", wt[i][:, :, 0:ncols], w[:, kq * 8:(kq + 1) * 8, col0:col0 + ncols], writes=[b_wt[i]])
            if rescale:
                P.op("dve", lambda e: e.tensor_tensor(out=wt[i][:, :, 0:ncols], in0=wt[i][:, :, 0:ncols],
                                                      in1=rcol[:, kq * 8:(kq + 1) * 8].unsqueeze(2).to_broadcast([128, 8, ncols]), op=ALU.mult),
                     reads=[b_wt[i], cB], writes=[b_wt[i]])
            return wt[i], b_wt[i]

        def projections(tb):
            t0 = tb * TB
            for cg in range(3):
                nmt = 4 if cg < 2 else 2
                for kq in range(4):
                    ws, bws = load_w(cg * 512, nmt * 128, kq, rescale=(cg == 2))
                    for mt in range(nmt):
                        for kci in range(8):
                            kc = kq * 8 + kci
                            P.op("pe", lambda e, mt=mt, kci=kci, kc=kc, ws=ws: e.matmul(
                                pA[mt][:], lhsT=ws[:, kci, mt * 128:(mt + 1) * 128], rhs=xnT[:, kc, :], start=(kc == 0), stop=(kc == 31)),
                                reads=[bws, b_xnT[kc // 8]], writes=[b_pA[mt]])
                for mt in range(nmt):
                    M = cg * 4 + mt
                    if M < 8:
                        dst, bd, scale = qT[:, M, :], b_qT, QSCALE
                    else:
                        gi = M - 8
                        dst, bd, scale = kT[:, gi, t0:t0 + TB], b_kT, 1.0
                        P.op("dve", lambda e, mt=mt, gi=gi: e.tensor_reduce(out=kms[:, gi, :], in_=pA[mt][:].rearrange("p (b t) -> p b t", b=2), axis=AX.X, op=ALU.add),
                             reads=[b_pA[mt]], writes=[b_kms])
                        P.op("dve", lambda e, gi=gi: e.tensor_scalar(out=kmean[:, gi, 2 * tb:2 * tb + 2], in0=kms[:, gi, :], scalar1=1.0 / 256, scalar2=None, op0=ALU.mult),
                             reads=[b_kms], writes=[b_km])
                    if evac_engine() == "act":
                        P.op("act", lambda e, dst=dst, mt=mt, scale=scale: e.activation(out=dst, in_=pA[mt][:], func=AF.Copy, scale=scale),
                             reads=[b_pA[mt]], writes=[bd])
                    else:
                        P.op("dve", lambda e, dst=dst, mt=mt, scale=scale: e.tensor_scalar(out=dst, in0=pA[mt][:], scalar1=scale, scalar2=None, op0=ALU.mult),
                             reads=[b_pA[mt]], writes=[bd])
            for kq in range(4):
                ws, bws = load_w(MQK, MV, kq, rescale=True)
                for tt in range(4):
                    for kci in range(8):
                        kc = kq * 8 + kci
                        P.op("pe", lambda e, tt=tt, kci=kci, kc=kc, ws=ws: e.matmul(
                            pA[tt][:, 0:MV], lhsT=xnT[:, kc, tt * 128:(tt + 1) * 128], rhs=ws[:, kci, 0:MV], start=(kc == 0), stop=(kc == 31)),
                            reads=[bws, b_xnT[kc // 8]], writes=[b_pA[tt]])
            for tt in range(4):
                kt = 4 * tb + tt
                P.op("act", lambda e, tt=tt, kt=kt: e.activation(out=Vaug[:, :, kt, 0:128], in_=pA[tt][:, 0:256].rearrange("p (b d) -> p b d", b=2), func=AF.Copy),
                     reads=[b_pA[tt]], writes=[b_V])
            for cg in range(2):
                for kq in range(4):
                    ws, bws = load_w(MQK + MV + cg * 512, 512, kq)
                    for tt in range(4):
                        for kci in range(8):
                            kc = kq * 8 + kci
                            P.op("pe", lambda e, tt=tt, kci=kci, kc=kc, ws=ws: e.matmul(
                                pA[tt][:], lhsT=xnT[:, kc, tt * 128:(tt + 1) * 128], rhs=ws[:, kci, :], start=(kc == 0), stop=(kc == 31)),
                                reads=[bws, b_xnT[kc // 8]], writes=[b_pA[tt]])
                for tt in range(4):
                    P.op("act", lambda e, tt=tt, cg=cg: e.activation(out=zs[:, tt, cg * 512:(cg + 1) * 512], in_=pA[tt][:], func=AF.Silu),
                         reads=[b_pA[tt]], writes=[b_zs])

        def gating(tb):
            P.dma("sp", mtab[:], mtab_d[:, tb, :], writes=[b_mtab])
            pa = 0
            for hl in range(8):
                gi = hl // 4
                for tt in range(4):
                    c0 = (hl * 4 + tt) * 16
                    P.op("pe", lambda e, hl=hl, tt=tt, gi=gi, c0=c0: e.matmul(pA[pa][:, c0:c0 + 16], lhsT=qT[:, hl, tt * 128:(tt + 1) * 128], rhs=kmean[:, gi, :], start=True, stop=True),
                         reads=[b_qT, b_km], writes=[b_pA[pa]])
            P.op("dve", lambda e: e.tensor_tensor(out=sblk[:], in0=pA[pa][:].rearrange("p (h c) -> p h c", h=8), in1=mtab[:].unsqueeze(1).to_broadcast([128, 8, 64]), op=ALU.add),
                 reads=[b_pA[pa], b_mtab], writes=[b_sblk])
            for hl in range(8):
                for tt in range(4):
                    idx = hl * 4 + tt
                    P.op("dve", lambda e, hl=hl, tt=tt, idx=idx: e.max(out=top[:, idx, :], in_=sblk[:, hl, tt * 16:(tt + 1) * 16]), reads=[b_sblk], writes=[b_top])
            for hl in range(8):
                for tt in range(4):
                    idx = hl * 4 + tt
                    P.op("dve", lambda e, hl=hl, tt=tt, idx=idx: e.tensor_scalar(out=selm[:, hl, tt, :], in0=sblk[:, hl, tt * 16:(tt + 1) * 16], scalar1=top[:, idx, 2:3], scalar2=1.0, op0=ALU.is_ge, op1=ALU.subtract),
                         reads=[b_sblk, b_top], writes=[b_selm])
            for tt in range(4):
                bt = (4 * tb + tt) // 2
                P.op("dve", lambda e, tt=tt, bt=bt: e.memset(selm[:, :, tt, bt:bt + 1], 0.0), writes=[b_selm])
            for h2 in range(4):
                pt = pT[state["ptr"] % 2]; bpt = b_pT[state["ptr"] % 2]; state["ptr"] += 1
                for k in range(8):
                    hl = h2 * 2 + k // 4; tt = k % 4
                    P.op("pe", lambda e, k=k, hl=hl, tt=tt: e.transpose(out=pt[0:16, k, :], in_=selm[:, hl, tt, :], identity=ident[:]),
                         reads=[b_selm, cB], writes=[bpt])
                P.op("dve", lambda e, h2=h2: e.tensor_copy(out=RS[0:16, 2 * h2:2 * h2 + 2, :].rearrange("p h (t c) -> p (h t) c", t=4), in_=pt[0:16, :, :]),
                     reads=[bpt], writes=[b_RS])

        def next_pa():
            i = state["pai"] % 4; state["pai"] += 1
            return i

        def next_pt():
            i = state["pti"] % NPT; state["pti"] += 1
            return i

        def attn(tb):
            qt = tb
            for hl in range(8):
                gi = hl // 4
                ff = [True, True]
                tiles = [(kt, 0, 512, False) for kt in range(4 * qt)] + [(4 * qt + u, 128 * u, 512, True) for u in range(4)]
                pend = []
                LOOK = 2

                def pv_(p_, gi=gi, ff=ff):
                    pti_, a_, bb_, kt_ = p_
                    for i in range(a_ // 128, bb_ // 128):
                        bk, jj = i // 2, i % 2
                        stf = ff[bk]; ff[bk] = False
                        P.op("pe", lambda e, i=i, bk=bk, jj=jj, stf=stf: e.matmul(
                            pO[bk][:, jj, 0:129], lhsT=PT[pti_][:, i * 128:(i + 1) * 128], rhs=Vaug[:, gi, kt_, :], start=stf, stop=False, skip_group_check=True),
                            reads=[b_PT[pti_], b_V], writes=[b_pO[bk]])
                for (kt, a, b_, diag) in tiles:
                    dl = kt - 4 * qt + 28
                    pa = next_pa()
                    P.op("pe", lambda e: e.matmul(pA[pa][:, a:b_], lhsT=kT[:, gi, kt * 128:(kt + 1) * 128], rhs=qT[:, hl, a:b_], start=True, stop=False),
                         reads=[b_kT, b_qT], writes=[b_pA[pa]])
                    P.op("pe", lambda e: e.matmul(pA[pa][:, a:b_], lhsT=EX[0:67, kt, :], rhs=RS[0:67, hl, a:b_], start=False, stop=(not diag)),
                         reads=[cB, b_RS], writes=[b_pA[pa]])
                    if diag:
                        P.op("pe", lambda e: e.matmul(pA[pa][:, a:a + 128], lhsT=ident[:], rhs=tric[:], start=False, stop=True),
                             reads=[cB], writes=[b_pA[pa]])
                    pti = next_pt()
                    P.op("act", lambda e: e.activation(out=PT[pti][:, a:b_], in_=pA[pa][:, a:b_], func=AF.Exp, bias=kb[:, hl, dl:dl + 1], scale=1.0),
                         reads=[b_pA[pa], cB], writes=[b_PT[pti]])
                    pend.append((pti, a, b_, kt))
                    if len(pend) > LOOK:
                        pv_(pend.pop(0))
                for p_ in pend:
                    pv_(p_)
                for bk in range(2):
                    P.op("dve", lambda e, bk=bk: e.tensor_scalar(out=rsb[:, 2 * bk:2 * bk + 2], in0=pO[bk][:, :, 128], scalar1=1e-30, scalar2=None, op0=ALU.max),
                         reads=[b_pO[bk]], writes=[b_rsb])
                P.op("dve", lambda e: e.reciprocal(out=rsb[:, 4:8], in_=rsb[:, 0:4]), reads=[b_rsb], writes=[b_rsb])
                for i in range(4):
                    bk, jj = i // 2, i % 2
                    P.op("dve", lambda e, i=i, bk=bk, jj=jj: e.scalar_tensor_tensor(
                        out=mix[:, i, hl * 128:(hl + 1) * 128], in0=pO[bk][:, jj, 0:128], scalar=rsb[:, 4 + i:5 + i], in1=zs[:, i, hl * 128:(hl + 1) * 128], op0=ALU.mult, op1=ALU.mult),
                        reads=[b_pO[bk], b_rsb, b_zs], writes=[b_mix])

        def store_mix(tb):
            t0 = tb * TB
            for tt in range(4):
                P.dma("pool", mix_d[t0 + tt * 128:t0 + (tt + 1) * 128, :], mix[:, tt, :], reads=[b_mix])

        for tb in range(NTB):
            front_end(tb)
            projections(tb)
            gating(tb)
            attn(tb)
            store_mix(tb)
        if dbg:
            def dump(name, t, bufs):
                d_ = dram("dbg_" + name, list(t.shape), t.dtype, kind="ExternalOutput")
                P.dma("sp", d_, t[:], reads=bufs)
            dump("qT", qT, [b_qT]); dump("kT", kT, [b_kT]); dump("V", Vaug, [b_V]); dump("zs", zs, [b_zs])
            dump("kmean", kmean, [b_km]); dump("sblk", sblk, [b_sblk]); dump("selm", selm, [b_selm]); dump("RS", RS, [b_RS]); dump("top", top, [b_top])
        if ctx is None:
            P.finish()
        else:
            P.barrier()
        print("ninst", P.ninst, "nwaits", P.nwaits, flush=True)
    return nc


def moba_inputs(z, h1b, j):
    m = {"x": (None if h1b is None else np.ascontiguousarray(h1b)), "gcol": np.ascontiguousarray(z["b_norm_g"][0].reshape(32, 128).T),
         "gkv": np.ascontiguousarray(z["kv_norm_g"].reshape(32, 128).T), "w": moba_weight(z, j)}
    m.update(moba_tables(j))
    return m


def build_outproj(final, NT=1024, ctx=None, io=None):
    nc = bass.Bass("TRN2", target_bir_lowering=False) if ctx is None else ctx["nc"]
    pfx = "" if ctx is None else ctx["pfx"]

    def dram(name, shape, dt, kind="ExternalInput"):
        if io is not None and name in io:
            return io[name]
        return nc.dram_tensor(pfx + name, shape, dt, kind=kind).ap()
    mixin = dram("mixin", [NT, D], BF16)
    res = dram("res", [NT, D], F32)
    w = dram("w", [128, 32, D], F32)
    idn = dram("idn", [128, 128], BF16)
    if final:
        gfin = dram("gfin", [128, D], F32)
    hout = dram("hout", [NT, D], F32, kind="ExternalOutput")
    with ExitStack() as st:
        P = Prog(nc, st) if ctx is None else ctx["P"]
        sb = lambda name, shape, dt: st.enter_context(nc.sbuf_tensor(pfx + "s_" + name, shape, dt))
        ps = lambda name, shape, dt: st.enter_context(nc.psum_tensor(pfx + "p_" + name, shape, dt))
        ident = sb("ident", [128, 128], BF16); cB = Buf()
        P.dma("sp", ident[:], idn, writes=[cB])
        if final:
            grep_ = sb("grep", [128, D], F32); b_grep = Buf()
            P.dma("sp", grep_[:], gfin, writes=[b_grep])
            ss = sb("ss", [128, 12], F32); b_ss = Buf()
            junk = sb("junk", [128, D], BF16); b_junk = Buf()
            epst = sb("epst", [128, 1], F32)
            P.op("dve", lambda e: e.memset(epst[:], EPS), writes=[cB])
        mixtok = [sb("mixtok%d" % i, [128, D], BF16) for i in range(2)]; b_mixtok = [Buf() for _ in range(2)]
        mixT = sb("mixT", [128, 32, 512], BF16); b_mixT = [Buf() for _ in range(4)]
        hb = [sb("hb%d" % i, [128, D], F32) for i in range(4)]; b_hb = [Buf() for _ in range(4)]
        NW = 3
        wt = [sb("wt%d" % i, [128, 16, 512], BF16) for i in range(NW)]; b_wt = [Buf() for _ in range(NW)]
        pacc = [ps("pacc%d" % i, [128, 512], F32) for i in range(4)]; b_pacc = [Buf("", True) for _ in range(4)]
        ptr = [ps("ptr%d" % i, [128, 8, 128], BF16) for i in range(2)]; b_ptr = [Buf("", True) for _ in range(2)]
        wi = 0
        tri = 0
        for half in range(NT // 512):
            t0 = half * 512
            for tt in range(4):
                P.dma("sp", hb[tt][:], res[t0 + tt * 128: t0 + (tt + 1) * 128, :], writes=[b_hb[tt]])
            for tt in range(4):
                mt = mixtok[tt % 2]; bmt = b_mixtok[tt % 2]
                P.dma("sp", mt[:], mixin[t0 + tt * 128: t0 + (tt + 1) * 128, :], writes=[bmt])
                for k8 in range(4):
                    pt = ptr[tri % 2]; bpt = b_ptr[tri % 2]; tri += 1
                    for j in range(8):
                        kc = k8 * 8 + j
                        P.op("pe", lambda e, pt=pt, j=j, kc=kc, mt=mt: e.transpose(out=pt[:, j, :], in_=mt[:, kc * 128:(kc + 1) * 128], identity=ident[:]),
                             reads=[bmt, cB], writes=[bpt])
                    if k8 % 2 == 0:
                        P.op("dve", lambda e, pt=pt, k8=k8, tt=tt: e.tensor_copy(out=mixT[:, k8 * 8:(k8 + 1) * 8, tt * 128:(tt + 1) * 128], in_=pt[:]),
                             reads=[bpt], writes=[b_mixT[k8]])
                    else:
                        P.op("act", lambda e, pt=pt, k8=k8, tt=tt: e.copy(out=mixT[:, k8 * 8:(k8 + 1) * 8, tt * 128:(tt + 1) * 128], in_=pt[:]),
                             reads=[bpt], writes=[b_mixT[k8]])
            for cg in range(8):
                for kh in range(2):
                    ws = wt[wi % NW]; bws = b_wt[wi % NW]; wi += 1
                    P.dma("pool", ws[:], w[:, kh * 16:(kh + 1) * 16, cg * 512:(cg + 1) * 512], writes=[bws])
                    for tt in range(4):
                        for kci in range(16):
                            kc = kh * 16 + kci
                            P.op("pe", lambda e, tt=tt, kci=kci, kc=kc, ws=ws: e.matmul(pacc[tt][:], lhsT=mixT[:, kc, tt * 128:(tt + 1) * 128], rhs=ws[:, kci, :], start=(kc == 0), stop=(kc == 31)),
                                 reads=[b_mixT[kc // 8], bws], writes=[b_pacc[tt]])
                for tt in range(4):
                    P.op("dve", lambda e, tt=tt, cg=cg: e.tensor_tensor(out=hb[tt][:, cg * 512:(cg + 1) * 512], in0=pacc[tt][:], in1=hb[tt][:, cg * 512:(cg + 1) * 512], op=ALU.add),
                         reads=[b_pacc[tt], b_hb[tt]], writes=[b_hb[tt]])
            for tt in range(4):
                if final:
                    P.op("dve", lambda e, tt=tt: e.memset(ss[:, tt:tt + 1], 0.0), writes=[b_ss])
                    P.op("act", lambda e, tt=tt: e.activation(out=junk[:], in_=hb[tt][:], func=AF.Square, accum_out=ss[:, tt:tt + 1]),
                         reads=[b_hb[tt]], writes=[b_junk, b_ss])
                    P.op("act", lambda e, tt=tt: e.activation(out=ss[:, 4 + tt:5 + tt], in_=ss[:, tt:tt + 1], func=AF.Ln, bias=epst[:], scale=1.0 / D),
                         reads=[b_ss, cB], writes=[b_ss])
                    P.op("act", lambda e, tt=tt: e.activation(out=ss[:, 8 + tt:9 + tt], in_=ss[:, 4 + tt:5 + tt], func=AF.Exp, scale=-0.5),
                         reads=[b_ss], writes=[b_ss])
                    P.op("dve", lambda e, tt=tt: e.scalar_tensor_tensor(out=hb[tt][:], in0=hb[tt][:], scalar=ss[:, 8 + tt:9 + tt], in1=grep_[:], op0=ALU.mult, op1=ALU.mult),
                         reads=[b_hb[tt], b_ss, b_grep], writes=[b_hb[tt]])
                P.dma("sp", hout[t0 + tt * 128: t0 + (tt + 1) * 128, :], hb[tt][:], reads=[b_hb[tt]])
        if ctx is None:
            P.finish()
        else:
            P.barrier()
    return nc


def build_fused():
    nc = bass.Bass("TRN2", target_bir_lowering=False)
    x = nc.dram_tensor("x", [S, D], F32, kind="ExternalInput").ap()
    out = nc.dram_tensor("out", [S, D], F32, kind="ExternalOutput").ap()
    mix0_s = nc.dram_tensor("mix0_s", [S, D], BF16, kind="Internal").ap()
    h1_s = nc.dram_tensor("h1_s", [S, D], F32, kind="Internal").ap()
    mix1_s = nc.dram_tensor("mix1_s", [S, D], BF16, kind="Internal").ap()
    with ExitStack() as st:
        P = Prog(nc, st)
        first = True
        for g in range(4):
            if not first:
                P.new_epoch()
            first = False
            build_nsa(8, ctx={"nc": nc, "P": P, "pfx": "a%d_" % g}, io={"x": x, "mix": mix0_s[:, g * 1024:(g + 1) * 1024]})
        P.new_epoch()
        build_outproj(False, NT=S, ctx={"nc": nc, "P": P, "pfx": "b_"}, io={"mixin": mix0_s, "res": x, "hout": h1_s})
        for j in range(4):
            P.new_epoch()
            build_moba(8, ctx={"nc": nc, "P": P, "pfx": "c%d_" % j}, io={"x": h1_s, "mix": mix1_s[:, j * 1024:(j + 1) * 1024]})
        P.new_epoch()
        build_outproj(True, NT=S, ctx={"nc": nc, "P": P, "pfx": "d_"}, io={"mixin": mix1_s, "res": h1_s, "hout": out})
        P.finish()
        print("fused ninst", P.ninst, "nwaits", P.nwaits, flush=True)
    return nc


def fused_inputs(z, b):
    m = {"x": np.ascontiguousarray(z["x"][b])}
    idn = np.eye(128, dtype=np.float32).astype(NPBF)
    for g in range(4):
        for k, v in nsa_inputs(z, b, g).items():
            if k != "x":
                m["a%d_%s" % (g, k)] = v
    m["b_w"] = w_layout(z["a_w_out"][0]); m["b_idn"] = idn
    for j in range(4):
        for k, v in moba_inputs(z, None, j).items():
            if k != "x":
                m["c%d_%s" % (j, k)] = v
    m["d_w"] = w_layout(z["b_w_out"][0]); m["d_idn"] = idn
    m["d_gfin"] = np.ascontiguousarray(np.broadcast_to(z["final_norm_g"].reshape(1, D), (128, D))).astype(np.float32)
    return m


def kernel(**z):
    z = {k: np.asarray(v) for k, v in z.items()}
    nc = build_fused()
    m0 = fused_inputs(z, 0)
    m1 = dict(m0); m1["x"] = np.ascontiguousarray(z["x"][1])
    res = run_bass_kernel_spmd(nc, [m0, m1], core_ids=[0, 1])
    out = np.stack([np.asarray(r["out"]) for r in res.results])
    return out.astype(np.float32)
```

```python
import os, sys, time
import numpy as np
import concourse.bass as bass
import concourse.mybir as mybir
from concourse.bass_utils import run_bass_kernel_spmd
from contextlib import ExitStack
import ml_dtypes

F32 = mybir.dt.float32
BF16 = mybir.dt.bfloat16
AF = mybir.ActivationFunctionType
ALU = mybir.AluOpType
AX = mybir.AxisListType
NPBF = ml_dtypes.bfloat16


class Buf:
    __slots__ = ("name", "w", "r", "excl")

    def __init__(self, name="", excl=False):
        self.name = name
        self.excl = excl
        self.w = {}
        self.r = {}


class Prog:
    NDMA = 16

    def __init__(self, nc, stack):
        self.nc = nc
        self.E = {"pe": nc.tensor, "act": nc.scalar, "dve": nc.vector, "pool": nc.gpsimd, "sp": nc.sync}
        self.sems = {}
        self.stack = stack
        self.epoch = 0
        self.cur = {}
        for k in self.E:
            self.cur[k] = k + "_0"
            self.sems[k + "_0"] = stack.enter_context(nc.semaphore("sem_" + k + "_0"))
        for i in range(self.NDMA):
            self.sems["d%d" % i] = stack.enter_context(nc.semaphore("sem_d%d" % i))
        self.cnt = {k: 0 for k in self.sems}
        self.waited = {k: {} for k in self.E}
        self.dma_rr = 0
        self.nwaits = 0
        self.ninst = 0

    def new_epoch(self):
        self.epoch += 1
        for k in self.E:
            sk = "%s_%d" % (k, self.epoch)
            self.cur[k] = sk
            self.sems[sk] = self.stack.enter_context(self.nc.semaphore("sem_" + sk))
            self.cnt[sk] = 0

    def _wait(self, eng, sk, v):
        w = self.waited[eng]
        if w.get(sk, 0) >= v:
            return
        if sk == self.cur[eng] and v <= self.cnt[sk] - 64:
            return
        w[sk] = v
        self.E[eng].wait_ge(self.sems[sk], v)
        self.nwaits += 1

    def _deps(self, eng, reads, writes):
        toks = {}
        for b in reads:
            for sk, v in b.w.items():
                if eng == "pe" and sk.startswith("pe_"):
                    continue
                if toks.get(sk, 0) < v:
                    toks[sk] = v
            if b.excl:
                for sk, v in b.r.items():
                    if sk.startswith(eng + "_"):
                        continue
                    if toks.get(sk, 0) < v:
                        toks[sk] = v
        for b in writes:
            for d in (b.w, b.r):
                for sk, v in d.items():
                    if sk.startswith(eng + "_"):
                        continue
                    if toks.get(sk, 0) < v:
                        toks[sk] = v
        for sk, v in toks.items():
            self._wait(eng, sk, v)

    def _commit(self, sk, v, reads, writes):
        for b in reads:
            if b.r.get(sk, 0) < v:
                b.r[sk] = v
        for b in writes:
            if b.w.get(sk, 0) < v:
                b.w[sk] = v

    def op(self, eng, fn, reads=(), writes=()):
        self._deps(eng, reads, writes)
        ins = fn(self.E[eng])
        sk = self.cur[eng]
        self.cnt[sk] += 1
        ins.then_inc(self.sems[sk], 1)
        self.ninst += 1
        self._commit(sk, self.cnt[sk], reads, writes)

    def dma(self, q, out, in_, reads=(), writes=(), **kw):
        sk = "d%d" % self.dma_rr
        self.dma_rr = (self.dma_rr + 1) % self.NDMA
        if self.cnt[sk] > 0:
            self._wait(q, sk, self.cnt[sk])
        self._deps(q, reads, writes)
        ins = self.E[q].dma_start(out=out, in_=in_, **kw)
        self.cnt[sk] += 16
        ins.then_inc(self.sems[sk], 16)
        self.ninst += 1
        self._commit(sk, self.cnt[sk], reads, writes)

    def finish(self):
        for sk in self.sems:
            if sk.startswith("d") and self.cnt[sk] > 0:
                self._wait("sp", sk, self.cnt[sk])


def w_layout(w):
    return np.ascontiguousarray(w.reshape(32, 128, -1).transpose(1, 0, 2))


def bf16_split3(v):
    v = v.astype(np.float32)
    r0 = v.astype(NPBF)
    e1 = v - r0.astype(np.float32)
    r1 = e1.astype(NPBF)
    e2 = e1 - r1.astype(np.float32)
    r2 = e2.astype(NPBF)
    return np.stack([r0, r1, r2])


S = 4096
D = 4096
DH = 128
TB = 512
EPS = 1e-6
NEGM = -30000.0
QSCALE = DH ** -0.5
NQK = 1536
NVG = 280
NZ = 3072
NCOL = NQK + NVG + NZ


def nsa_tables(g):
    hl = np.arange(8)
    m = (2.0 ** (-8.0 * (8 * g + hl + 1) / 32)).astype(np.float32)
    p = np.arange(128)
    t = {}
    dl = np.arange(32) - 28
    kb = m[None, :, None] * (128.0 * dl[None, None, :] + p[:, None, None])
    t["kb"] = kb.astype(np.float32)
    ct = np.arange(2); qt = np.arange(8)
    kbc = m[None, :, None, None] * (16.0 * (128 * ct[None, None, :, None] + p[:, None, None, None]) + 31 - 512.0 * qt[None, None, None, :])
    t["kbc"] = kbc.astype(np.float32).reshape(128, 8, 16)
    c = np.arange(512, dtype=np.float32)
    v = -(m[:, None] * c[None, :]).astype(np.float32)
    t["rsinit"] = bf16_split3(v)
    ex = np.zeros((67, 32, 128), np.float32)
    for kt in range(32):
        for pp in range(128):
            ex[2 * kt + pp // 64, kt, pp] = 30000.0
    ex[64:67] = 1.0
    t["ex"] = ex.astype(NPBF)
    r = p[:, None]; cc = np.arange(128)[None, :]
    t["tric"] = np.where(r > cc, NEGM, 0.0).astype(NPBF)
    t["triw"] = np.where(r <= cc, NEGM, 0.0).astype(NPBF)
    cm = np.zeros((128, 8, 2, 512), np.float32)
    for q_ in range(8):
        for c_ in range(2):
            n = 128 * c_ + p
            tt = 512 * q_ + np.arange(512)
            valid = (16 * n[:, None] + 31 <= tt[None, :]) & (n[:, None] <= 254)
            cm[:, q_, c_, :] = np.where(valid, 0.0, NEGM)
    t["cmpm"] = cm.astype(NPBF)
    tq = np.arange(S); j = np.arange(64)
    bq = tq // 64
    forced = (j[None, :] == 0) | (j[None, :] == bq[:, None]) | (j[None, :] == bq[:, None] - 1)
    fut = j[None, :] > bq[:, None]
    t["bimp"] = np.where(forced, 1e9, np.where(fut, -1e30, 0.0)).astype(np.float32)
    n = np.arange(256)
    ov = ((16 * n[:, None] <= 64 * j[None, :] + 63) & (16 * n[:, None] + 31 >= 64 * j[None, :]) & (n[:, None] <= 254))
    t["ov"] = np.ascontiguousarray(ov.reshape(2, 128, 64).transpose(1, 0, 2)).astype(NPBF)
    t["idn"] = np.eye(128, dtype=np.float32).astype(NPBF)
    dd = np.arange(1, 512, dtype=np.float64)
    e = np.exp(-m.astype(np.float64)[:, None] * dd[None, :])
    csum = np.concatenate([np.cumsum(e[:, ::-1], axis=1)[:, ::-1], np.zeros((8, 1))], axis=1)
    tt = np.arange(512)
    wx = csum[:, tt]
    t["winx"] = np.ascontiguousarray(wx.T.reshape(4, 128, 8).transpose(1, 0, 2)).astype(np.float32)
    return t


def nsa_weight_cols(g):
    H, G = 32, 4
    q_end = H * DH
    kv_end = q_end + 3 * 2 * G * DH
    z_end = kv_end + 3 * H * DH
    cols = []
    d = np.arange(DH)
    for hl in range(8):
        cols.append((8 * g + hl) * DH + d)
    kvcol = lambda br, kvi: q_end + ((br * 2 + kvi) * G + g) * DH + d
    cols += [kvcol(0, 0), kvcol(1, 0), kvcol(2, 0), kvcol(0, 1)]
    cols += [kvcol(1, 1), kvcol(2, 1)]
    cols.append(np.array([z_end + br * H + 8 * g + hl for br in range(3) for hl in range(8)]))
    for br in range(3):
        for hl in range(8):
            cols.append(kv_end + (br * H + 8 * g + hl) * DH + d)
    cols = np.concatenate(cols)
    assert cols.shape[0] == NCOL
    return cols


def build_nsa(NTB=8, stage=9, dbg=False):
    nc = bass.Bass("TRN2", target_bir_lowering=False)
    dram = lambda name, shape, dt, kind="ExternalInput": nc.dram_tensor(name, shape, dt, kind=kind).ap()
    x = dram("x", [S, D], F32)
    gcol_d = dram("gcol", [128, 32], F32)
    w = dram("w", [128, 32, NCOL], F32)
    w1k_d = dram("w1k", [128, 32, 128], F32); w1v_d = dram("w1v", [128, 32, 128], F32)
    w2k_d = dram("w2k", [128, 128], F32); w2v_d = dram("w2v", [128, 128], F32)
    posk_d = dram("posk", [128, 32], F32); posv_d = dram("posv", [128, 32], F32)
    kb_d = dram("kb", [128, 8, 32], F32); kbc_d = dram("kbc", [128, 8, 16], F32)
    rsinit_d = dram("rsinit", [3, 8, 512], BF16)
    ex_d = dram("ex", [67, 32, 128], BF16)
    tric_d = dram("tric", [128, 128], BF16); triw_d = dram("triw", [128, 128], BF16)
    cmpm_d = dram("cmpm", [128, 8, 2, 512], BF16)
    bimp_d = dram("bimp", [S, 64], F32)
    ov_d = dram("ov", [128, 2, 64], BF16)
    idn_d = dram("idn", [128, 128], BF16)
    winx_d = dram("winx", [128, 4, 8], F32)
    mix_d = dram("mix", [S, 1024], BF16, kind="ExternalOutput")

    with ExitStack() as st:
        P = Prog(nc, st)
        sb = lambda name, shape, dt: st.enter_context(nc.sbuf_tensor("s_" + name, shape, dt))
        ps = lambda name, shape, dt: st.enter_context(nc.psum_tensor("p_" + name, shape, dt))
        cB = Buf("const")
        ident = sb("ident", [128, 128], BF16)
        gcol = sb("gcol", [128, 32], F32)
        kb = sb("kb", [128, 8, 32], F32); kbc = sb("kbc", [128, 8, 16], F32)
        EX = sb("EX", [67, 32, 128], BF16)
        tric = sb("tric", [128, 128], BF16); triw = sb("triw", [128, 128], BF16)
        OV = sb("OV", [128, 2, 64], BF16)
        w2k = sb("w2k", [128, 128], BF16); w2v = sb("w2v", [128, 128], BF16)
        posk = sb("posk", [128, 32], BF16); posv = sb("posv", [128, 32], BF16)
        RS = sb("RS", [67, 8, 512], BF16); b_RS = Buf("RS")
        winx = sb("winx", [128, 4, 8], F32)
        for (dst, src, q) in [(winx, winx_d, "sp"), (ident, idn_d, "sp"), (gcol, gcol_d, "sp"), (kb, kb_d, "sp"), (kbc, kbc_d, "sp"),
                              (EX, ex_d, "sp"), (tric, tric_d, "sp"), (triw, triw_d, "sp"), (OV, ov_d, "sp"),
                              (w2k, w2k_d, "pool"), (w2v, w2v_d, "pool"), (posk, posk_d, "pool"), (posv, posv_d, "pool")]:
            bb = Buf()
            P.dma(q, dst[:], src, writes=[bb])
            for sk, v in bb.w.items():
                cB.w[sk] = max(cB.w.get(sk, 0), v)
        P.dma("sp", RS[64:67, :, :], rsinit_d, writes=[b_RS])
        epst = sb("epst", [128, 1], F32)
        P.op("dve", lambda e: e.memset(epst[:], EPS), writes=[cB])
        kT = sb("kT", [128, 2, S], BF16); b_kT = Buf("kT")
        Vaug = sb("Vaug", [128, 2, 32, 129], BF16); b_V = Buf("V")
        rawT = sb("rawT", [128, 2, 528], BF16); b_raw = Buf("raw")
        kcT = sb("kcT", [128, 256], BF16); b_kc = Buf("kc")
        vcaug = sb("vcaug", [128, 2, 129], BF16); b_vc = Buf("vc")
        cbias = sb("cbias", [128, 2], F32); b_cbias = Buf("cbias")
        P.op("dve", lambda e: e.memset(Vaug[:], 1.0), writes=[b_V])
        P.op("dve", lambda e: e.memset(rawT[:], 0.0), writes=[b_raw])
        P.op("dve", lambda e: e.memset(kcT[:], 0.0), writes=[b_kc])
        P.op("dve", lambda e: e.memset(vcaug[:], 0.0), writes=[b_vc])
        P.op("dve", lambda e: e.memset(vcaug[:, :, 128:129], 1.0), writes=[b_vc])
        xf = sb("xf", [128, D], F32); b_xf = Buf("xf")
        xh = sb("xh", [128, D], BF16); b_xh = Buf("xh")
        junk = xh
        ss = sb("ss", [128, 4], F32); b_ss = Buf("ss")
        xnT = sb("xnT", [128, 32, TB], BF16); b_xnT = [Buf("xnT%d" % i) for i in range(4)]
        NW = 4
        wt = [sb("wt%d" % i, [128, 8, 512], BF16) for i in range(NW)]; b_wt = [Buf("wt%d" % i) for i in range(NW)]
        qT = sb("qT", [128, 8, TB], BF16); b_qT = Buf("qT")
        zs = sb("zs", [128, 4, NZ], BF16); b_zs = Buf("zs")
        gates = sb("gates", [128, 4, 24], F32); b_gates = Buf("gates")
        mix = sb("mix", [128, 4, 1024], F32); b_mix = Buf("mix")
        NPT = 4
        PT = [sb("PT%d" % i, [128, TB], BF16) for i in range(NPT)]; b_PT = [Buf("PT%d" % i) for i in range(NPT)]
        cmpm = sb("cmpm", [128, 2, 512], BF16); b_cmpm = Buf("cmpm")
        bimp = sb("bimp", [128, 4, 64], F32); b_bimp = Buf("bimp")
        impacc = sb("impacc", [128, 4, 64], F32); b_imp = Buf("imp")
        impw = sb("impw", [128, 4, 64], F32); b_impw = Buf("impw")
        top = sb("top", [128, 4, 16], F32); b_top = Buf("top")
        selm = sb("selm", [128, 4, 64], BF16); b_selm = Buf("selm")
        rsb = sb("rsb", [128, 8], F32); b_rsb = Buf("rsb")
        otmp = sb("otmp", [128, 4, 128], F32); b_otmp = Buf("otmp")
        hid = sb("hid", [128, 2, 32], BF16); b_hid = Buf("hid")
        vctmp = sb("vctmp", [32, 128], BF16); b_vctmp = Buf("vctmp")
        pA = [ps("pA%d" % i, [128, 512], F32) for i in range(4)]; b_pA = [Buf("pA%d" % i, True) for i in range(4)]
        pO = [ps("pO%d" % i, [128, 2, 256], F32) for i in range(2)]; b_pO = [Buf("pO%d" % i, True) for i in range(2)]
        pT = [ps("pT%d" % i, [128, 8, 128], BF16) for i in range(2)]; b_pT = [Buf("pT%d" % i, True) for i in range(2)]
        print("sbuf remaining", nc.sbuf_bytes_remaining, flush=True)

        state = {"wi": 0, "pti": 0, "pai": 0, "ptr": 0, "alt": 0}

        def evac_engine():
            state["alt"] ^= 1
            return "dve" if state["alt"] else "act"

        def front_end(tb):
            t0 = tb * TB
            for tt in range(4):
                P.dma("sp", xf[:], x[t0 + tt * 128: t0 + (tt + 1) * 128, :], writes=[b_xf])
                P.op("dve", lambda e: e.memset(ss[:, 0:1], 0.0), writes=[b_ss])
                P.op("act", lambda e, tt=tt: e.activation(out=junk[:], in_=xf[:], func=AF.Square, accum_out=ss[:, 0:1]),
                     reads=[b_xf], writes=[b_xh, b_ss])
                P.op("act", lambda e: e.activation(out=ss[:, 1:2], in_=ss[:, 0:1], func=AF.Ln, bias=epst[:], scale=1.0 / D),
                     reads=[b_ss, cB], writes=[b_ss])
                P.op("act", lambda e: e.activation(out=ss[:, 2:3], in_=ss[:, 1:2], func=AF.Exp, scale=-0.5),
                     reads=[b_ss], writes=[b_ss])
                P.op("dve", lambda e: e.tensor_scalar(out=xh[:], in0=xf[:], scalar1=ss[:, 2:3], scalar2=None, op0=ALU.mult),
                     reads=[b_xf, b_ss], writes=[b_xh])
                for k8 in range(4):
                    pt = pT[state["ptr"] % 2]; bpt = b_pT[state["ptr"] % 2]; state["ptr"] += 1
                    for j in range(8):
                        kc = k8 * 8 + j
                        P.op("pe", lambda e, pt=pt, j=j, kc=kc: e.transpose(out=pt[:, j, :], in_=xh[:, kc * 128:(kc + 1) * 128], identity=ident[:]),
                             reads=[b_xh, cB], writes=[bpt])
                    P.op("dve", lambda e, pt=pt, k8=k8, tt=tt: e.tensor_tensor(
                        out=xnT[:, k8 * 8:(k8 + 1) * 8, tt * 128:(tt + 1) * 128], in0=pt[:],
                        in1=gcol[:, k8 * 8:(k8 + 1) * 8].unsqueeze(2).to_broadcast([128, 8, 128]), op=ALU.mult),
                        reads=[bpt, cB], writes=[b_xnT[k8]])

        def load_w(col0, ncols, kq):
            i = state["wi"] % NW; state["wi"] += 1
            P.dma("pool", wt[i][:, :, 0:ncols], w[:, kq * 8:(kq + 1) * 8, col0:col0 + ncols], writes=[b_wt[i]])
            return wt[i], b_wt[i]

        def projections(tb):
            t0 = tb * TB
            for cg in range(3):
                for kq in range(4):
                    ws, bws = load_w(cg * 512, 512, kq)
                    order = range(4)
                    for mt in order:
                        for kci in range(8):
                            kc = kq * 8 + kci
                            P.op("pe", lambda e, mt=mt, kci=kci, kc=kc, ws=ws: e.matmul(
                                pA[mt][:], lhsT=ws[:, kci, mt * 128:(mt + 1) * 128], rhs=xnT[:, kc, :], start=(kc == 0), stop=(kc == 31)),
                                reads=[bws, b_xnT[kc // 8]], writes=[b_pA[mt]])
                for mt in range(4):
                    M = cg * 4 + mt
                    if M < 8:
                        dst, bd, scale = qT[:, M, :], b_qT, QSCALE
                    elif M == 8:
                        dst, bd, scale = rawT[:, 0, 16:528], b_raw, 1.0
                    elif M == 9:
                        dst, bd, scale = kT[:, 0, t0:t0 + TB], b_kT, 1.0
                    elif M == 10:
                        dst, bd, scale = kT[:, 1, t0:t0 + TB], b_kT, 1.0
                    else:
                        dst, bd, scale = rawT[:, 1, 16:528], b_raw, 1.0
                    if evac_engine() == "act":
                        P.op("act", lambda e, dst=dst, mt=mt, scale=scale: e.activation(out=dst, in_=pA[mt][:], func=AF.Copy, scale=scale),
                             reads=[b_pA[mt]], writes=[bd])
                    else:
                        P.op("dve", lambda e, dst=dst, mt=mt, scale=scale: e.tensor_scalar(out=dst, in0=pA[mt][:], scalar1=scale, scalar2=None, op0=ALU.mult),
                             reads=[b_pA[mt]], writes=[bd])
            for kq in range(4):
                ws, bws = load_w(NQK, NVG, kq)
                for tt in range(4):
                    for kci in range(8):
                        kc = kq * 8 + kci
                        P.op("pe", lambda e, tt=tt, kci=kci, kc=kc, ws=ws: e.matmul(
                            pA[tt][:, 0:NVG], lhsT=xnT[:, kc, tt * 128:(tt + 1) * 128], rhs=ws[:, kci, 0:NVG], start=(kc == 0), stop=(kc == 31)),
                            reads=[bws, b_xnT[kc // 8]], writes=[b_pA[tt]])
            for tt in range(4):
                kt = 4 * tb + tt
                P.op("dve", lambda e, tt=tt, kt=kt: e.tensor_copy(out=Vaug[:, :, kt, 0:128], in_=pA[tt][:, 0:256].rearrange("p (b d) -> p b d", b=2)),
                     reads=[b_pA[tt]], writes=[b_V])
                P.op("act", lambda e, tt=tt: e.activation(out=gates[:, tt, :], in_=pA[tt][:, 256:280], func=AF.Sigmoid),
                     reads=[b_pA[tt]], writes=[b_gates])
            for cg in range(6):
                for kq in range(4):
                    ws, bws = load_w(NQK + NVG + cg * 512, 512, kq)
                    for tt in range(4):
                        for kci in range(8):
                            kc = kq * 8 + kci
                            P.op("pe", lambda e, tt=tt, kci=kci, kc=kc, ws=ws: e.matmul(
                                pA[tt][:], lhsT=xnT[:, kc, tt * 128:(tt + 1) * 128], rhs=ws[:, kci, :], start=(kc == 0), stop=(kc == 31)),
                                reads=[bws, b_xnT[kc // 8]], writes=[b_pA[tt]])
                for tt in range(4):
                    P.op("act", lambda e, tt=tt, cg=cg: e.activation(out=zs[:, tt, cg * 512:(cg + 1) * 512], in_=pA[tt][:], func=AF.Silu),
                         reads=[b_pA[tt]], writes=[b_zs])

        def compress(tb):
            t0 = tb * TB
            n_lo = max(0, 32 * tb - 1)
            n_hi = 32 * tb + 30
            cnt = n_hi - n_lo + 1
            col0 = 16 * n_lo - (t0 - 16)
            i = state["wi"] % NW; state["wi"] += 1
            w1 = wt[i]; bw1 = b_wt[i]
            w1v_ = w1[:].rearrange("p a c -> p (a c)").rearrange("p (l e) -> p l e", e=128)
            i2 = state["wi"] % NW; state["wi"] += 1
            w1b = wt[i2]; bw1b = b_wt[i2]
            w1bv_ = w1b[:].rearrange("p a c -> p (a c)").rearrange("p (l e) -> p l e", e=128)
            P.dma("pool", w1v_, w1k_d, writes=[bw1])
            P.dma("pool", w1bv_, w1v_d, writes=[bw1b])
            if tb == 0:
                for kv, (wv, bw, pos) in enumerate([(w1v_, bw1, posk), (w1bv_, bw1b, posv)]):
                    for l in range(32):
                        P.op("pe", lambda e, wv=wv, l=l, pos=pos, kv=kv: e.matmul(pA[kv][:, 0:1], lhsT=wv[:, l, :], rhs=pos[:, l:l + 1], start=(l == 0), stop=(l == 31)),
                             reads=[bw, cB], writes=[b_pA[kv]])
                    P.op("dve", lambda e, kv=kv: e.tensor_copy(out=cbias[:, kv:kv + 1], in_=pA[kv][:, 0:1]), reads=[b_pA[kv]], writes=[b_cbias])
            for kv, (wv, bw) in enumerate([(w1v_, bw1), (w1bv_, bw1b)]):
                for l in range(32):
                    P.op("pe", lambda e, wv=wv, l=l, kv=kv: e.matmul(pA[2 + kv][:, 0:cnt], lhsT=wv[:, l, :],
                                                                   rhs=rawT[:, kv, col0 + l: col0 + l + 16 * (cnt - 1) + 1: 16], start=(l == 0), stop=(l == 31)),
                         reads=[bw, b_raw], writes=[b_pA[2 + kv]])
                P.op("act", lambda e, kv=kv: e.activation(out=hid[:, kv, 0:cnt], in_=pA[2 + kv][:, 0:cnt], func=AF.Silu, bias=cbias[:, kv:kv + 1], scale=1.0),
                     reads=[b_pA[2 + kv], b_cbias], writes=[b_hid])
            P.op("pe", lambda e: e.matmul(pA[0][:, 0:cnt], lhsT=w2k[:], rhs=hid[:, 0, 0:cnt], start=True, stop=True),
                 reads=[b_hid, cB], writes=[b_pA[0]])
            P.op("dve", lambda e: e.tensor_copy(out=kcT[:, n_lo:n_lo + cnt], in_=pA[0][:, 0:cnt]), reads=[b_pA[0]], writes=[b_kc])
            P.op("pe", lambda e: e.matmul(pA[1][0:cnt, 0:128], lhsT=hid[:, 1, 0:cnt], rhs=w2v[:], start=True, stop=True),
                 reads=[b_hid, cB], writes=[b_pA[1]])
            P.op("dve", lambda e: e.tensor_copy(out=vctmp[0:cnt, :], in_=pA[1][0:cnt, 0:128]), reads=[b_pA[1]], writes=[b_vctmp])
            a = n_lo
            while a <= n_hi:
                ctt = a // 128
                bnd = min(n_hi, 128 * ctt + 127)
                P.dma("sp", vcaug[a - 128 * ctt: bnd - 128 * ctt + 1, ctt, 0:128], vctmp[a - n_lo: bnd - n_lo + 1, :], reads=[b_vctmp], writes=[b_vc])
                a = bnd + 1
            P.op("dve", lambda e: e.tensor_copy(out=rawT[:, :, 0:16], in_=rawT[:, :, 512:528]), reads=[b_raw], writes=[b_raw])

        def next_pa():
            i = state["pai"] % 4; state["pai"] += 1
            return i

        def next_pt():
            i = state["pti"] % NPT; state["pti"] += 1
            return i

        def combine(tb, hl, br, first):
            for bk in range(2):
                P.op("dve", lambda e, bk=bk: e.tensor_scalar(out=rsb[:, 2 * bk:2 * bk + 2], in0=pO[bk][:, :, 128], scalar1=1e-30, scalar2=None, op0=ALU.max),
                     reads=[b_pO[bk]], writes=[b_rsb])
            if br == 2 and tb == 0:
                P.op("dve", lambda e: e.tensor_tensor(out=rsb[:, 0:4], in0=rsb[:, 0:4], in1=winx[:, :, hl], op=ALU.add), reads=[b_rsb, cB], writes=[b_rsb])
            P.op("dve", lambda e: e.reciprocal(out=rsb[:, 4:8], in_=rsb[:, 0:4]), reads=[b_rsb], writes=[b_rsb])
            return

        def combine2(tb, hl, br):
            gi = br * 8 + hl
            P.op("dve", lambda e: e.tensor_tensor(out=rsb[:, 0:4], in0=rsb[:, 4:8], in1=gates[:, :, gi], op=ALU.mult),
                 reads=[b_rsb, b_gates], writes=[b_rsb])
            for i in range(4):
                bk, j = i // 2, i % 2
                zsl = zs[:, i, gi * 128:(gi + 1) * 128]
                if br == 0:
                    P.op("dve", lambda e, i=i, bk=bk, j=j, zsl=zsl: e.scalar_tensor_tensor(
                        out=mix[:, i, hl * 128:(hl + 1) * 128], in0=pO[bk][:, j, 0:128], scalar=rsb[:, i:i + 1], in1=zsl, op0=ALU.mult, op1=ALU.mult),
                        reads=[b_pO[bk], b_rsb, b_zs], writes=[b_mix])
                else:
                    P.op("dve", lambda e, i=i, bk=bk, j=j, zsl=zsl: e.scalar_tensor_tensor(
                        out=otmp[:, i, :], in0=pO[bk][:, j, 0:128], scalar=rsb[:, i:i + 1], in1=zsl, op0=ALU.mult, op1=ALU.mult),
                        reads=[b_pO[bk], b_rsb, b_zs], writes=[b_otmp])
                    P.op("dve", lambda e, i=i: e.tensor_tensor(out=mix[:, i, hl * 128:(hl + 1) * 128], in0=mix[:, i, hl * 128:(hl + 1) * 128], in1=otmp[:, i, :], op=ALU.add),
                         reads=[b_otmp, b_mix], writes=[b_mix])

        def pv(pt_i, a, b_, vrhs, bv, first_flags, last_sub):
            for i in range(a // 128, b_ // 128):
                bk, j = i // 2, i % 2
                st_flag = first_flags[bk]
                first_flags[bk] = False
                P.op("pe", lambda e, i=i, bk=bk, j=j, st_flag=st_flag: e.matmul(
                    pO[bk][:, j, 0:129], lhsT=PT[pt_i][:, i * 128:(i + 1) * 128], rhs=vrhs, start=st_flag, stop=False, skip_group_check=True),
                    reads=[b_PT[pt_i], bv], writes=[b_pO[bk]])

        def score_tile(hl, klhs, bk_, a, b_, bias_ap, ex_lhs, ex_rhs_fn, masks, extra_reads):
            pa = next_pa()
            n_extra = 1 + len(masks)
            P.op("pe", lambda e: e.matmul(pA[pa][:, a:b_], lhsT=klhs, rhs=qT[:, hl, a:b_], start=True, stop=False),
                 reads=[bk_, b_qT], writes=[b_pA[pa]])
            P.op("pe", lambda e: e.matmul(pA[pa][:, a:b_], lhsT=ex_lhs, rhs=ex_rhs_fn(a, b_), start=False, stop=(len(masks) == 0)),
                 reads=[cB, b_RS], writes=[b_pA[pa]])
            for mi, (c0, c1, tab, btab) in enumerate(masks):
                P.op("pe", lambda e, c0=c0, c1=c1, tab=tab, mi=mi: e.matmul(pA[pa][:, c0:c1], lhsT=ident[:], rhs=tab, start=False, stop=(mi == len(masks) - 1)),
                     reads=[cB, btab], writes=[b_pA[pa]])
            pti = next_pt()
            P.op("act", lambda e: e.activation(out=PT[pti][:, a:b_], in_=pA[pa][:, a:b_], func=AF.Exp, bias=bias_ap, scale=1.0),
                 reads=[b_pA[pa], cB], writes=[b_PT[pti]])
            return pti

        def attn_cmp(tb):
            qt = tb
            P.dma("sp", cmpm[:], cmpm_d[:, qt, :, :], writes=[b_cmpm])
            cts = [0] if qt < 4 else [0, 1]
            for hl in range(8):
                ff = [True, True]
                ptis = []
                for ct in cts:
                    need_mask = not (ct == 0 and qt >= 5)
                    masks = [(0, 512, cmpm[:, ct, :], b_cmpm)] if need_mask else []
                    pti = score_tile(hl, kcT[:, ct * 128:(ct + 1) * 128], b_kc, 0, 512,
                                     kbc[:, hl, ct * 8 + qt: ct * 8 + qt + 1],
                                     EX[64:67, 0, :], lambda a, b_, hl=hl: RS[64:67, hl, a:b_], masks, [])
                    ptis.append((ct, pti))
                    pv(pti, 0, 512, vcaug[:, ct, :], b_vc, ff, None)
                pa = next_pa()
                for i in range(4):
                    for k, (ct, pti) in enumerate(ptis):
                        P.op("pe", lambda e, i=i, ct=ct, pti=pti, k=k: e.matmul(
                            pA[pa][:, i * 64:(i + 1) * 64], lhsT=PT[pti][:, i * 128:(i + 1) * 128], rhs=OV[:, ct, :], start=(k == 0), stop=(k == len(ptis) - 1)),
                            reads=[b_PT[pti], cB], writes=[b_pA[pa]])
                combine(tb, hl, 0, None)
                for i in range(4):
                    if hl == 0:
                        P.op("dve", lambda e, i=i: e.tensor_scalar(out=impacc[:, i, :], in0=pA[pa][:, i * 64:(i + 1) * 64], scalar1=rsb[:, 4 + i:5 + i], scalar2=None, op0=ALU.mult),
                             reads=[b_pA[pa], b_rsb], writes=[b_imp])
                    else:
                        P.op("dve", lambda e, i=i: e.scalar_tensor_tensor(out=impacc[:, i, :], in0=pA[pa][:, i * 64:(i + 1) * 64], scalar=rsb[:, 4 + i:5 + i], in1=impacc[:, i, :], op0=ALU.mult, op1=ALU.add),
                             reads=[b_pA[pa], b_rsb, b_imp], writes=[b_imp])
                combine2(tb, hl, 0)

        def topk(tb):
            t0 = tb * TB
            P.dma("sp", bimp[:], bimp_d[t0:t0 + TB, :].rearrange("(t p) j -> p t j", p=128), writes=[b_bimp])
            P.op("dve", lambda e: e.tensor_tensor(out=impacc[:], in0=impacc[:], in1=bimp[:], op=ALU.add), reads=[b_imp, b_bimp], writes=[b_imp])
            for i in range(4):
                P.op("dve", lambda e, i=i: e.max(out=top[:, i, 0:8], in_=impacc[:, i, :]), reads=[b_imp], writes=[b_top])
                P.op("dve", lambda e, i=i: e.match_replace(out=impw[:, i, :], in_to_replace=top[:, i, 0:8], in_values=impacc[:, i, :], imm_value=-3.0e38),
                     reads=[b_imp, b_top], writes=[b_impw])
                P.op("dve", lambda e, i=i: e.max(out=top[:, i, 8:16], in_=impw[:, i, :]), reads=[b_impw], writes=[b_top])
                P.op("dve", lambda e, i=i: e.tensor_scalar(out=selm[:, i, :], in0=impacc[:, i, :], scalar1=top[:, i, 15:16], scalar2=1.0, op0=ALU.is_ge, op1=ALU.subtract),
                     reads=[b_imp, b_top], writes=[b_selm])

        def sel_transpose(tb):
            pt = pT[state["ptr"] % 2]; bpt = b_pT[state["ptr"] % 2]; state["ptr"] += 1
            for i in range(4):
                P.op("pe", lambda e, i=i: e.transpose(out=pt[0:64, i, :], in_=selm[:, i, :], identity=ident[:]),
                     reads=[b_selm, cB], writes=[bpt])
            P.op("dve", lambda e: e.tensor_copy(out=RS[0:64, 0, :].rearrange("p (i c) -> p i c", i=4), in_=pt[0:64, 0:4, :]), reads=[bpt], writes=[b_RS])
            for h in range(1, 8):
                P.op("dve", lambda e, h=h: e.tensor_copy(out=RS[0:64, h, :], in_=RS[0:64, 0, :]), reads=[b_RS], writes=[b_RS])

        def attn_dense(tb, br):
            qt = tb
            bi = br - 1
            for hl in range(8):
                ff = [True, True]
                tiles = []
                if br == 2:
                    for u in range(4):
                        kt = 4 * qt - 4 + u
                        if kt >= 0:
                            tiles.append((kt, 0, 128 * (u + 1), [(128 * u, 128 * u + 128, triw[:], cB)]))
                else:
                    for kt in range(4 * qt):
                        tiles.append((kt, 0, 512, []))
                for u in range(4):
                    tiles.append((4 * qt + u, 128 * u, 512, [(128 * u, 128 * u + 128, tric[:], cB)]))
                pend = []
                LOOK = 2
                for (kt, a, b_, masks) in tiles:
                    dl = kt - 4 * qt + 28
                    if br == 1:
                        ex_lhs = EX[0:67, kt, :]
                        ex_rhs = lambda a, b_, hl=hl: RS[0:67, hl, a:b_]
                    else:
                        ex_lhs = EX[64:67, 0, :]
                        ex_rhs = lambda a, b_, hl=hl: RS[64:67, hl, a:b_]
                    pti = score_tile(hl, kT[:, bi, kt * 128:(kt + 1) * 128], b_kT, a, b_, kb[:, hl, dl:dl + 1], ex_lhs, ex_rhs, masks, [])
                    pend.append((pti, a, b_, Vaug[:, bi, kt, :]))
                    if len(pend) > LOOK:
                        p_ = pend.pop(0)
                        pv(p_[0], p_[1], p_[2], p_[3], b_V, ff, None)
                for p_ in pend:
                    pv(p_[0], p_[1], p_[2], p_[3], b_V, ff, None)
                combine(tb, hl, br, None)
                combine2(tb, hl, br)

        def store_mix(tb):
            t0 = tb * TB
            for tt in range(4):
                P.dma("pool", mix_d[t0 + tt * 128:t0 + (tt + 1) * 128, :], mix[:, tt, :], reads=[b_mix])

        for tb in range(NTB):
            front_end(tb)
            if stage >= 1: projections(tb)
            if stage >= 2: compress(tb)
            if stage >= 3: attn_cmp(tb)
            if stage >= 4: topk(tb)
            if stage >= 5: attn_dense(tb, 2)
            if stage >= 6: sel_transpose(tb)
            if stage >= 7: attn_dense(tb, 1)
            if stage >= 3: store_mix(tb)
        if dbg:
            def dump(name, t, bufs, q="sp"):
                shp = list(t.shape)
                d_ = dram("dbg_" + name, shp, t.dtype, kind="ExternalOutput")
                P.dma(q, d_, t[:], reads=bufs)
            dump("xnT", xnT, b_xnT); dump("qT", qT, [b_qT]); dump("kT", kT, [b_kT]); dump("raw", rawT, [b_raw])
            dump("zs", zs, [b_zs]); dump("gates", gates, [b_gates]); dump("V", Vaug, [b_V])
            dump("kc", kcT, [b_kc]); dump("vc", vcaug, [b_vc]); dump("mixf", mix, [b_mix])
            dump("imp", impacc, [b_imp]); dump("selm", selm, [b_selm]); dump("RS", RS, [b_RS]); dump("top", top, [b_top])
        P.finish()
        print("ninst", P.ninst, "nwaits", P.nwaits, flush=True)
    return nc


def nsa_inputs(z, b, g, tabs=None):
    cols = nsa_weight_cols(g)
    wl = w_layout(np.ascontiguousarray(z["a_w_in"][0][:, cols]))
    m = {"x": np.ascontiguousarray(z["x"][b]), "gcol": np.ascontiguousarray(z["a_norm_g"][0].reshape(32, 128).T), "w": wl}
    m["w1k"] = np.ascontiguousarray(z["a_cmp_w1_k"][0].transpose(1, 0, 2)); m["w1v"] = np.ascontiguousarray(z["a_cmp_w1_v"][0].transpose(1, 0, 2))
    m["w2k"] = z["a_cmp_w2_k"][0]; m["w2v"] = z["a_cmp_w2_v"][0]
    m["posk"] = np.ascontiguousarray(z["a_cmp_pos_k"][0].T); m["posv"] = np.ascontiguousarray(z["a_cmp_pos_v"][0].T)
    m.update(nsa_tables(g))
    return m


MQK = 1280
MV = 256
MZ = 1024
MCOL = MQK + MV + MZ


def moba_tables(j):
    hl = np.arange(8)
    m = (2.0 ** (-8.0 * (8 * j + hl + 1) / 32)).astype(np.float32)
    p = np.arange(128)
    t = {}
    dl = np.arange(32) - 28
    t["kb"] = (m[None, :, None] * (128.0 * dl[None, None, :] + p[:, None, None])).astype(np.float32)
    c = np.arange(512, dtype=np.float32)
    v = -(m[:, None] * c[None, :]).astype(np.float32)
    t["rsinit"] = bf16_split3(v)
    ex = np.zeros((67, 32, 128), np.float32)
    for kt in range(32):
        ex[kt // 2, kt, :] = 30000.0
    ex[64:67] = 1.0
    t["ex"] = ex.astype(NPBF)
    r = p[:, None]; cc = np.arange(128)[None, :]
    t["tric"] = np.where(r > cc, NEGM, 0.0).astype(NPBF)
    t["idn"] = np.eye(128, dtype=np.float32).astype(NPBF)
    mt = np.zeros((8, 4, 16), np.float32)
    for tb in range(8):
        for tt in range(4):
            bt = (4 * tb + tt) // 2
            mt[tb, tt, bt:] = -1e30
    t["mtab"] = np.ascontiguousarray(np.broadcast_to(mt.reshape(1, 8, 64), (128, 8, 64))).astype(np.float32)
    return t


def moba_weight(z, j):
    H = 32
    d = np.arange(DH)
    wq = [z["b_w_in"][0][:, (8 * j + hl) * DH + d] for hl in range(8)]
    wk = [z["kv_w"][:, (0 * 8 + 2 * j + gi) * DH + d] for gi in range(2)]
    wv = [z["kv_w"][:, (1 * 8 + 2 * j + gi) * DH + d] for gi in range(2)]
    wz = [z["b_w_in"][0][:, H * DH + (8 * j + hl) * DH + d] for hl in range(8)]
    w = np.concatenate(wq + wk + wv + wz, axis=1)
    assert w.shape[1] == MCOL
    return w_layout(w)


def build_moba(NTB=8, dbg=False):
    nc = bass.Bass("TRN2", target_bir_lowering=False)
    dram = lambda name, shape, dt, kind="ExternalInput": nc.dram_tensor(name, shape, dt, kind=kind).ap()
    x = dram("x", [S, D], F32)
    gcol_d = dram("gcol", [128, 32], F32)
    gkv_d = dram("gkv", [128, 32], F32)
    w = dram("w", [128, 32, MCOL], F32)
    kb_d = dram("kb", [128, 8, 32], F32)
    rsinit_d = dram("rsinit", [3, 8, 512], BF16)
    ex_d = dram("ex", [67, 32, 128], BF16)
    tric_d = dram("tric", [128, 128], BF16)
    idn_d = dram("idn", [128, 128], BF16)
    mtab_d = dram("mtab", [128, 8, 64], F32)
    mix_d = dram("mix", [S, 1024], BF16, kind="ExternalOutput")

    with ExitStack() as st:
        P = Prog(nc, st)
        sb = lambda name, shape, dt: st.enter_context(nc.sbuf_tensor("s_" + name, shape, dt))
        ps = lambda name, shape, dt: st.enter_context(nc.psum_tensor("p_" + name, shape, dt))
        cB = Buf("const")
        ident = sb("ident", [128, 128], BF16)
        gcol = sb("gcol", [128, 32], F32); gkv = sb("gkv", [128, 32], F32); rcol = sb("rcol", [128, 32], F32)
        kb = sb("kb", [128, 8, 32], F32)
        EX = sb("EX", [67, 32, 128], BF16)
        tric = sb("tric", [128, 128], BF16)
        RS = sb("RS", [67, 8, 512], BF16); b_RS = Buf("RS")
        P.op("dve", lambda e: e.memset(RS[0:64, :, :], 0.0), writes=[b_RS])
        for (dst, src) in [(ident, idn_d), (gcol, gcol_d), (gkv, gkv_d), (kb, kb_d), (EX, ex_d), (tric, tric_d)]:
            bb = Buf()
            P.dma("sp", dst[:], src, writes=[bb])
            for sk, v in bb.w.items():
                cB.w[sk] = max(cB.w.get(sk, 0), v)
        P.dma("sp", RS[64:67, :, :], rsinit_d, writes=[b_RS])
        epst = sb("epst", [128, 1], F32)
        P.op("dve", lambda e: e.memset(epst[:], EPS), writes=[cB])
        P.op("dve", lambda e: e.reciprocal(out=rcol[:], in_=gcol[:]), reads=[cB], writes=[cB])
        P.op("dve", lambda e: e.tensor_tensor(out=rcol[:], in0=rcol[:], in1=gkv[:], op=ALU.mult), reads=[cB], writes=[cB])
        kT = sb("kT", [128, 2, S], BF16); b_kT = Buf("kT")
        Vaug = sb("Vaug", [128, 2, 32, 129], BF16); b_V = Buf("V")
        kmean = sb("kmean", [128, 2, 16], BF16); b_km = Buf("km")
        kms = sb("kms", [128, 2, 2], F32); b_kms = Buf("kms")
        P.op("dve", lambda e: e.memset(Vaug[:], 1.0), writes=[b_V])
        P.op("dve", lambda e: e.memset(kmean[:], 0.0), writes=[b_km])
        xf = sb("xf", [128, D], F32); b_xf = Buf("xf")
        xh = sb("xh", [128, D], BF16); b_xh = Buf("xh")
        ss = sb("ss", [128, 4], F32); b_ss = Buf("ss")
        xnT = sb("xnT", [128, 32, TB], BF16); b_xnT = [Buf("xnT%d" % i) for i in range(4)]
        NW = 4
        wt = [sb("wt%d" % i, [128, 8, 512], BF16) for i in range(NW)]; b_wt = [Buf("wt%d" % i) for i in range(NW)]
        qT = sb("qT", [128, 8, TB], BF16); b_qT = Buf("qT")
        zs = sb("zs", [128, 4, MZ], BF16); b_zs = Buf("zs")
        mix = sb("mix", [128, 4, 1024], F32); b_mix = Buf("mix")
        NPT = 4
        PT = [sb("PT%d" % i, [128, TB], BF16) for i in range(NPT)]; b_PT = [Buf("PT%d" % i) for i in range(NPT)]
        mtab = sb("mtab", [128, 64], F32); b_mtab = Buf("mtab")
        sblk = sb("sblk", [128, 8, 64], F32); b_sblk = Buf("sblk")
        top = sb("top", [128, 32, 8], F32); b_top = Buf("top")
        selm = sb("selm", [128, 8, 4, 16], BF16); b_selm = Buf("selm")
        rsb = sb("rsb", [128, 8], F32); b_rsb = Buf("rsb")
        pA = [ps("pA%d" % i, [128, 512], F32) for i in range(4)]; b_pA = [Buf("pA%d" % i, True) for i in range(4)]
        pO = [ps("pO%d" % i, [128, 2, 256], F32) for i in range(2)]; b_pO = [Buf("pO%d" % i, True) for i in range(2)]
        pT = [ps("pT%d" % i, [128, 8, 128], BF16) for i in range(2)]; b_pT = [Buf("pT%d" % i, True) for i in range(2)]
        print("sbuf remaining", nc.sbuf_bytes_remaining, flush=True)
        state = {"wi": 0, "pti": 0, "pai": 0, "ptr": 0, "alt": 0}

        def evac_engine():
            state["alt"] ^= 1
            return "dve" if state["alt"] else "act"

        def front_end(tb):
            t0 = tb * TB
            for tt in range(4):
                P.dma("sp", xf[:], x[t0 + tt * 128: t0 + (tt + 1) * 128, :], writes=[b_xf])
                P.op("dve", lambda e: e.memset(ss[:, 0:1], 0.0), writes=[b_ss])
                P.op("act", lambda e: e.activation(out=xh[:], in_=xf[:], func=AF.Square, accum_out=ss[:, 0:1]), reads=[b_xf], writes=[b_xh, b_ss])
                P.op("act", lambda e: e.activation(out=ss[:, 1:2], in_=ss[:, 0:1], func=AF.Ln, bias=epst[:], scale=1.0 / D), reads=[b_ss, cB], writes=[b_ss])
                P.op("act", lambda e: e.activation(out=ss[:, 2:3], in_=ss[:, 1:2], func=AF.Exp, scale=-0.5), reads=[b_ss], writes=[b_ss])
                P.op("dve", lambda e: e.tensor_scalar(out=xh[:], in0=xf[:], scalar1=ss[:, 2:3], scalar2=None, op0=ALU.mult), reads=[b_xf, b_ss], writes=[b_xh])
                for k8 in range(4):
                    pt = pT[state["ptr"] % 2]; bpt = b_pT[state["ptr"] % 2]; state["ptr"] += 1
                    for j in range(8):
                        kc = k8 * 8 + j
                        P.op("pe", lambda e, pt=pt, j=j, kc=kc: e.transpose(out=pt[:, j, :], in_=xh[:, kc * 128:(kc + 1) * 128], identity=ident[:]),
                             reads=[b_xh, cB], writes=[bpt])
                    P.op("dve", lambda e, pt=pt, k8=k8, tt=tt: e.tensor_tensor(
                        out=xnT[:, k8 * 8:(k8 + 1) * 8, tt * 128:(tt + 1) * 128], in0=pt[:],
                        in1=gcol[:, k8 * 8:(k8 + 1) * 8].unsqueeze(2).to_broadcast([128, 8, 128]), op=ALU.mult),
                        reads=[bpt, cB], writes=[b_xnT[k8]])

        def load_w(col0, ncols, kq, rescale=False):
            i = state["wi"] % NW; state["wi"] += 1
            P.dma("pool", wt[i][:, :, 0:ncols], w[:, kq * 8:(kq + 1) * 8, col0:col0 + ncols], writes=[b_wt[i]])
            if rescale:
                P.op("dve", lambda e: e.tensor_tensor(out=wt[i][:, :, 0:ncols], in0=wt[i][:, :, 0:ncols],
                                                      in1=rcol[:, kq * 8:(kq + 1) * 8].unsqueeze(2).to_broadcast([128, 8, ncols]), op=ALU.mult),
                     reads=[b_wt[i], cB], writes=[b_wt[i]])
            return wt[i], b_wt[i]

        def projections(tb):
            t0 = tb * TB
            for cg in range(3):
                nmt = 4 if cg < 2 else 2
                for kq in range(4):
                    ws, bws = load_w(cg * 512, nmt * 128, kq, rescale=(cg == 2))
                    for mt in range(nmt):
                        for kci in range(8):
                            kc = kq * 8 + kci
                            P.op("pe", lambda e, mt=mt, kci=kci, kc=kc, ws=ws: e.matmul(
                                pA[mt][:], lhsT=ws[:, kci, mt * 128:(mt + 1) * 128], rhs=xnT[:, kc, :], start=(kc == 0), stop=(kc == 31)),
                                reads=[bws, b_xnT[kc // 8]], writes=[b_pA[mt]])
                for mt in range(nmt):
                    M = cg * 4 + mt
                    if M < 8:
                        dst, bd, scale = qT[:, M, :], b_qT, QSCALE
                    else:
                        gi = M - 8
                        dst, bd, scale = kT[:, gi, t0:t0 + TB], b_kT, 1.0
                        P.op("dve", lambda e, mt=mt, gi=gi: e.tensor_reduce(out=kms[:, gi, :], in_=pA[mt][:].rearrange("p (b t) -> p b t", b=2), axis=AX.X, op=ALU.add),
                             reads=[b_pA[mt]], writes=[b_kms])
                        P.op("dve", lambda e, gi=gi: e.tensor_scalar(out=kmean[:, gi, 2 * tb:2 * tb + 2], in0=kms[:, gi, :], scalar1=1.0 / 256, scalar2=None, op0=ALU.mult),
                             reads=[b_kms], writes=[b_km])
                    if evac_engine() == "act":
                        P.op("act", lambda e, dst=dst, mt=mt, scale=scale: e.activation(out=dst, in_=pA[mt][:], func=AF.Copy, scale=scale),
                             reads=[b_pA[mt]], writes=[bd])
                    else:
                        P.op("dve", lambda e, dst=dst, mt=mt, scale=scale: e.tensor_scalar(out=dst, in0=pA[mt][:], scalar1=scale, scalar2=None, op0=ALU.mult),
                             reads=[b_pA[mt]], writes=[bd])
            for kq in range(4):
                ws, bws = load_w(MQK, MV, kq, rescale=True)
                for tt in range(4):
                    for kci in range(8):
                        kc = kq * 8 + kci
                        P.op("pe", lambda e, tt=tt, kci=kci, kc=kc, ws=ws: e.matmul(
                            pA[tt][:, 0:MV], lhsT=xnT[:, kc, tt * 128:(tt + 1) * 128], rhs=ws[:, kci, 0:MV], start=(kc == 0), stop=(kc == 31)),
                            reads=[bws, b_xnT[kc // 8]], writes=[b_pA[tt]])
            for tt in range(4):
                kt = 4 * tb + tt
                P.op("act", lambda e, tt=tt, kt=kt: e.activation(out=Vaug[:, :, kt, 0:128], in_=pA[tt][:, 0:256].rearrange("p (b d) -> p b d", b=2), func=AF.Copy),
                     reads=[b_pA[tt]], writes=[b_V])
            for cg in range(2):
                for kq in range(4):
                    ws, bws = load_w(MQK + MV + cg * 512, 512, kq)
                    for tt in range(4):
                        for kci in range(8):
                            kc = kq * 8 + kci
                            P.op("pe", lambda e, tt=tt, kci=kci, kc=kc, ws=ws: e.matmul(
                                pA[tt][:], lhsT=xnT[:, kc, tt * 128:(tt + 1) * 128], rhs=ws[:, kci, :], start=(kc == 0), stop=(kc == 31)),
                                reads=[bws, b_xnT[kc // 8]], writes=[b_pA[tt]])
                for tt in range(4):
                    P.op("act", lambda e, tt=tt, cg=cg: e.activation(out=zs[:, tt, cg * 512:(cg + 1) * 512], in_=pA[tt][:], func=AF.Silu),
                         reads=[b_pA[tt]], writes=[b_zs])

        def gating(tb):
            P.dma("sp", mtab[:], mtab_d[:, tb, :], writes=[b_mtab])
            pa = 0
            for hl in range(8):
                gi = hl // 4
                for tt in range(4):
                    c0 = (hl * 4 + tt) * 16
                    P.op("pe", lambda e, hl=hl, tt=tt, gi=gi, c0=c0: e.matmul(pA[pa][:, c0:c0 + 16], lhsT=qT[:, hl, tt * 128:(tt + 1) * 128], rhs=kmean[:, gi, :], start=True, stop=True),
                         reads=[b_qT, b_km], writes=[b_pA[pa]])
            P.op("dve", lambda e: e.tensor_tensor(out=sblk[:], in0=pA[pa][:].rearrange("p (h c) -> p h c", h=8), in1=mtab[:].unsqueeze(1).to_broadcast([128, 8, 64]), op=ALU.add),
                 reads=[b_pA[pa], b_mtab], writes=[b_sblk])
            for hl in range(8):
                for tt in range(4):
                    idx = hl * 4 + tt
                    P.op("dve", lambda e, hl=hl, tt=tt, idx=idx: e.max(out=top[:, idx, :], in_=sblk[:, hl, tt * 16:(tt + 1) * 16]), reads=[b_sblk], writes=[b_top])
            for hl in range(8):
                for tt in range(4):
                    idx = hl * 4 + tt
                    P.op("dve", lambda e, hl=hl, tt=tt, idx=idx: e.tensor_scalar(out=selm[:, hl, tt, :], in0=sblk[:, hl, tt * 16:(tt + 1) * 16], scalar1=top[:, idx, 2:3], scalar2=1.0, op0=ALU.is_ge, op1=ALU.subtract),
                         reads=[b_sblk, b_top], writes=[b_selm])
            for tt in range(4):
                bt = (4 * tb + tt) // 2
                P.op("dve", lambda e, tt=tt, bt=bt: e.memset(selm[:, :, tt, bt:bt + 1], 0.0), writes=[b_selm])
            for h2 in range(4):
                pt = pT[state["ptr"] % 2]; bpt = b_pT[state["ptr"] % 2]; state["ptr"] += 1
                for k in range(8):
                    hl = h2 * 2 + k // 4; tt = k % 4
                    P.op("pe", lambda e, k=k, hl=hl, tt=tt: e.transpose(out=pt[0:16, k, :], in_=selm[:, hl, tt, :], identity=ident[:]),
                         reads=[b_selm, cB], writes=[bpt])
                P.op("dve", lambda e, h2=h2: e.tensor_copy(out=RS[0:16, 2 * h2:2 * h2 + 2, :].rearrange("p h (t c) -> p (h t) c", t=4), in_=pt[0:16, :, :]),
                     reads=[bpt], writes=[b_RS])

        def next_pa():
            i = state["pai"] % 4; state["pai"] += 1
            return i

        def next_pt():
            i = state["pti"] % NPT; state["pti"] += 1
            return i

        def attn(tb):
            qt = tb
            for hl in range(8):
                gi = hl // 4
                ff = [True, True]
                tiles = [(kt, 0, 512, False) for kt in range(4 * qt)] + [(4 * qt + u, 128 * u, 512, True) for u in range(4)]
                pend = []
                LOOK = 2

                def pv_(p_, gi=gi, ff=ff):
                    pti_, a_, bb_, kt_ = p_
                    for i in range(a_ // 128, bb_ // 128):
                        bk, jj = i // 2, i % 2
                        stf = ff[bk]; ff[bk] = False
                        P.op("pe", lambda e, i=i, bk=bk, jj=jj, stf=stf: e.matmul(
                            pO[bk][:, jj, 0:129], lhsT=PT[pti_][:, i * 128:(i + 1) * 128], rhs=Vaug[:, gi, kt_, :], start=stf, stop=False, skip_group_check=True),
                            reads=[b_PT[pti_], b_V], writes=[b_pO[bk]])
                for (kt, a, b_, diag) in tiles:
                    dl = kt - 4 * qt + 28
                    pa = next_pa()
                    P.op("pe", lambda e: e.matmul(pA[pa][:, a:b_], lhsT=kT[:, gi, kt * 128:(kt + 1) * 128], rhs=qT[:, hl, a:b_], start=True, stop=False),
                         reads=[b_kT, b_qT], writes=[b_pA[pa]])
                    P.op("pe", lambda e: e.matmul(pA[pa][:, a:b_], lhsT=EX[0:67, kt, :], rhs=RS[0:67, hl, a:b_], start=False, stop=(not diag)),
                         reads=[cB, b_RS], writes=[b_pA[pa]])
                    if diag:
                        P.op("pe", lambda e: e.matmul(pA[pa][:, a:a + 128], lhsT=ident[:], rhs=tric[:], start=False, stop=True),
                             reads=[cB], writes=[b_pA[pa]])
                    pti = next_pt()
                    P.op("act", lambda e: e.activation(out=PT[pti][:, a:b_], in_=pA[pa][:, a:b_], func=AF.Exp, bias=kb[:, hl, dl:dl + 1], scale=1.0),
                         reads=[b_pA[pa], cB], writes=[b_PT[pti]])
                    pend.append((pti, a, b_, kt))
                    if len(pend) > LOOK:
                        pv_(pend.pop(0))
                for p_ in pend:
                    pv_(p_)
                for bk in range(2):
                    P.op("dve", lambda e, bk=bk: e.tensor_scalar(out=rsb[:, 2 * bk:2 * bk + 2], in0=pO[bk][:, :, 128], scalar1=1e-30, scalar2=None, op0=ALU.max),
                         reads=[b_pO[bk]], writes=[b_rsb])
                P.op("dve", lambda e: e.reciprocal(out=rsb[:, 4:8], in_=rsb[:, 0:4]), reads=[b_rsb], writes=[b_rsb])
                for i in range(4):
                    bk, jj = i // 2, i % 2
                    P.op("dve", lambda e, i=i, bk=bk, jj=jj: e.scalar_tensor_tensor(
                        out=mix[:, i, hl * 128:(hl + 1) * 128], in0=pO[bk][:, jj, 0:128], scalar=rsb[:, 4 + i:5 + i], in1=zs[:, i, hl * 128:(hl + 1) * 128], op0=ALU.mult, op1=ALU.mult),
                        reads=[b_pO[bk], b_rsb, b_zs], writes=[b_mix])

        def store_mix(tb):
            t0 = tb * TB
            for tt in range(4):
                P.dma("pool", mix_d[t0 + tt * 128:t0 + (tt + 1) * 128, :], mix[:, tt, :], reads=[b_mix])

        for tb in range(NTB):
            front_end(tb)
            projections(tb)
            gating(tb)
            attn(tb)
            store_mix(tb)
        if dbg:
            def dump(name, t, bufs):
                d_ = dram("dbg_" + name, list(t.shape), t.dtype, kind="ExternalOutput")
                P.dma("sp", d_, t[:], reads=bufs)
            dump("qT", qT, [b_qT]); dump("kT", kT, [b_kT]); dump("V", Vaug, [b_V]); dump("zs", zs, [b_zs])
            dump("kmean", kmean, [b_km]); dump("sblk", sblk, [b_sblk]); dump("selm", selm, [b_selm]); dump("RS", RS, [b_RS]); dump("top", top, [b_top])
        P.finish()
        print("ninst", P.ninst, "nwaits", P.nwaits, flush=True)
    return nc


def moba_inputs(z, h1b, j):
    m = {"x": np.ascontiguousarray(h1b), "gcol": np.ascontiguousarray(z["b_norm_g"][0].reshape(32, 128).T),
         "gkv": np.ascontiguousarray(z["kv_norm_g"].reshape(32, 128).T), "w": moba_weight(z, j)}
    m.update(moba_tables(j))
    return m


def build_outproj(final, NT=1024):
    nc = bass.Bass("TRN2", target_bir_lowering=False)
    mixin = nc.dram_tensor("mixin", [NT, D], BF16, kind="ExternalInput").ap()
    res = nc.dram_tensor("res", [NT, D], F32, kind="ExternalInput").ap()
    w = nc.dram_tensor("w", [128, 32, D], F32, kind="ExternalInput").ap()
    idn = nc.dram_tensor("idn", [128, 128], BF16, kind="ExternalInput").ap()
    if final:
        gfin = nc.dram_tensor("gfin", [128, D], F32, kind="ExternalInput").ap()
    hout = nc.dram_tensor("hout", [NT, D], F32, kind="ExternalOutput").ap()
    with ExitStack() as st:
        P = Prog(nc, st)
        sb = lambda name, shape, dt: st.enter_context(nc.sbuf_tensor("s_" + name, shape, dt))
        ps = lambda name, shape, dt: st.enter_context(nc.psum_tensor("p_" + name, shape, dt))
        ident = sb("ident", [128, 128], BF16); cB = Buf()
        P.dma("sp", ident[:], idn, writes=[cB])
        if final:
            grep_ = sb("grep", [128, D], F32); b_grep = Buf()
            P.dma("sp", grep_[:], gfin, writes=[b_grep])
            ss = sb("ss", [128, 12], F32); b_ss = Buf()
            junk = sb("junk", [128, D], BF16); b_junk = Buf()
            epst = sb("epst", [128, 1], F32)
            P.op("dve", lambda e: e.memset(epst[:], EPS), writes=[cB])
        mixtok = [sb("mixtok%d" % i, [128, D], BF16) for i in range(2)]; b_mixtok = [Buf() for _ in range(2)]
        mixT = sb("mixT", [128, 32, 512], BF16); b_mixT = [Buf() for _ in range(4)]
        hb = [sb("hb%d" % i, [128, D], F32) for i in range(4)]; b_hb = [Buf() for _ in range(4)]
        NW = 3
        wt = [sb("wt%d" % i, [128, 16, 512], BF16) for i in range(NW)]; b_wt = [Buf() for _ in range(NW)]
        pacc = [ps("pacc%d" % i, [128, 512], F32) for i in range(4)]; b_pacc = [Buf("", True) for _ in range(4)]
        ptr = [ps("ptr%d" % i, [128, 8, 128], BF16) for i in range(2)]; b_ptr = [Buf("", True) for _ in range(2)]
        wi = 0
        tri = 0
        for half in range(NT // 512):
            t0 = half * 512
            for tt in range(4):
                P.dma("sp", hb[tt][:], res[t0 + tt * 128: t0 + (tt + 1) * 128, :], writes=[b_hb[tt]])
            for tt in range(4):
                mt = mixtok[tt % 2]; bmt = b_mixtok[tt % 2]
                P.dma("sp", mt[:], mixin[t0 + tt * 128: t0 + (tt + 1) * 128, :], writes=[bmt])
                for k8 in range(4):
                    pt = ptr[tri % 2]; bpt = b_ptr[tri % 2]; tri += 1
                    for j in range(8):
                        kc = k8 * 8 + j
                        P.op("pe", lambda e, pt=pt, j=j, kc=kc, mt=mt: e.transpose(out=pt[:, j, :], in_=mt[:, kc * 128:(kc + 1) * 128], identity=ident[:]),
                             reads=[bmt, cB], writes=[bpt])
                    if k8 % 2 == 0:
                        P.op("dve", lambda e, pt=pt, k8=k8, tt=tt: e.tensor_copy(out=mixT[:, k8 * 8:(k8 + 1) * 8, tt * 128:(tt + 1) * 128], in_=pt[:]),
                             reads=[bpt], writes=[b_mixT[k8]])
                    else:
                        P.op("act", lambda e, pt=pt, k8=k8, tt=tt: e.copy(out=mixT[:, k8 * 8:(k8 + 1) * 8, tt * 128:(tt + 1) * 128], in_=pt[:]),
                             reads=[bpt], writes=[b_mixT[k8]])
            for cg in range(8):
                for kh in range(2):
                    ws = wt[wi % NW]; bws = b_wt[wi % NW]; wi += 1
                    P.dma("pool", ws[:], w[:, kh * 16:(kh + 1) * 16, cg * 512:(cg + 1) * 512], writes=[bws])
                    for tt in range(4):
                        for kci in range(16):
                            kc = kh * 16 + kci
                            P.op("pe", lambda e, tt=tt, kci=kci, kc=kc, ws=ws: e.matmul(pacc[tt][:], lhsT=mixT[:, kc, tt * 128:(tt + 1) * 128], rhs=ws[:, kci, :], start=(kc == 0), stop=(kc == 31)),
                                 reads=[b_mixT[kc // 8], bws], writes=[b_pacc[tt]])
                for tt in range(4):
                    P.op("dve", lambda e, tt=tt, cg=cg: e.tensor_tensor(out=hb[tt][:, cg * 512:(cg + 1) * 512], in0=pacc[tt][:], in1=hb[tt][:, cg * 512:(cg + 1) * 512], op=ALU.add),
                         reads=[b_pacc[tt], b_hb[tt]], writes=[b_hb[tt]])
            for tt in range(4):
                if final:
                    P.op("dve", lambda e, tt=tt: e.memset(ss[:, tt:tt + 1], 0.0), writes=[b_ss])
                    P.op("act", lambda e, tt=tt: e.activation(out=junk[:], in_=hb[tt][:], func=AF.Square, accum_out=ss[:, tt:tt + 1]),
                         reads=[b_hb[tt]], writes=[b_junk, b_ss])
                    P.op("act", lambda e, tt=tt: e.activation(out=ss[:, 4 + tt:5 + tt], in_=ss[:, tt:tt + 1], func=AF.Ln, bias=epst[:], scale=1.0 / D),
                         reads=[b_ss, cB], writes=[b_ss])
                    P.op("act", lambda e, tt=tt: e.activation(out=ss[:, 8 + tt:9 + tt], in_=ss[:, 4 + tt:5 + tt], func=AF.Exp, scale=-0.5),
                         reads=[b_ss], writes=[b_ss])
                    P.op("dve", lambda e, tt=tt: e.scalar_tensor_tensor(out=hb[tt][:], in0=hb[tt][:], scalar=ss[:, 8 + tt:9 + tt], in1=grep_[:], op0=ALU.mult, op1=ALU.mult),
                         reads=[b_hb[tt], b_ss, b_grep], writes=[b_hb[tt]])
                P.dma("sp", hout[t0 + tt * 128: t0 + (tt + 1) * 128, :], hb[tt][:], reads=[b_hb[tt]])
        P.finish()
    return nc


_CACHE = {}


def _prog(key, fn):
    return fn()


def kernel(**z):
    z = {k: np.asarray(v) for k, v in z.items()}
    cores = [(b, g) for b in range(2) for g in range(4)]
    idn = np.eye(128, dtype=np.float32).astype(NPBF)
    maps = [nsa_inputs(z, b, g) for (b, g) in cores]
    resA = run_bass_kernel_spmd(_prog("nsa", build_nsa), maps, core_ids=list(range(8)))
    del maps
    mixA = [np.asarray(r["mix"]) for r in resA.results]
    wl = w_layout(z["a_w_out"][0])
    maps = []
    for (b, r) in cores:
        mixin = np.concatenate([mixA[b * 4 + g][r * 1024:(r + 1) * 1024] for g in range(4)], axis=1)
        maps.append({"mixin": np.ascontiguousarray(mixin), "res": np.ascontiguousarray(z["x"][b, r * 1024:(r + 1) * 1024]), "w": wl, "idn": idn})
    resB = run_bass_kernel_spmd(_prog("opB", lambda: build_outproj(False)), maps, core_ids=list(range(8)))
    del maps
    h1 = np.stack([np.asarray(r["hout"]) for r in resB.results]).reshape(2, S, D)
    maps = [moba_inputs(z, h1[b], j) for (b, j) in cores]
    resC = run_bass_kernel_spmd(_prog("moba", build_moba), maps, core_ids=list(range(8)))
    del maps
    mixC = [np.asarray(r["mix"]) for r in resC.results]
    wl = w_layout(z["b_w_out"][0])
    gf = np.ascontiguousarray(np.broadcast_to(z["final_norm_g"].reshape(1, D), (128, D))).astype(np.float32)
    maps = []
    for (b, r) in cores:
        mixin = np.concatenate([mixC[b * 4 + j][r * 1024:(r + 1) * 1024] for j in range(4)], axis=1)
        maps.append({"mixin": np.ascontiguousarray(mixin), "res": np.ascontiguousarray(h1[b, r * 1024:(r + 1) * 1024]), "w": wl, "idn": idn, "gfin": gf})
    resD = run_bass_kernel_spmd(_prog("opD", lambda: build_outproj(True)), maps, core_ids=list(range(8)))
    out = np.stack([np.asarray(r["hout"]) for r in resD.results]).reshape(2, S, D)
    return out.astype(np.float32)
```
